# Optimizing a Trainium2 kernel written in Bass

```python
import jax, jax.numpy as jnp
from jax import lax
import numpy as np

D_MODEL = 1024
BATCH = 4
SEQ = 8192
DEPTH = 1

CTX_LEN = 256
GRID_W = 64
HG_DK = 128
HG_DV = 128
HG_HEADS = (D_MODEL // 2) // HG_DV
ML_DK = 128
ML_DV = 128
ML_HEADS = (D_MODEL // 2) // ML_DV
HG_KW = HG_HEADS * HG_DK
HG_VW = HG_HEADS * HG_DV
ML_KW = ML_HEADS * ML_DK
ML_VW = ML_HEADS * ML_DV
MIX_WIDTH = HG_VW + ML_VW
HG_CHUNK = 32
ML_CHUNK = 64
CONV_K = 3
N_EXPERTS = 16
EC_CAPACITY = 2
EXPERT_FF = 1024
NORM_EPS = 1e-6

HG_Q = 0
HG_I = HG_Q + HG_KW
HG_G = HG_I + HG_VW
HG_FF = HG_G + HG_VW
ML_Q = HG_FF + 2 * HG_KW
ML_V = ML_Q + 2 * ML_KW
ML_O = ML_V + ML_VW
ML_GATES = ML_O + ML_VW
PROJ_OUT = ML_GATES + 4 * ML_HEADS

kernel_name = "hgrn2_mlstm_ec_moe_diffusion_block"


def rms_norm(x, w):
    xf = x.astype(jnp.float32)
    y = xf * lax.rsqrt(jnp.mean(xf * xf, axis=-1, keepdims=True) + NORM_EPS)
    return (y * w).astype(x.dtype)


def _head_rms_norm(t, w, n_heads):
    B, T, W = t.shape
    tf = t.astype(jnp.float32).reshape(B, T, n_heads, -1)
    tf = tf * lax.rsqrt(jnp.mean(tf * tf, axis=-1, keepdims=True) + NORM_EPS)
    return (tf.reshape(B, T, W) * w).astype(t.dtype)


def _head_layer_norm(t, w, n_heads):
    B, T, W = t.shape
    tf = t.astype(jnp.float32).reshape(B, T, n_heads, -1)
    tf = tf - jnp.mean(tf, axis=-1, keepdims=True)
    tf = tf * lax.rsqrt(jnp.mean(tf * tf, axis=-1, keepdims=True) + NORM_EPS)
    return (tf.reshape(B, T, W) * w).astype(t.dtype)


def _to_heads(t, n_heads):
    B, T, _ = t.shape
    return t.reshape(B, T, n_heads, -1).transpose(0, 2, 1, 3)


def _from_heads(t):
    B, H, T, d = t.shape
    return t.transpose(0, 2, 1, 3).reshape(B, T, H * d)


def _to_chunks(t, L):
    B, H, T = t.shape[:3]
    return jnp.moveaxis(t.reshape(B, H, T // L, L, *t.shape[3:]), 2, 0)


def _from_chunks(t):
    N, B, H, L = t.shape[:4]
    return jnp.moveaxis(t, 0, 2).reshape(B, H, N * L, *t.shape[4:])


def hgrn2_scan(q, k, v, log_f, s0):
    L = HG_CHUNK
    mask = jnp.tril(jnp.ones((L, L), dtype=bool))
    f32 = jnp.float32

    def step(s, inp):
        qc, kc, vc, gc = inp
        a = jnp.cumsum(gc, axis=2)
        diff = a[:, :, :, None, :] - a[:, :, None, :, :]
        decay = jnp.exp(jnp.where(mask[:, :, None], diff, -jnp.inf))
        scores = jnp.einsum('bhid,bhjd,bhijd->bhij', qc, kc, decay)
        o = jnp.einsum('bhij,bhjv->bhiv', scores, vc) + jnp.einsum('bhid,bhdv->bhiv', qc * jnp.exp(a), s)
        a_last = a[:, :, -1:, :]
        s_new = jnp.exp(a_last[:, :, 0, :])[..., None] * s + jnp.einsum('bhjd,bhjv->bhdv', kc * jnp.exp(a_last - a), vc)
        return s_new, o

    xs = (_to_chunks(q.astype(f32), L), _to_chunks(k.astype(f32), L), _to_chunks(v.astype(f32), L), _to_chunks(log_f.astype(f32), L))
    s_fin, o = lax.scan(step, s0, xs)
    return _from_chunks(o).astype(v.dtype), s_fin


def mlstm_scan(q, k, v, log_i, log_f, state):
    L = ML_CHUNK
    mask = jnp.tril(jnp.ones((L, L), dtype=bool))
    f32 = jnp.float32

    def step(carry, inp):
        c, n, m = carry
        qc, kc, vc, ic, fc = inp
        b = jnp.cumsum(fc, axis=-1)
        d = jnp.where(mask, b[..., :, None] - b[..., None, :] + ic[..., None, :], -jnp.inf)
        inter = b + m[..., None]
        m_row = jnp.maximum(inter, jnp.max(d, axis=-1))
        w_inter = jnp.exp(inter - m_row)
        qk = jnp.einsum('bhid,bhjd->bhij', qc, kc) * jnp.exp(d - m_row[..., None])
        num = jnp.einsum('bhij,bhjv->bhiv', qk, vc) + w_inter[..., None] * jnp.einsum('bhid,bhdv->bhiv', qc, c)
        den = jnp.sum(qk, axis=-1) + w_inter * jnp.einsum('bhid,bhd->bhi', qc, n)
        h = num / jnp.maximum(jnp.abs(den), jnp.exp(-m_row))[..., None]
        m_new = m_row[..., -1]
        wk = jnp.exp(b[..., -1:] - b + ic - m_new[..., None])
        ws = jnp.exp(b[..., -1] + m - m_new)
        c_new = ws[..., None, None] * c + jnp.einsum('bhj,bhjd,bhjv->bhdv', wk, kc, vc)
        n_new = ws[..., None] * n + jnp.einsum('bhj,bhjd->bhd', wk, kc)
        return (c_new, n_new, m_new), h

    xs = (_to_chunks(q.astype(f32), L), _to_chunks(k.astype(f32), L), _to_chunks(v.astype(f32), L),
          _to_chunks(log_i.astype(f32), L), _to_chunks(log_f.astype(f32), L))
    st, h = lax.scan(step, state, xs)
    return _from_chunks(h).astype(v.dtype), st


def _short_conv(t, conv_w, conv_b, on_grid):
    B, T, C = t.shape
    w = conv_w.astype(t.dtype)
    if on_grid:
        rows = T // GRID_W
        y = lax.conv_general_dilated(t.reshape(B, rows, GRID_W, C), w[:, :, None, :], (1, 1), 'SAME',
                                     dimension_numbers=('NHWC', 'HWIO', 'NHWC'), feature_group_count=C)
        y = y.reshape(B, T, C)
    else:
        y = lax.conv_general_dilated(t, w[CONV_K // 2][:, None, :], (1,), 'SAME',
                                     dimension_numbers=('NWC', 'WIO', 'NWC'), feature_group_count=C)
    return jax.nn.silu(y + conv_b)


def _prep(u, w_in, conv_w, conv_b, lb, ml_gate_b, on_grid):
    B, T, _ = u.shape
    p = jnp.einsum('btd,dk->btk', u, w_in)
    f_logit = p[..., HG_FF:ML_Q].astype(jnp.float32).reshape(B, T, 2, HG_KW)
    f = lb + (1.0 - lb) * jax.nn.sigmoid(f_logit)
    qk = _short_conv(p[..., ML_Q:ML_V], conv_w, conv_b, on_grid)
    gates = (p[..., ML_GATES:].astype(jnp.float32) + ml_gate_b).reshape(B, T, 2, 2, ML_HEADS)
    gates = jnp.transpose(gates, (2, 3, 0, 4, 1))
    return dict(
        hg_q=_to_heads(jax.nn.silu(p[..., HG_Q:HG_I]), HG_HEADS),
        hg_v=_to_heads(p[..., HG_I:HG_G], HG_HEADS),
        hg_g=p[..., HG_G:HG_FF],
        hg_k=[_to_heads(1.0 - f[:, :, d], HG_HEADS) for d in range(2)],
        hg_logf=[_to_heads(jnp.log(f[:, :, d]), HG_HEADS) for d in range(2)],
        ml_q=_to_heads(qk[..., :ML_KW], ML_HEADS),
        ml_k=_to_heads(qk[..., ML_KW:], ML_HEADS) * (ML_DK ** -0.5),
        ml_v=_to_heads(p[..., ML_V:ML_O], ML_HEADS),
        ml_o=p[..., ML_O:ML_GATES],
        ml_logi=gates[:, 0],
        ml_logf=jax.nn.log_sigmoid(gates[:, 1]),
    )


def _scan_direction(pr, d, hg_s0, ml_s0):
    fl = (lambda a: jnp.flip(a, axis=2)) if d == 1 else (lambda a: a)
    hg_o, hg_s = hgrn2_scan(fl(pr['hg_q']), fl(pr['hg_k'][d]), fl(pr['hg_v']), fl(pr['hg_logf'][d]), hg_s0)
    ml_h, ml_s = mlstm_scan(fl(pr['ml_q']), fl(pr['ml_k']), fl(pr['ml_v']), fl(pr['ml_logi'][d]), fl(pr['ml_logf'][d]), ml_s0)
    return fl(hg_o), hg_s, fl(ml_h), ml_s


def _zero_states(B):
    f32 = jnp.float32
    hg = jnp.zeros((B, HG_HEADS, HG_DK, HG_DV), f32)
    ml = (jnp.zeros((B, ML_HEADS, ML_DK, ML_DV), f32), jnp.zeros((B, ML_HEADS, ML_DK), f32), jnp.zeros((B, ML_HEADS), f32))
    return hg, ml


def _mixer_out(pr, hg_o, ml_h, hg_norm_w, ml_norm_w, w_out):
    hg = _head_rms_norm(_from_heads(hg_o), hg_norm_w, HG_HEADS) * jax.nn.silu(pr['hg_g'])
    ml = _head_layer_norm(_from_heads(ml_h), ml_norm_w, ML_HEADS) * jax.nn.sigmoid(pr['ml_o'])
    return jnp.einsum('btk,kd->btd', jnp.concatenate([hg, ml], axis=-1), w_out)


def ec_moe(u, router_w, w_gate, w_up, w_down):
    B, n, D = u.shape
    cap = EC_CAPACITY * n // N_EXPERTS
    aff = jax.nn.softmax(jnp.einsum('bnd,de->bne', u, router_w).astype(jnp.float32), axis=-1)
    gate, idx = lax.top_k(jnp.swapaxes(aff, 1, 2), cap)
    xs = jax.vmap(lambda t, i: t[i])(u, idx)
    h = jax.nn.silu(jnp.einsum('becd,edf->becf', xs, w_gate)) * jnp.einsum('becd,edf->becf', xs, w_up)
    y = jnp.einsum('becf,efd->becd', h, w_down) * gate[..., None].astype(u.dtype)
    return jax.vmap(lambda ye, i: jnp.zeros((n, D), ye.dtype).at[i.reshape(-1)].add(ye.reshape(-1, D)))(y, idx)


def setup_inputs(seed: int = 0) -> dict:
    key = jax.random.key(seed)
    ks = jax.random.split(key, 24)
    f32 = jnp.float32
    nrm = lambda k, shape, s: s * jax.random.normal(k, shape, f32)
    D = D_MODEL
    gate_base = jnp.stack([jnp.zeros((ML_HEADS,), f32), jnp.linspace(3.0, 6.0, ML_HEADS, dtype=f32)])
    ml_gate_b = (jnp.broadcast_to(gate_base, (DEPTH, 2, 2, ML_HEADS)) + nrm(ks[10], (DEPTH, 2, 2, ML_HEADS), 0.1)).reshape(DEPTH, 4 * ML_HEADS)
    return {
        "x": nrm(ks[0], (BATCH, SEQ, D), 1.0),
        "c": nrm(ks[1], (BATCH, D), 1.0),
        "ctx": nrm(ks[2], (BATCH, CTX_LEN, D), 1.0),
        "c_ctx": nrm(ks[3], (D,), 1.0),
        "ada_w": nrm(ks[4], (DEPTH, D, 6 * D), 0.5 * D ** -0.5),
        "ada_b": nrm(ks[5], (DEPTH, 6 * D), 0.02),
        "norm1_w": 1.0 + nrm(ks[6], (DEPTH, D), 0.02),
        "w_in": nrm(ks[7], (DEPTH, D, PROJ_OUT), D ** -0.5),
        "conv_w": nrm(ks[8], (DEPTH, CONV_K, CONV_K, 2 * ML_KW), 1.0 / CONV_K),
        "conv_b": nrm(ks[9], (DEPTH, 2 * ML_KW), 0.02),
        "hg_lb_logits": nrm(ks[11], (DEPTH + 1, 2, HG_KW), 0.5),
        "ml_gate_b": ml_gate_b,
        "hg_norm_w": 1.0 + nrm(ks[12], (DEPTH, HG_VW), 0.02),
        "ml_norm_w": 1.0 + nrm(ks[13], (DEPTH, ML_VW), 0.02),
        "w_out": nrm(ks[14], (DEPTH, MIX_WIDTH, D), MIX_WIDTH ** -0.5),
        "norm2_w": 1.0 + nrm(ks[15], (DEPTH, D), 0.02),
        "router_w": nrm(ks[16], (DEPTH, D, N_EXPERTS), D ** -0.5),
        "exp_w_gate": nrm(ks[17], (DEPTH, N_EXPERTS, D, EXPERT_FF), D ** -0.5),
        "exp_w_up": nrm(ks[18], (DEPTH, N_EXPERTS, D, EXPERT_FF), D ** -0.5),
        "exp_w_down": nrm(ks[19], (DEPTH, N_EXPERTS, EXPERT_FF, D), EXPERT_FF ** -0.5),
        "final_norm_w": 1.0 + nrm(ks[20], (D,), 0.02),
    }


def reference(x, c, ctx, c_ctx, ada_w, ada_b, norm1_w, w_in, conv_w, conv_b, hg_lb_logits, ml_gate_b,
              hg_norm_w, ml_norm_w, w_out, norm2_w, router_w, exp_w_gate, exp_w_up, exp_w_down, final_norm_w):
    B = x.shape[0]
    lb_all = jnp.cumsum(jax.nn.softmax(hg_lb_logits.astype(jnp.float32), axis=0), axis=0)
    silu_c = jax.nn.silu(c)
    silu_cc = jax.nn.silu(c_ctx)
    for l in range(DEPTH):
        last = l == DEPTH - 1
        mod_x = jnp.split(silu_c @ ada_w[l] + ada_b[l], 6, axis=-1)
        mod_c = jnp.split(silu_cc @ ada_w[l] + ada_b[l], 6, axis=-1)
        sh1, sc1, g1, sh2, sc2, g2 = [m[:, None, :] for m in mod_x]
        csh1, csc1, cg1, csh2, csc2, cg2 = mod_c
        lb = lb_all[l]
        u_x = rms_norm(x, norm1_w[l]) * (1.0 + sc1) + sh1
        u_c = rms_norm(ctx, norm1_w[l]) * (1.0 + csc1) + csh1
        pr_c = _prep(u_c, w_in[l], conv_w[l], conv_b[l], lb, ml_gate_b[l], False)
        pr_x = _prep(u_x, w_in[l], conv_w[l], conv_b[l], lb, ml_gate_b[l], True)
        hg0, ml0 = _zero_states(B)
        c_hg_f, c_hs_f, c_ml_f, c_ms_f = _scan_direction(pr_c, 0, hg0, ml0)
        c_hg_b, c_hs_b, c_ml_b, c_ms_b = _scan_direction(pr_c, 1, hg0, ml0)
        x_hg_f, _, x_ml_f, _ = _scan_direction(pr_x, 0, c_hs_f, c_ms_f)
        x_hg_b, _, x_ml_b, _ = _scan_direction(pr_x, 1, c_hs_b, c_ms_b)
        x = x + g1 * _mixer_out(pr_x, x_hg_f + x_hg_b, x_ml_f + x_ml_b, hg_norm_w[l], ml_norm_w[l], w_out[l])
        if not last:
            ctx = ctx + cg1 * _mixer_out(pr_c, c_hg_f + c_hg_b, c_ml_f + c_ml_b, hg_norm_w[l], ml_norm_w[l], w_out[l])
        v_x = rms_norm(x, norm2_w[l]) * (1.0 + sc2) + sh2
        x = x + g2 * ec_moe(v_x, router_w[l], exp_w_gate[l], exp_w_up[l], exp_w_down[l])
        if not last:
            v_c = rms_norm(ctx, norm2_w[l]) * (1.0 + csc2) + csh2
            ctx = ctx + cg2 * ec_moe(v_c, router_w[l], exp_w_gate[l], exp_w_up[l], exp_w_down[l])
    return rms_norm(x, final_norm_w)
```

```python
import numpy as np
from contextlib import ExitStack
import concourse.bass as bass
import concourse.mybir as mybir
from concourse.bass_utils import run_bass_kernel_spmd

F32 = mybir.dt.float32
BF16 = mybir.dt.bfloat16
U32 = mybir.dt.uint32
I32 = mybir.dt.int32
ALU = mybir.AluOpType
AF = mybir.ActivationFunctionType
AX = mybir.AxisListType

D = 1024
SEQ = 8192
CTX = 256
T = SEQ + CTX
NT = T // 128
NXT = SEQ // 128
PROJ = 4624
NE = 16
CAP = 1024
EPS = 1e-6
ENG = ['pe', 'act', 'dve', 'pool', 'sp']
import os as _os
P5MODE = _os.environ.get('P5MODE', 'hg,ml')
FASTSIM = _os.environ.get('FASTSIM', '') == '1'


class Res:
    __slots__ = ('name', 'w', 'r', 'excl')

    def __init__(self, name='', excl=False):
        self.name = name
        self.w = None
        self.r = {}
        self.excl = excl


class Sched:
    def __init__(self, nc, es):
        self.nc = nc
        self.es = es
        self.prog = {e: [] for e in ENG}
        self.cnt = {e: 0 for e in ENG}
        self.sem = {}
        for e in ENG:
            self.sem[e] = es.enter_context(nc.semaphore('S_' + e))
        self.waited = {e: {} for e in ENG}
        self.dcnt = {}
        self.nops = 0
        self.chanmap = {}
        self.freeslots = []

    def _need(self, eng, ev, waits):
        if ev is None:
            return
        k, v = ev
        if k == eng and eng == 'pe':
            return
        if self.waited[eng].get(k, 0) >= v:
            return
        if waits.get(k, 0) < v:
            waits[k] = v

    def op(self, eng, fn, reads=(), writes=(), chan=None):
        if any(r.excl for r in reads):
            writes = list(writes) + [r for r in reads if r.excl and r not in writes]
            reads = [r for r in reads if not r.excl]
        waits = {}
        for r in reads:
            self._need(eng, r.w, waits)
        for w in writes:
            self._need(eng, w.w, waits)
            for k, v in w.r.items():
                self._need(eng, (k, v), waits)
        if chan is not None:
            if chan not in self.chanmap:
                if self.freeslots:
                    self.chanmap[chan] = self.freeslots.pop()
                else:
                    slot = 'slot%d' % len(self.sem)
                    self.sem[slot] = self.es.enter_context(self.nc.semaphore('D_%d' % len(self.sem)))
                    self.dcnt[slot] = 0
                    self.chanmap[chan] = slot
            chan = self.chanmap[chan]
            if self.dcnt[chan] > 0:
                self._need(eng, (chan, self.dcnt[chan]), waits)
            self.dcnt[chan] += 16
            ev = (chan, self.dcnt[chan])
            inc = 16
        else:
            self.cnt[eng] += 1
            ev = (eng, self.cnt[eng])
            inc = 1
        for k, v in waits.items():
            self.prog[eng].append(('w', k, v))
            self.waited[eng][k] = v
        self.prog[eng].append(('o', fn, ev[0], inc))
        self.nops += 1
        for r in reads:
            if r.r.get(ev[0], 0) < ev[1]:
                r.r[ev[0]] = ev[1]
        for w in writes:
            w.w = ev
            w.r = {}
        return ev

    def barrier(self):
        for e in ENG:
            for k in list(self.sem.keys()):
                v = self.cnt[k] if k in self.cnt else self.dcnt.get(k, 0)
                if v > 0 and k != e and self.waited[e].get(k, 0) < v:
                    self.prog[e].append(('w', k, v))
                    self.waited[e][k] = v
        self.freeslots = sorted(set(self.chanmap.values()) | set(self.freeslots))
        self.chanmap = {}

    def emit(self):
        nc = self.nc
        for k in list(self.sem.keys()):
            v = self.cnt[k] if k in self.cnt else self.dcnt.get(k, 0)
            if v > 0 and k != 'sp':
                self.prog['sp'].append(('w', k, v))
        sem = self.sem
        prog = self.prog

        def run(name):
            def f(eng):
                for it in prog[name]:
                    if it[0] == 'w':
                        eng.wait_ge(sem[it[1]], it[2])
                    else:
                        it[1](eng).then_inc(sem[it[2]], it[3])
            return f

        with nc.Block() as block:
            block.tensor(run('pe'))
            block.scalar(run('act'))
            block.vector(run('dve'))
            block.gpsimd(run('pool'))
            block.sync(run('sp'))


class Alloc:
    def __init__(self, nc, lo=17408, hi=229376):
        self.nc = nc
        self.lo = lo
        self.hi = hi
        self.cur = lo
        self.n = 0

    def mark(self):
        return self.cur

    def reset(self, m):
        self.cur = m

    def tile(self, shape, dtype, name=None):
        esz = 2 if dtype == BF16 else 4
        n = 1
        for s in shape[1:]:
            n *= s
        nbytes = (n * esz + 63) // 64 * 64
        assert self.cur + nbytes <= self.hi, ('SBUF overflow', name, self.cur, nbytes)
        self.n += 1
        t = self.nc.alloc_sbuf_tensor_at('%s_%d' % (name or 't', self.n), list(shape), dtype, offset=self.cur)
        self.cur += nbytes
        return t


def build(stage=99, debug=()):
    nc = bass.Bass("TRN2", target_bir_lowering=False)
    es = ExitStack()
    S = Sched(nc, es)
    A = Alloc(nc)
    dbg = set(debug)

    def dram_in(name, shape, dt=F32):
        return nc.dram_tensor(name, list(shape), dt, kind="ExternalInput").ap()

    def dram_scr(name, shape, dt=F32):
        kind = "ExternalOutput" if name in dbg else "Internal"
        return nc.dram_tensor(name, list(shape), dt, kind=kind).ap()

    xc = dram_in("xc", [T, D])
    cvec = dram_in("cvec", [128, 8, 2])
    ada_w = dram_in("ada_w", [D, 6 * D])
    ada_b2 = dram_in("ada_b2", [2, 6 * D])
    n1w = dram_in("n1w", [128, 8])
    w_in = dram_in("w_in", [D, PROJ])
    cident = dram_in("cident", [128, 128])
    csel = dram_in("csel", [2, 128])
    lbl = dram_in("lbl", [128, 4 * 512])
    gateb = dram_in("gateb", [16, 1])
    gatebrow = dram_in("gatebrow", [128, 16])
    jm_in = dram_in("jm_in", [128, 128])
    convw = dram_in("convw", [128, 8, 9])
    convb = dram_in("convb", [128, 8])
    maskin = dram_in("maskin", [128, 2, 128])
    trixin = dram_in("trixin", [128, 2, 132])
    selhin = dram_in("selhin", [4, 4, 128])
    bd_in = dram_in("bd_in", [128, 128])
    lt_in = dram_in("lt_in", [128, 128])
    sv_in = dram_in("sv_in", [128, 8])
    wg_in = dram_in("wg_in", [NE, D, D])
    wu_in = dram_in("wu_in", [NE, D, D])
    wd_in = dram_in("wd_in", [NE, D, D])
    fnw_in = dram_in("fnw_in", [128, D])
    hgnw_in = dram_in("hgnw_in", [128, 512])
    mlnw_in = dram_in("mlnw_in", [128, 512])
    n2r_in = dram_in("n2r_in", [128, D])
    rw_in = dram_in("rw_in", [D, 16])
    wout_in = dram_in("wout_in", [D, D])
    out = nc.dram_tensor("out", [SEQ if stage >= 99 else 128, D], F32, kind="ExternalOutput").ap()

    MODd = nc.dram_tensor("MODd", [2, 6 * D], F32, kind=("ExternalOutput" if "MODd" in dbg else "Internal")).ap()
    QTH = dram_scr("QTH", [512, T], BF16)
    PRE = dram_scr("PRE", [1024, T], BF16)
    GATES = dram_scr("GATES", [16, T], F32)
    GATEST = dram_scr("GATEST", [T, 16], F32)
    HGV = dram_scr("HGV", [T, 512], BF16)
    MLV = dram_scr("MLV", [T, 512], BF16)
    GG = [dram_scr("GG%d" % d, [T, 512], F32) for d in range(2)]
    KK = [dram_scr("KK%d" % d, [T, 512], BF16) for d in range(2)]
    HGG = dram_scr("HGG", [T, 512], BF16)
    MLO = dram_scr("MLO", [T, 512], BF16)
    QKT = dram_scr("QKT", [1024, T], BF16)
    TOKd = dram_scr("TOKd", [128, NT * 12], F32)
    OH = [dram_scr("OH%d" % d, [SEQ, 512], F32) for d in range(2)]
    XMID = dram_scr("XMID", [SEQ, D], F32)
    MOE = dram_scr("MOE", [SEQ, D], F32)
    CUMd2 = dram_scr("CUMd", [128, 1024], F32)
    CUMd3 = CUMd2.rearrange("a (t r) -> (a t) r", r=128)
    CTd = dram_scr("CTd", [128, 8], F32)
    TIDXd = dram_scr("TIDXd", [128, 128], U32)
    GATEd = dram_scr("GATEd", [128, 128], F32)
    VX = dram_scr("VX", [SEQ, D], BF16)
    AFF = dram_scr("AFF", [SEQ, 16], F32)
    AFFTd = dram_scr("AFFTd", [16, SEQ], F32)
    OM = [dram_scr("OM%d" % d, [SEQ, 512], F32) for d in range(2)]

    def R(name=''):
        return Res(name)

    ident = A.tile([128, 128], F32, 'ident')
    identb = A.tile([128, 128], BF16, 'identb')
    sel2 = A.tile([2, 128], F32, 'sel2')
    r_ident, r_identb, r_sel2 = R(), R(), R()
    S.op('sp', lambda e: e.dma_start(out=ident[:], in_=cident), writes=[r_ident], chan='c_ident')
    S.op('sp', lambda e: e.dma_start(out=sel2[:], in_=csel), writes=[r_sel2], chan='c_sel2')
    S.op('dve', lambda e: e.tensor_copy(out=identb[:], in_=ident[:]), reads=[r_ident], writes=[r_identb])

    epst = A.tile([128, 1], F32, 'epst')
    r_epst = R()
    S.op('dve', lambda e: e.memset(epst[:], EPS), writes=[r_epst])
    m_keep = A.mark()
    MODT = A.tile([128, 4, 8, 2], F32, 'MODT')
    n1t = A.tile([128, 8], F32, 'n1t')
    scale1 = A.tile([128, 8, 2], F32, 'scale1')
    lbr = A.tile([128, 2, 512], F32, 'lbr')
    omlb = A.tile([128, 2, 512], F32, 'omlb')
    gbt = A.tile([16, 1], F32, 'gbt')
    gbrow = A.tile([128, 16], F32, 'gbrow')
    Jm = A.tile([128, 128], F32, 'Jm')
    m1 = A.mark()
    cv = A.tile([128, 8, 2], F32, 'cv')
    scv = A.tile([128, 8, 2], F32, 'scv')
    adab = A.tile([2, 6 * D], F32, 'adab')
    MOD = A.tile([2, 6 * D], F32, 'MOD')
    lbt = A.tile([128, 4 * 512], F32, 'lbt')
    r_cv, r_scv, r_adab, r_MOD = R(), R(), R(), R()
    S.op('sp', lambda e: e.dma_start(out=cv[:], in_=cvec), writes=[r_cv], chan='c_cv')
    S.op('sp', lambda e: e.dma_start(out=adab[:], in_=ada_b2), writes=[r_adab], chan='c_adab')
    S.op('act', lambda e: e.activation(out=scv[:], in_=cv[:], func=AF.Silu), reads=[r_cv], writes=[r_scv])
    awb = [A.tile([128, 8, 512], F32, 'awb') for _ in range(2)]
    r_awb = [R(), R()]
    ps_mod = [nc.alloc_psum_tensor('ps_mod%d' % i, [128, 512], F32) for i in range(2)]
    r_psmod = [Res('psmod0', True), Res('psmod1', True)]
    ada_v = ada_w.rearrange("(k p) c -> p k c", p=128)
    for cb in range(13):
        if cb < 12:
            b = cb % 2
            S.op('sp', (lambda b, cb: lambda e: e.dma_start(out=awb[b][:], in_=ada_v[:, :, cb * 512:(cb + 1) * 512]))(b, cb),
                 writes=[r_awb[b]], chan='awb%d' % b)
        if cb > 0:
            c0 = cb - 1
            b = c0 % 2
            for k in range(8):
                S.op('pe', (lambda b, k: lambda e: e.matmul(ps_mod[b][0:2, :], lhsT=scv[:, k, :], rhs=awb[b][:, k, :],
                                                           start=(k == 0), stop=(k == 7)))(b, k),
                     reads=[r_scv, r_awb[b]], writes=[r_psmod[b]])
            S.op('dve', (lambda b, c0: lambda e: e.tensor_tensor(out=MOD[0:2, c0 * 512:(c0 + 1) * 512], in0=ps_mod[b][0:2, :],
                                                                in1=adab[0:2, c0 * 512:(c0 + 1) * 512], op=ALU.add))(b, c0),
                 reads=[r_psmod[b], r_adab], writes=[r_MOD])
    r_modd = R()
    S.op('sp', lambda e: e.dma_start(out=MODd, in_=MOD[:]), reads=[r_MOD], writes=[r_modd], chan='c_modd')

    r_MODT = R()
    ps_t = ps_mod[0]
    r_pst = r_psmod[0]
    offs = [0, 1024, 3072, 4096]
    for vi in range(4):
        for k in range(8):
            c0 = offs[vi] + k * 128
            j = (vi * 8 + k) * 2
            S.op('pe', (lambda c0, j: lambda e: e.matmul(ps_t[:, j:j + 2], lhsT=MOD[0:2, c0:c0 + 128], rhs=ident[0:2, 0:2],
                                                        start=True, stop=True))(c0, j),
                 reads=[r_MOD, r_ident], writes=[r_pst])
    S.op('dve', lambda e: e.tensor_copy(out=MODT[:].rearrange("p a k s -> p (a k s)"), in_=ps_t[:, 0:64]),
         reads=[r_pst], writes=[r_MODT])
    r_n1t = R()
    S.op('sp', lambda e: e.dma_start(out=n1t[:], in_=n1w), writes=[r_n1t], chan='c_n1t')
    r_scale1 = R()
    S.op('dve', lambda e: e.scalar_tensor_tensor(out=scale1[:], in0=MODT[:, 1, :, :], scalar=1.0,
                                                 in1=n1t[:].unsqueeze(2).to_broadcast([128, 8, 2]), op0=ALU.add, op1=ALU.mult),
         reads=[r_MODT, r_n1t], writes=[r_scale1])

    r_lbt, r_lbr = R(), R()
    S.op('sp', lambda e: e.dma_start(out=lbt[:], in_=lbl), writes=[r_lbt], chan='c_lbt')
    S.op('dve', lambda e: e.tensor_tensor(out=lbr[:].rearrange("p a c -> p (a c)"), in0=lbt[:, 0:1024], in1=lbt[:, 1024:2048],
                                          op=ALU.subtract), reads=[r_lbt], writes=[r_lbr])
    S.op('act', lambda e: e.activation(out=lbr[:], in_=lbr[:], func=AF.Sigmoid), reads=[r_lbr], writes=[r_lbr])
    S.op('dve', lambda e: e.tensor_scalar(out=omlb[:], in0=lbr[:], scalar1=-1.0, scalar2=1.0, op0=ALU.mult, op1=ALU.add),
         reads=[r_lbr], writes=[r_lbr])
    r_gbt = R()
    S.op('sp', lambda e: e.dma_start(out=gbt[:], in_=gateb), writes=[r_gbt], chan='c_gbt')
    S.op('sp', lambda e: e.dma_start(out=gbrow[:], in_=gatebrow), writes=[r_gbt], chan='c_gbrow')
    S.op('sp', lambda e: e.dma_start(out=Jm[:], in_=jm_in), writes=[r_gbt], chan='c_jm')
    S.barrier()
    A.reset(m1)

    if stage >= 2:
        m2 = A.mark()
        WIN = A.tile([128, 8, PROJ], BF16, 'WIN')
        r_WIN = R()
        w_in_v = w_in.rearrange("(k p) c -> p k c", p=128)
        CB = [(0, 1536), (1536, 3072), (3072, PROJ)]
        if FASTSIM:
            S.op('dve', lambda e: e.memset(WIN[:], 0.01), writes=[r_WIN])
        for k in range(0 if FASTSIM else 8):
            for (c0, c1) in CB:
                S.op('pool', (lambda k, c0, c1: lambda e: e.dma_start(out=WIN[:, k, c0:c1], in_=w_in_v[:, k, c0:c1]))(k, c0, c1),
                     writes=[r_WIN], chan='c_win')
        NXB = 3
        xt = [A.tile([128, D], F32, 'xt') for _ in range(NXB)]
        r_xt = [R() for _ in range(NXB)]
        xs = [A.tile([128, D], BF16, 'xs') for _ in range(2)]
        r_xs = [R(), R()]
        junk = A.tile([128, D], BF16, 'junk')
        r_junk = R()
        ss = [A.tile([128, 2], F32, 'ss') for _ in range(2)]
        r_ss = [R(), R()]
        uT = [A.tile([128, 8, 512], BF16, 'uT') for _ in range(2)]
        r_uT = [R(), R()]
        ps_tr = [nc.alloc_psum_tensor('ps_tr%d' % i, [128, 1024], BF16) for i in range(2)]
        r_pstr = [Res('pstr0', True), Res('pstr1', True)]
        NPS = 3
        ps_mm = [nc.alloc_psum_tensor('ps_mm%d' % i, [128, 512], F32) for i in range(NPS)]
        r_psmm = [Res('psmm%d' % i, True) for i in range(NPS)]
        NEV = 6
        ev_f = [A.tile([128, 512], F32, 'ev_f') for _ in range(NEV)]
        ev_b = [A.tile([128, 512], BF16, 'ev_b') for _ in range(NEV)]
        ev_s = [A.tile([128, 512], F32, 'ev_s') for _ in range(NEV)]
        r_evf = [R() for _ in range(NEV)]
        r_evb = [R() for _ in range(NEV)]
        r_evs = [R() for _ in range(NEV)]
        r_dram = R('p2dram')
        cnt = {'ps': 0, 'ev': 0, 'q': 0}

        def next_ps():
            i = cnt['ps'] % NPS
            cnt['ps'] += 1
            return i

        def next_ev():
            i = cnt['ev'] % NEV
            cnt['ev'] += 1
            return i

        def dq():
            return 'sp'

        sbs = [(i, min(i + 4, NT)) for i in range(0, NT, 4)]
        if FASTSIM:
            sbs = sbs[:1]
        def p2_norm(sbi):
            t0, t1 = sbs[sbi]
            nt_sb = t1 - t0
            ntok = nt_sb * 128
            ub = sbi % 2
            for ti in range(t0, t1):
                xb = ti % NXB
                sb2 = ti % 2
                s_col = 1 if ti < 2 else 0
                lt = ti - t0
                S.op('sp', (lambda xb, ti: lambda e: e.dma_start(out=xt[xb][:], in_=xc[ti * 128:(ti + 1) * 128, :]))(xb, ti),
                     writes=[r_xt[xb]], chan='xt%d' % xb)
                S.op('act', (lambda xb, sb2: lambda e: e.activation(out=junk[:], in_=xt[xb][:], func=AF.Square,
                                                                   accum_out=ss[sb2][:, 0:1]))(xb, sb2),
                     reads=[r_xt[xb]], writes=[r_junk, r_ss[sb2]])
                S.op('act', (lambda sb2: lambda e: e.activation(out=ss[sb2][:, 1:2], in_=ss[sb2][:, 0:1], func=AF.Sqrt,
                                                               scale=1.0 / D, bias=epst[:, 0:1]))(sb2),
                     reads=[r_ss[sb2], r_epst], writes=[r_ss[sb2]])
                S.op('dve', (lambda sb2: lambda e: e.reciprocal(out=ss[sb2][:, 1:2], in_=ss[sb2][:, 1:2]))(sb2),
                     reads=[r_ss[sb2]], writes=[r_ss[sb2]])
                S.op('act', (lambda xb, sb2: lambda e: e.activation(out=xs[sb2][:], in_=xt[xb][:], func=AF.Copy,
                                                                   scale=ss[sb2][:, 1:2]))(xb, sb2),
                     reads=[r_xt[xb], r_ss[sb2]], writes=[r_xs[sb2]])
                for k in range(8):
                    S.op('pe', (lambda sb2, k: lambda e: e.transpose(out=ps_tr[sb2][:, k * 128:(k + 1) * 128],
                                                                    in_=xs[sb2][:, k * 128:(k + 1) * 128], identity=identb[:]))(sb2, k),
                         reads=[r_xs[sb2], r_identb], writes=[r_pstr[sb2]])
                S.op('dve', (lambda sb2, ub, lt, s_col: lambda e: e.tensor_tensor(
                    out=uT[ub][:, :, lt * 128:(lt + 1) * 128], in0=ps_tr[sb2][:, :].rearrange("p (k t) -> p k t", k=8),
                    in1=scale1[:, :, s_col:s_col + 1].to_broadcast([128, 8, 128]), op=ALU.mult))(sb2, ub, lt, s_col),
                     reads=[r_pstr[sb2], r_scale1], writes=[r_uT[ub]])
                S.op('pool', (lambda ub, lt, s_col: lambda e: e.tensor_tensor(
                    out=uT[ub][:, :, lt * 128:(lt + 1) * 128], in0=uT[ub][:, :, lt * 128:(lt + 1) * 128],
                    in1=MODT[:, 0, :, s_col:s_col + 1].to_broadcast([128, 8, 128]), op=ALU.add))(ub, lt, s_col),
                     reads=[r_uT[ub], r_MODT], writes=[r_uT[ub]])

        def p2_mm(sbi):
            t0, t1 = sbs[sbi]
            nt_sb = t1 - t0
            ntok = nt_sb * 128
            ub = sbi % 2
            fm = []
            for h in range(4):
                fm.append((h * 128, QTH[h * 128:(h + 1) * 128, :], 'silu', 128))
            for g in range(8):
                fm.append((2560 + g * 128, PRE[g * 128:(g + 1) * 128, :], 'copy', 128))
            fm.append((4608, GATES[:, :], 'gate', 16))
            for (c0, dst, kind, M) in fm:
                pi = next_ps()
                for k in range(8):
                    S.op('pe', (lambda pi, k, c0, M, ub, ntok: lambda e: e.matmul(
                        ps_mm[pi][0:M, 0:ntok], lhsT=WIN[:, k, c0:c0 + M], rhs=uT[ub][:, k, 0:ntok],
                        start=(k == 0), stop=(k == 7)))(pi, k, c0, M, ub, ntok),
                         reads=[r_WIN, r_uT[ub]], writes=[r_psmm[pi]])
                ei = next_ev()
                if kind == 'gate':
                    S.op('act', (lambda pi, ei, ntok: lambda e: e.activation(out=ev_f[ei][0:16, 0:ntok], in_=ps_mm[pi][0:16, 0:ntok],
                                                                            func=AF.Identity, bias=gbt[:, 0:1], scale=1.0))(pi, ei, ntok),
                         reads=[r_psmm[pi], r_gbt], writes=[r_evf[ei]])
                    S.op(dq(), (lambda ei, ntok, t0, dst: lambda e: e.dma_start(out=dst[:, t0 * 128:t0 * 128 + ntok],
                                                                               in_=ev_f[ei][0:16, 0:ntok]))(ei, ntok, t0, dst),
                         reads=[r_evf[ei]], writes=[r_dram], chan='evf%d' % ei)
                else:
                    fn = AF.Silu if kind == 'silu' else AF.Copy
                    cnt['cp'] = cnt.get('cp', 0) + 1
                    if kind == 'copy' and cnt['cp'] % 2:
                        S.op('dve', (lambda pi, ei, ntok: lambda e: e.tensor_copy(out=ev_b[ei][:, 0:ntok], in_=ps_mm[pi][:, 0:ntok]))(pi, ei, ntok),
                             reads=[r_psmm[pi]], writes=[r_evb[ei]])
                    else:
                        S.op('act', (lambda pi, ei, ntok, fn: lambda e: e.activation(out=ev_b[ei][:, 0:ntok], in_=ps_mm[pi][:, 0:ntok],
                                                                                    func=fn))(pi, ei, ntok, fn),
                             reads=[r_psmm[pi]], writes=[r_evb[ei]])
                    S.op(dq(), (lambda ei, ntok, t0, dst: lambda e: e.dma_start(out=dst[:, t0 * 128:t0 * 128 + ntok],
                                                                               in_=ev_b[ei][:, 0:ntok]))(ei, ntok, t0, dst),
                         reads=[r_evb[ei]], writes=[r_dram], chan='evb%d' % ei)

            tmb = [(512, HGV, 'copy'), (3584, MLV, 'copy'), (1536, 0, 'ff'), (2048, 1, 'ff'),
                   (1024, HGG, 'silu'), (4096, MLO, 'sigm'), (4608, GATEST, 'gatet')]
            for lt in range(nt_sb):
                ti = t0 + lt
                for (c0, dst, kind) in tmb:
                    if kind in ('silu', 'sigm') and ti < 2:
                        continue
                    pi = next_ps()
                    ncol = 16 if kind == 'gatet' else 512
                    for k in range(8):
                        S.op('pe', (lambda pi, k, c0, ub, lt, ncol: lambda e: e.matmul(
                            ps_mm[pi][:, 0:ncol], lhsT=uT[ub][:, k, lt * 128:(lt + 1) * 128], rhs=WIN[:, k, c0:c0 + ncol],
                            start=(k == 0), stop=(k == 7)))(pi, k, c0, ub, lt, ncol),
                             reads=[r_WIN, r_uT[ub]], writes=[r_psmm[pi]])
                    ei = next_ev()
                    rows = slice(ti * 128, (ti + 1) * 128)
                    if kind == 'gatet':
                        S.op('dve', (lambda pi, ei: lambda e: e.tensor_tensor(out=ev_f[ei][:, 0:16], in0=ps_mm[pi][:, 0:16], in1=gbrow[:, :],
                                                                             op=ALU.add))(pi, ei),
                             reads=[r_psmm[pi], r_gbt], writes=[r_evf[ei]])
                        S.op(dq(), (lambda ei, dst, rows: lambda e: e.dma_start(out=dst[rows, :], in_=ev_f[ei][:, 0:16]))(ei, dst, rows),
                             reads=[r_evf[ei]], writes=[r_dram], chan='evf%d' % ei)
                        continue
                    if kind == 'ff':
                        d = dst
                        S.op('act', (lambda pi, ei: lambda e: e.activation(out=ev_s[ei][:], in_=ps_mm[pi][:, :], func=AF.Sigmoid))(pi, ei),
                             reads=[r_psmm[pi]], writes=[r_evs[ei]])
                        S.op('dve', (lambda ei, d: lambda e: e.tensor_tensor(out=ev_s[ei][:], in0=ev_s[ei][:], in1=omlb[:, d, :],
                                                                            op=ALU.mult))(ei, d),
                             reads=[r_evs[ei], r_lbr], writes=[r_evs[ei]])
                        S.op('pool', (lambda ei, d: lambda e: e.tensor_tensor(out=ev_s[ei][:], in0=ev_s[ei][:], in1=lbr[:, d, :],
                                                                             op=ALU.add))(ei, d),
                             reads=[r_evs[ei], r_lbr], writes=[r_evs[ei]])
                        S.op('act', (lambda ei: lambda e: e.activation(out=ev_f[ei][:], in_=ev_s[ei][:], func=AF.Ln))(ei),
                             reads=[r_evs[ei]], writes=[r_evf[ei]])
                        S.op('pool', (lambda ei: lambda e: e.tensor_scalar(out=ev_b[ei][:], in0=ev_s[ei][:], scalar1=-1.0, scalar2=1.0,
                                                                          op0=ALU.mult, op1=ALU.add))(ei),
                             reads=[r_evs[ei]], writes=[r_evb[ei]])
                        S.op(dq(), (lambda ei, d, rows: lambda e: e.dma_start(out=GG[d][rows, :], in_=ev_f[ei][:]))(ei, d, rows),
                             reads=[r_evf[ei]], writes=[r_dram], chan='evf%d' % ei)
                        S.op(dq(), (lambda ei, d, rows: lambda e: e.dma_start(out=KK[d][rows, :], in_=ev_b[ei][:]))(ei, d, rows),
                             reads=[r_evb[ei]], writes=[r_dram], chan='evb%d' % ei)
                    else:
                        fn = {'copy': AF.Copy, 'silu': AF.Silu, 'sigm': AF.Sigmoid}[kind]
                        if kind == 'copy':
                            S.op('dve', (lambda pi, ei: lambda e: e.tensor_copy(out=ev_b[ei][:], in_=ps_mm[pi][:, :]))(pi, ei),
                                 reads=[r_psmm[pi]], writes=[r_evb[ei]])
                        else:
                            S.op('act', (lambda pi, ei, fn: lambda e: e.activation(out=ev_b[ei][:], in_=ps_mm[pi][:, :], func=fn))(pi, ei, fn),
                                 reads=[r_psmm[pi]], writes=[r_evb[ei]])
                        S.op(dq(), (lambda ei, dst, rows: lambda e: e.dma_start(out=dst[rows, :], in_=ev_b[ei][:]))(ei, dst, rows),
                             reads=[r_evb[ei]], writes=[r_dram], chan='evb%d' % ei)
        p2_norm(0)
        for sbi in range(len(sbs)):
            if sbi + 1 < len(sbs):
                p2_norm(sbi + 1)
            p2_mm(sbi)
        S.barrier()
        A.reset(m2)

    ps_x = nc.alloc_psum_tensor('ps_x', [128, 512], F32)
    r_psx = Res('psx', True)
    r_psx2 = r_psx
    if stage >= 3:
        m3 = A.mark()
        cwt = A.tile([128, 8, 9], F32, 'cwt')
        cbt = A.tile([128, 8], F32, 'cbt')
        r_cw = R()
        S.op('sp', lambda e: e.dma_start(out=cwt[:], in_=convw), writes=[r_cw], chan='c_cw')
        S.op('sp', lambda e: e.dma_start(out=cbt[:], in_=convb), writes=[r_cw], chan='c_cb')
        pre = [A.tile([128, T], BF16, 'pre') for _ in range(2)]
        acc = [A.tile([128, T], F32, 'acc') for _ in range(2)]
        post = [A.tile([128, T], BF16, 'post') for _ in range(2)]
        r_pre, r_acc, r_post = [R(), R()], [R(), R()], [R(), R()]
        r_qkt = R('qkt')
        for g in range(1 if FASTSIM else 8):
            b = g % 2
            ce = 'dve'
            S.op('sp', (lambda b, g: lambda e: e.dma_start(out=pre[b][:], in_=PRE[g * 128:(g + 1) * 128, :]))(b, g),
                 reads=[r_dram], writes=[r_pre[b]], chan='pre%d' % b)
            S.op(ce, (lambda b, g: lambda e: e.tensor_scalar(out=acc[b][:], in0=pre[b][:], scalar1=cwt[:, g, 4:5], scalar2=None,
                                                             op0=ALU.mult))(b, g),
                 reads=[r_pre[b], r_cw], writes=[r_acc[b]])
            for dx in (0, 2):
                ox = dx - 1
                d0, d1 = max(0, -ox), CTX - max(0, ox)
                S.op(ce, (lambda b, g, dx, ox, d0, d1: lambda e: e.scalar_tensor_tensor(
                    out=acc[b][:, d0:d1], in0=pre[b][:, d0 + ox:d1 + ox], scalar=cwt[:, g, 3 + dx:4 + dx],
                    in1=acc[b][:, d0:d1], op0=ALU.mult, op1=ALU.add))(b, g, dx, ox, d0, d1),
                     reads=[r_pre[b], r_cw], writes=[r_acc[b]])
            for dy in range(3):
                for dx in range(3):
                    if dy == 1 and dx == 1:
                        continue
                    oy, ox = dy - 1, dx - 1
                    r0, r1 = max(0, -oy), 128 - max(0, oy)
                    c0, c1 = max(0, -ox), 64 - max(0, ox)

                    def mk(b, g, dy, dx, oy, ox, r0, r1, c0, c1):
                        def f(e):
                            av = acc[b][:, CTX:].rearrange("p (r c) -> p r c", c=64)
                            pv = pre[b][:, CTX:].rearrange("p (r c) -> p r c", c=64)
                            return e.scalar_tensor_tensor(out=av[:, r0:r1, c0:c1], in0=pv[:, r0 + oy:r1 + oy, c0 + ox:c1 + ox],
                                                          scalar=cwt[:, g, dy * 3 + dx:dy * 3 + dx + 1], in1=av[:, r0:r1, c0:c1],
                                                          op0=ALU.mult, op1=ALU.add)
                        return f
                    S.op(ce, mk(b, g, dy, dx, oy, ox, r0, r1, c0, c1), reads=[r_pre[b], r_cw], writes=[r_acc[b]])
            S.op('act', (lambda b, g: lambda e: e.activation(out=post[b][:], in_=acc[b][:], func=AF.Silu, bias=cbt[:, g:g + 1]))(b, g),
                 reads=[r_acc[b], r_cw], writes=[r_post[b]])
            S.op('sp', (lambda b, g: lambda e: e.dma_start(out=QKT[g * 128:(g + 1) * 128, :], in_=post[b][:]))(b, g),
                 reads=[r_post[b]], writes=[r_qkt], chan='post%d' % b)
        S.barrier()
        A.reset(m3)

    if stage >= 4:
        cmask = A.tile([128, 2, 128], F32, 'cmask')
        ctrix = A.tile([128, 2, 132], F32, 'ctrix')
        cselh = A.tile([4, 4, 128], F32, 'cselh')
        r_cm = R()
        S.op('sp', lambda e: e.dma_start(out=cmask[:], in_=maskin), writes=[r_cm], chan='c_m1')
        S.op('sp', lambda e: e.dma_start(out=ctrix[:], in_=trixin), writes=[r_cm], chan='c_m2')
        S.op('sp', lambda e: e.dma_start(out=cselh[:], in_=selhin), writes=[r_cm], chan='c_m3')
        TOK = [A.tile([128, NT, 8], F32, 'TOK') for _ in range(2)]
        WSB = [A.tile([128, 4, NT], F32, 'WSB') for _ in range(2)]
        r_TOK = [R(), R()]
        m4 = A.mark()
        X = [A.tile([4, T], F32, 'X%d' % i) for i in range(4)]
        AE = A.tile([4, NT], F32, 'AE')
        WS = A.tile([4, NT], F32, 'WS')
        cln = A.tile([4, 2], F32, 'cln')
        r_rows = R()
        r_zr = R()
        S.op('dve', lambda e: e.memset(cln[:, 0:1], 0.5 * float(np.log(128.0))), writes=[r_zr])
        S.op('dve', lambda e: e.memset(cln[:, 1:2], 0.0), writes=[r_zr])
        ps_g = ps_mm[0]
        r_psg = r_psmm[0]

        def rop(eng, fn):
            S.op(eng, fn, reads=[r_rows, r_zr], writes=[r_rows])

        GT = A.tile([128, NT, 16], F32, 'GT')
        TOKr = A.tile([128, NT, 8], F32, 'TOKr')
        r_GT = R()
        r_TOKr = R()
        S.op('sp', lambda e: e.dma_start(out=GT[:], in_=GATEST.rearrange("(t p) g -> p t g", p=128)), reads=[r_dram], writes=[r_GT], chan='c_gt')
        order1 = [1, 0] + list(range(NT - 1, 1, -1))
        ps_a, r_psa = ps_mm[1], r_psmm[1]
        ps_b, r_psb = ps_mm[2], r_psmm[2]

        for p in range(2):
            if p == 0:
                S.op('sp', lambda e: e.dma_start(out=X[0][:], in_=GATES[0:4, :]), reads=[r_dram], writes=[r_rows], chan='c_li')
                S.op('sp', lambda e: e.dma_start(out=X[1][:], in_=GATES[4:8, :]), reads=[r_dram], writes=[r_rows], chan='c_fz')
                LIb, LFb, Bb, Mb = X[0], X[1], X[2], X[3]
            else:
                LIb, LFb, Bb, Mb = X[2], X[3], X[0], X[1]
                for c0 in range(0, NT, 4):
                    c1 = min(c0 + 4, NT)
                    for c in range(c0, c1):
                        ti = order1[c]
                        j = (c - c0) * 128
                        S.op('pe', (lambda ti, j: lambda e: e.matmul(ps_a[0:4, j:j + 128], lhsT=GT[:, ti, 8:12], rhs=Jm[:, :], start=True, stop=True))(ti, j),
                             reads=[r_GT, r_gbt], writes=[r_psa])
                        S.op('pe', (lambda ti, j: lambda e: e.matmul(ps_b[0:4, j:j + 128], lhsT=GT[:, ti, 12:16], rhs=Jm[:, :], start=True, stop=True))(ti, j),
                             reads=[r_GT, r_gbt], writes=[r_psb])
                    n = (c1 - c0) * 128
                    S.op('dve', (lambda c0, n, LIb: lambda e: e.tensor_copy(out=LIb[:, c0 * 128:c0 * 128 + n], in_=ps_a[0:4, 0:n]))(c0, n, LIb),
                         reads=[r_psa, r_rows], writes=[r_rows])
                    S.op('act', (lambda c0, n, LFb: lambda e: e.activation(out=LFb[:, c0 * 128:c0 * 128 + n], in_=ps_b[0:4, 0:n], func=AF.Copy))(c0, n, LFb),
                         reads=[r_psb, r_rows], writes=[r_rows])
            rop('act', (lambda LFb: lambda e: e.activation(out=LFb[:], in_=LFb[:], func=AF.Sigmoid))(LFb))
            rop('act', (lambda LFb: lambda e: e.activation(out=LFb[:], in_=LFb[:], func=AF.Ln))(LFb))

            def body(LIb=LIb, LFb=LFb, Bb=Bb, Mb=Mb):
                rop('dve', lambda e: e.tensor_tensor_scan(out=Bb[:], data0=LFb[:], data1=LFb[:], initial=0.0, op0=ALU.add, op1=ALU.min))
                rop('dve', lambda e: e.tensor_tensor_scan(out=Mb[:], data0=LFb[:], data1=LIb[:], initial=0.0, op0=ALU.add, op1=ALU.max))
                rop('dve', lambda e: e.tensor_tensor(out=Bb[:], in0=Bb[:], in1=Mb[:], op=ALU.subtract))
                rop('dve', lambda e: e.memset(AE[:, 0:1], 0.0))
                rop('dve', lambda e: e.tensor_copy(out=AE[:, 1:NT], in_=Bb[:, 127:T - 1:128]))
                rop('dve', lambda e: e.tensor_tensor(out=Bb[:].rearrange("p (c t) -> p c t", t=128),
                                                     in0=Bb[:].rearrange("p (c t) -> p c t", t=128),
                                                     in1=AE[:].unsqueeze(2).to_broadcast([4, NT, 128]), op=ALU.subtract))
                rop('act', lambda e: e.activation(out=WS[:], in_=Bb[:, 127:T:128], func=AF.Exp))
                rop('dve', lambda e: e.tensor_tensor(out=Mb[:], in0=Mb[:], in1=Bb[:], op=ALU.add))
                rop('dve', lambda e: e.tensor_tensor(out=LIb[:], in0=LIb[:], in1=Mb[:], op=ALU.subtract))
                rop('act', lambda e: e.activation(out=LIb[:], in_=LIb[:], func=AF.Exp))
                rop('act', lambda e: e.activation(out=Mb[:], in_=Mb[:], func=AF.Exp, scale=-1.0, bias=cln[:, 0:1]))
            body()
            SKn, EMn = LIb, Mb
            dstT = TOK[0] if p == 0 else TOKr
            r_dstT = r_TOK[0] if p == 0 else r_TOKr
            for c0 in range(0, NT, 64):
                c1 = min(NT, c0 + 64)
                for c in range(c0, c1):
                    j = (c - c0) * 8
                    S.op('pe', (lambda c, j, SKn: lambda e: e.matmul(ps_g[:, j:j + 4], lhsT=SKn[0:4, c * 128:(c + 1) * 128],
                                                                    rhs=ident[0:4, 0:4], start=True, stop=True))(c, j, SKn),
                         reads=[r_rows, r_ident], writes=[r_psg])
                    S.op('pe', (lambda c, j, EMn: lambda e: e.matmul(ps_g[:, j + 4:j + 8], lhsT=EMn[0:4, c * 128:(c + 1) * 128],
                                                                    rhs=ident[0:4, 0:4], start=True, stop=True))(c, j, EMn),
                         reads=[r_rows, r_ident], writes=[r_psg])
                if p == 0:
                    S.op('dve', (lambda c0, c1: lambda e: e.tensor_copy(out=TOK[0][:, c0:c1, :].rearrange("p a b -> p (a b)"),
                                                                       in_=ps_g[:, 0:(c1 - c0) * 8]))(c0, c1),
                         reads=[r_psg], writes=[r_TOK[0]])
                else:
                    for c in range(c0, c1):
                        ti = order1[c]
                        j = (c - c0) * 8
                        S.op('dve' if c % 2 else 'act',
                             (lambda ti, j: (lambda e: e.tensor_copy(out=TOKr[:, ti, :], in_=ps_g[:, j:j + 8])))(ti, j) if c % 2 else
                             (lambda ti, j: (lambda e: e.activation(out=TOKr[:, ti, :], in_=ps_g[:, j:j + 8], func=AF.Copy)))(ti, j),
                             reads=[r_psg], writes=[r_TOKr])
            if p == 1:
                TRf = TOKr[:].rearrange("p a b -> p (a b)")
                TKf = TOK[1][:].rearrange("p a b -> p (a b)")
                half = NT * 4
                for hh in range(2):
                    S.op('pe', (lambda hh: lambda e: e.matmul(ps_g[:, 0:half], lhsT=Jm[:, :], rhs=TRf[:, hh * half:(hh + 1) * half], start=True, stop=True))(hh),
                         reads=[r_TOKr, r_gbt], writes=[r_psg])
                    S.op('dve', (lambda hh: lambda e: e.tensor_copy(out=TKf[:, hh * half:(hh + 1) * half], in_=ps_g[:, 0:half]))(hh),
                         reads=[r_psg], writes=[r_TOK[1]])
            for h in range(4):
                S.op('pe', (lambda h: lambda e: e.matmul(ps_g[:, h * NT:(h + 1) * NT], lhsT=cselh[0:4, h, :], rhs=WS[0:4, :],
                                                        start=True, stop=True))(h),
                     reads=[r_rows, r_cm], writes=[r_psg])
            S.op('dve', (lambda p: lambda e: e.tensor_copy(out=WSB[p][:].rearrange("p a b -> p (a b)"), in_=ps_g[:, 0:4 * NT]))(p),
                 reads=[r_psg], writes=[r_TOK[p]])
        if "TOKd" in dbg:
            S.op('sp', lambda e: e.dma_start(out=TOKd[:, 0:NT * 8], in_=TOK[1][:].rearrange("p a b -> p (a b)")), reads=[r_TOK[1]], chan='c_tokd')
            S.op('sp', lambda e: e.dma_start(out=TOKd[:, NT * 8:NT * 12], in_=WSB[1][:].rearrange("p a b -> p (a b)")), reads=[r_TOK[1]], chan='c_tokd')
        S.barrier()
        A.reset(m4)

    if stage >= 5:
        m5 = A.mark()
        hgb = [ps_mm[0], ps_mm[1]]
        hgo = [ps_mod[0], ps_mod[1]]
        mlb = [ps_mm[2], ps_x]
        r_pA = [r_psmm[0], r_psmm[1]]; r_pB = r_pA; r_pS = r_pA
        r_pO = [r_psmod[0], r_psmod[1]]; r_pU = r_pO
        r_pT = [r_pstr[0], r_pstr[1]]; r_mT = r_pT
        r_mS = [r_psmm[2], r_psx]; r_mO = r_mS; r_mU = r_mS
        def two(shape, dt, nm):
            return [A.tile(shape, dt, nm) for _ in range(2)], [R(), R()]
        EQ, r_EQ = two([128, 128], F32, 'EQ')
        QtT, r_QtT = two([128, 128], BF16, 'QtT')
        EK, r_EK = two([128, 128], F32, 'EK')
        Kt, r_Kt = two([128, 128], BF16, 'Kt')
        KtT, r_KtT = two([128, 128], BF16, 'KtT')
        ET, r_ET = two([128, 4], F32, 'ET')
        Spb, r_Spb = two([128, 128], BF16, 'Spb')
        ATh, r_ATh = two([128, 128], BF16, 'ATh')
        ATm, r_ATm = two([128, 128], BF16, 'ATm')
        Kh, r_Kh = two([128, 128], BF16, 'Kh')
        Cb, r_Cb = two([128, 132], BF16, 'Cb')
        dn, r_dn = two([128, 2], F32, 'dn')
        obuf, r_obuf = two([128, 512], F32, 'obuf')
        hbuf, r_hbuf = two([128, 512], F32, 'hbuf')
        o0, r_o0 = two([128, 512], F32, 'o0')
        h0, r_h0 = two([128, 512], F32, 'h0')
        gt, r_gt = two([128, 512], F32, 'gt')
        kt, r_kt = two([128, 512], BF16, 'kt')
        hv, r_hv = two([128, 512], BF16, 'hv')
        vb, r_vb = two([128, 4, 132], BF16, 'vb')
        qth, r_qth = two([128, 4, 128], BF16, 'qth')
        qtm, r_qtm = two([128, 4, 128], BF16, 'qtm')
        ktm, r_ktm = two([128, 4, 128], BF16, 'ktm')
        Sst = [A.tile([128, 128], F32, 'Sst') for _ in range(4)]
        r_S = [R() for _ in range(4)]
        Cst = [A.tile([128, 132], F32, 'Cst') for _ in range(4)]
        r_C = [R() for _ in range(4)]
        r_oh = [R(), R()]
        for b in range(2):
            S.op('pool', (lambda b: lambda e: e.memset(vb[b][:], 1.0))(b), writes=[r_vb[b]])
        QTHv = QTH.rearrange("(h d) t -> d h t", d=128)
        QTMv = QKT[0:512, :].rearrange("(h d) t -> d h t", d=128)
        KTMv = QKT[512:1024, :].rearrange("(h d) t -> d h t", d=128)
        MLVv = MLV.rearrange("t (h v) -> t h v", v=128)
        for p in range(2):
            for qq in range(2):
                S.op('pool', (lambda qq: lambda e: e.memset(ATh[qq][:], 0.0))(qq), writes=[r_ATh[qq]])
            for h in range(4):
                S.op('pool', (lambda h: lambda e: e.memset(Sst[h][:], 0.0))(h), writes=[r_S[h]])
                S.op('pool', (lambda h: lambda e: e.memset(Cst[h][:], 0.0))(h), writes=[r_C[h]])
            order = list(range(NT)) if p == 0 else [1, 0] + list(range(NT - 1, 1, -1))
            if FASTSIM:
                order = order[:4]
            MK = cmask[:, p, :]
            TX = ctrix[:, p, 0:132]
            TM = ctrix[:, p, 0:128]

            def loads(c, ti, gg_=GG[p], kk_=KK[p]):
                b = c % 2
                rows = slice(ti * 128, (ti + 1) * 128)
                cols = slice(ti * 128, (ti + 1) * 128)
                S.op('sp', lambda e: e.dma_start(out=gt[b][:], in_=gg_[rows, :]), reads=[r_dram], writes=[r_gt[b]], chan='l_gt%d' % b)
                S.op('sp', lambda e: e.dma_start(out=kt[b][:], in_=kk_[rows, :]), reads=[r_dram], writes=[r_kt[b]], chan='l_kt%d' % b)
                S.op('sp', lambda e: e.dma_start(out=hv[b][:], in_=HGV[rows, :]), reads=[r_dram], writes=[r_hv[b]], chan='l_hv%d' % b)
                S.op('sp', lambda e: e.dma_start(out=vb[b][:, :, 0:128], in_=MLVv[rows, :, :]), reads=[r_dram], writes=[r_vb[b]], chan='l_vb%d' % b)
                S.op('sp', lambda e: e.dma_start(out=ktm[b][:], in_=KTMv[:, :, cols]), reads=[r_qkt], writes=[r_ktm[b]], chan='l_ktm%d' % b)
                if ti >= 2:
                    S.op('sp', lambda e: e.dma_start(out=qth[b][:], in_=QTHv[:, :, cols]), reads=[r_dram], writes=[r_qth[b]], chan='l_qth%d' % b)
                    S.op('sp', lambda e: e.dma_start(out=qtm[b][:], in_=QTMv[:, :, cols]), reads=[r_qkt], writes=[r_qtm[b]], chan='l_qtm%d' % b)
                    if p == 1:
                        xr = slice((ti - 2) * 128, (ti - 1) * 128)
                        S.op('sp', lambda e: e.dma_start(out=o0[b][:], in_=OH[0][xr, :]), reads=[r_oh[0]], writes=[r_o0[b]], chan='l_o0%d' % b)
                        S.op('sp', lambda e: e.dma_start(out=h0[b][:], in_=OM[0][xr, :]), reads=[r_oh[0]], writes=[r_h0[b]], chan='l_h0%d' % b)

            def hg_step(c, ti, h, MK=MK, TX=TX, TM=TM, p=p):
                b = c % 2
                q = (c * 4 + h) % 2
                isx = ti >= 2
                hs = slice(h * 128, (h + 1) * 128)
                pA = hgb[q][:, 0:132]; pB = hgb[q][:, 132:260]; pS = hgb[q][:, 260:388]
                pO = hgo[q][:, 0:128]; pU = hgo[q][:, 128:256]; pT = ps_tr[q][:, 0:128]
                S.op('pe', lambda e: e.matmul(pA, lhsT=gt[b][:, hs], rhs=TX, start=True, stop=True), reads=[r_gt[b], r_cm], writes=[r_pA[q]])
                yield
                S.op('pe', lambda e: e.matmul(pB, lhsT=TM, rhs=gt[b][:, hs], start=True, stop=True), reads=[r_gt[b], r_cm], writes=[r_pB[q]])
                yield
                S.op('act', lambda e: e.activation(out=ET[q][:, 0:3], in_=hgb[q][:, 128:131], func=AF.Exp), reads=[r_pA[q]], writes=[r_ET[q]])
                yield
                if isx:
                    S.op('act', lambda e: e.activation(out=EQ[q][:], in_=hgb[q][:, 0:128], func=AF.Exp), reads=[r_pA[q]], writes=[r_EQ[q]])
                    yield
                    S.op('dve', lambda e: e.tensor_tensor(out=QtT[q][:], in0=EQ[q][:], in1=qth[b][:, h, :], op=ALU.mult),
                         reads=[r_EQ[q], r_qth[b]], writes=[r_QtT[q]])
                    yield
                S.op('act', lambda e: e.activation(out=EK[q][:], in_=pB, func=AF.Exp, scale=-1.0), reads=[r_pB[q]], writes=[r_EK[q]])
                yield
                S.op('dve', lambda e: e.tensor_tensor(out=Kt[q][:], in0=EK[q][:], in1=kt[b][:, hs], op=ALU.mult),
                     reads=[r_EK[q], r_kt[b]], writes=[r_Kt[q]])
                yield
                if isx:
                    S.op('pe', lambda e: e.transpose(out=pT, in_=Kt[q][:], identity=identb[:]), reads=[r_Kt[q], r_identb], writes=[r_pT[q]])
                    yield
                    S.op('act', lambda e: e.activation(out=KtT[q][:], in_=pT, func=AF.Copy), reads=[r_pT[q]], writes=[r_KtT[q]])
                    yield
                    S.op('pool', lambda e: e.tensor_scalar(out=Spb[q][:], in0=Sst[h][:], scalar1=ET[q][:, 1:2], scalar2=None, op0=ALU.mult),
                         reads=[r_S[h], r_ET[q]], writes=[r_Spb[q]])
                    yield
                    fr = slice(0, 64) if p == 0 else slice(64, 128)
                    hr = slice(64, 128) if p == 0 else slice(0, 64)
                    S.op('pe', lambda e: e.matmul(hgb[q][fr, 260:388], lhsT=KtT[q][:, fr], rhs=QtT[q][:], start=True, stop=True),
                         reads=[r_KtT[q], r_QtT[q]], writes=[r_pS[q]])
                    yield
                    S.op('pe', lambda e: e.matmul(hgb[q][hr, 260 + hr.start:260 + hr.stop], lhsT=KtT[q][:, hr], rhs=QtT[q][:, hr], start=True, stop=True),
                         reads=[r_KtT[q], r_QtT[q]], writes=[r_pS[q]])
                    yield
                    S.op('dve', lambda e: e.tensor_tensor(out=ATh[q][fr, :], in0=hgb[q][fr, 260:388], in1=cmask[fr, p, :], op=ALU.mult),
                         reads=[r_pS[q], r_cm], writes=[r_ATh[q]])
                    yield
                    S.op('dve', lambda e: e.tensor_tensor(out=ATh[q][hr, hr], in0=hgb[q][hr, 260 + hr.start:260 + hr.stop], in1=cmask[hr, p, hr], op=ALU.mult),
                         reads=[r_pS[q], r_cm], writes=[r_ATh[q]])
                    yield
                    S.op('pe', lambda e: e.matmul(pO, lhsT=ATh[q][:], rhs=hv[b][:, hs], start=True, stop=False),
                         reads=[r_ATh[q], r_hv[b]], writes=[r_pO[q]])
                    yield
                    S.op('pe', lambda e: e.matmul(pO, lhsT=QtT[q][:], rhs=Spb[q][:], start=False, stop=True),
                         reads=[r_QtT[q], r_Spb[q]], writes=[r_pO[q]])
                    yield
                S.op('pe', lambda e: e.matmul(pU, lhsT=Kt[q][:], rhs=hv[b][:, hs], start=True, stop=True),
                     reads=[r_Kt[q], r_hv[b]], writes=[r_pU[q]])
                yield
                S.op('pool', lambda e: e.tensor_scalar(out=Sst[h][:], in0=Sst[h][:], scalar1=ET[q][:, 2:3], scalar2=None, op0=ALU.mult),
                     reads=[r_S[h], r_ET[q]], writes=[r_S[h]])
                yield
                S.op('dve', lambda e: e.scalar_tensor_tensor(out=Sst[h][:], in0=pU, scalar=ET[q][:, 0:1], in1=Sst[h][:],
                                                             op0=ALU.mult, op1=ALU.add),
                     reads=[r_pU[q], r_ET[q], r_S[h]], writes=[r_S[h]])
                yield
                if isx:
                    if p == 0:
                        S.op('act', lambda e: e.activation(out=obuf[b][:, hs], in_=pO, func=AF.Copy), reads=[r_pO[q]], writes=[r_obuf[b]])
                        yield
                    else:
                        S.op('dve', lambda e: e.tensor_tensor(out=obuf[b][:, hs], in0=pO, in1=o0[b][:, hs], op=ALU.add),
                             reads=[r_pO[q], r_o0[b]], writes=[r_obuf[b]])
                        yield

            def ml_step(c, ti, h, MK=MK, p=p):
                b = c % 2
                q = (c * 4 + h) % 2
                isx = ti >= 2
                hs = slice(h * 128, (h + 1) * 128)
                pS = mlb[q][:, 0:128]; pO = mlb[q][:, 128:258]; pU = mlb[q][:, 260:390]; pT = ps_tr[q][:, 128:256]
                sk = TOK[p][:, ti, h:h + 1]
                em = TOK[p][:, ti, 4 + h:5 + h]
                cidx = c
                ws = WSB[p][:, h, cidx:cidx + 1]
                if isx:
                    S.op('pe', lambda e: e.matmul(pS, lhsT=ktm[b][:, h, :], rhs=qtm[b][:, h, :], start=True, stop=True),
                         reads=[r_ktm[b], r_qtm[b]], writes=[r_mS[q]])
                    yield
                    S.op('dve', lambda e: e.scalar_tensor_tensor(out=ATm[q][:], in0=pS, scalar=sk, in1=MK, op0=ALU.mult, op1=ALU.mult),
                         reads=[r_mS[q], r_TOK[p], r_cm], writes=[r_ATm[q]])
                    yield
                S.op('pe', lambda e: e.transpose(out=pT, in_=ktm[b][:, h, :], identity=identb[:]), reads=[r_ktm[b], r_identb], writes=[r_mT[q]])
                yield
                S.op('act', lambda e: e.activation(out=Kh[q][:], in_=pT, func=AF.Copy, scale=sk), reads=[r_mT[q], r_TOK[p]], writes=[r_Kh[q]])
                yield
                if isx:
                    S.op('pool', lambda e: e.tensor_copy(out=Cb[q][:, 0:130], in_=Cst[h][:, 0:130]), reads=[r_C[h]], writes=[r_Cb[q]])
                    yield
                    S.op('pe', lambda e: e.matmul(pO, lhsT=ATm[q][:], rhs=vb[b][:, h, 0:130], start=True, stop=False),
                         reads=[r_ATm[q], r_vb[b]], writes=[r_mO[q]])
                    yield
                    S.op('pe', lambda e: e.matmul(pO, lhsT=qtm[b][:, h, :], rhs=Cb[q][:, 0:130], start=False, stop=True),
                         reads=[r_qtm[b], r_Cb[q]], writes=[r_mO[q]])
                    yield
                S.op('pe', lambda e: e.matmul(pU, lhsT=Kh[q][:], rhs=vb[b][:, h, 0:130], start=True, stop=True),
                     reads=[r_Kh[q], r_vb[b]], writes=[r_mU[q]])
                yield
                S.op('pool', lambda e: e.tensor_scalar(out=Cst[h][:, 0:130], in0=Cst[h][:, 0:130], scalar1=ws, scalar2=None, op0=ALU.mult),
                     reads=[r_C[h], r_TOK[p]], writes=[r_C[h]])
                yield
                S.op('dve', lambda e: e.scalar_tensor_tensor(out=Cst[h][:, 0:130], in0=pU, scalar=ws, in1=Cst[h][:, 0:130],
                                                             op0=ALU.mult, op1=ALU.add),
                     reads=[r_mU[q], r_TOK[p], r_C[h]], writes=[r_C[h]])
                yield
                if isx:
                    S.op('act', lambda e: e.activation(out=dn[q][:, 0:1], in_=mlb[q][:, 256:257], func=AF.Abs),
                         reads=[r_mO[q]], writes=[r_dn[q]])
                    yield
                    S.op('dve', lambda e: e.tensor_tensor(out=dn[q][:, 0:1], in0=dn[q][:, 0:1], in1=em, op=ALU.max),
                         reads=[r_dn[q], r_TOK[p]], writes=[r_dn[q]])
                    yield
                    S.op('dve', lambda e: e.reciprocal(out=dn[q][:, 1:2], in_=dn[q][:, 0:1]), reads=[r_dn[q]], writes=[r_dn[q]])
                    yield
                    if p == 0:
                        S.op('act', lambda e: e.activation(out=hbuf[b][:, hs], in_=mlb[q][:, 128:256], func=AF.Copy, scale=dn[q][:, 1:2]),
                             reads=[r_mO[q], r_dn[q]], writes=[r_hbuf[b]])
                        yield
                    else:
                        S.op('dve', lambda e: e.scalar_tensor_tensor(out=hbuf[b][:, hs], in0=mlb[q][:, 128:256], scalar=dn[q][:, 1:2],
                                                                     in1=h0[b][:, hs], op0=ALU.mult, op1=ALU.add),
                             reads=[r_mO[q], r_dn[q], r_h0[b]], writes=[r_hbuf[b]])
                        yield

            for c in range(len(order) + 1):
                if c < len(order):
                    loads(c, order[c])
                if c > 0:
                    cc = c - 1
                    ti = order[cc]
                    for hbase in (0, 2):
                        gens = []
                        for h in (hbase, hbase + 1):
                            gens.append(hg_step(cc, ti, h))
                            gens.append(ml_step(cc, ti, h))
                        while gens:
                            for g in list(gens):
                                try:
                                    next(g)
                                except StopIteration:
                                    gens.remove(g)
                    if ti >= 2:
                        b = cc % 2
                        xr = slice((ti - 2) * 128, (ti - 1) * 128)
                        S.op('sp', (lambda b, xr, dst: lambda e: e.dma_start(out=dst[xr, :], in_=obuf[b][:]))(b, xr, OH[p]),
                             reads=[r_obuf[b]], writes=[r_oh[p]], chan='s_ob%d' % b)
                        S.op('sp', (lambda b, xr, dst: lambda e: e.dma_start(out=dst[xr, :], in_=hbuf[b][:]))(b, xr, OM[p]),
                             reads=[r_hbuf[b]], writes=[r_oh[p]], chan='s_hb%d' % b)
        S.barrier()
        A.reset(m5)

    if stage >= 6:
        m6 = A.mark()
        hgnw = A.tile([128, 512], F32, 'hgnw')
        mlnw = A.tile([128, 512], F32, 'mlnw')
        n2r = A.tile([128, D], F32, 'n2r')
        RW = A.tile([128, 8, 16], F32, 'RW')
        WOG = A.tile([128, 8, D], BF16, 'WOG')
        SC2R = A.tile([128, D], F32, 'SC2R')
        SH2R = A.tile([128, D], F32, 'SH2R')
        AFFT = A.tile([16, SEQ], F32, 'AFFT')
        r_c6 = R()
        r_AFFT = R()
        S.op('sp', lambda e: e.dma_start(out=hgnw[:], in_=hgnw_in), writes=[r_c6], chan='c6a')
        S.op('sp', lambda e: e.dma_start(out=mlnw[:], in_=mlnw_in), writes=[r_c6], chan='c6b')
        S.op('sp', lambda e: e.dma_start(out=n2r[:], in_=n2r_in), writes=[r_c6], chan='c6c')
        S.op('sp', lambda e: e.dma_start(out=RW[:], in_=rw_in.rearrange("(k p) e -> p k e", p=128)), writes=[r_c6], chan='c6d')
        m6b = A.mark()
        MOD2 = A.tile([2, 6 * D], F32, 'MOD2')
        wof = A.tile([128, 8, D], F32, 'wof')
        r_m2 = R()
        S.op('sp', lambda e: e.dma_start(out=MOD2[:], in_=MODd), reads=[r_modd], writes=[r_m2], chan='c6e')
        S.op('sp', lambda e: e.dma_start(out=wof[:], in_=wout_in.rearrange("(k p) c -> p k c", p=128)), writes=[r_m2], chan='c6f')
        G1R = A.tile([128, D], F32, 'G1R')
        pbs = [ps_mm[0], ps_mm[1]]
        rpb = [r_psmm[0], r_psmm[1]]
        for vi, (off, dst) in enumerate([(2048, G1R), (3072, SH2R), (4096, SC2R)]):
            for hb in range(2):
                q = (vi * 2 + hb) % 2
                S.op('pe', (lambda q, off, hb: lambda e: e.matmul(pbs[q][:, :], lhsT=sel2[0:2, :], rhs=MOD2[0:2, off + hb * 512:off + (hb + 1) * 512],
                                                                  start=True, stop=True))(q, off, hb),
                     reads=[r_sel2, r_m2], writes=[rpb[q]])
                if off == 4096:
                    S.op('dve', (lambda q, hb: lambda e: e.scalar_tensor_tensor(out=SC2R[:, hb * 512:(hb + 1) * 512], in0=pbs[q][:, :], scalar=1.0,
                                                                               in1=n2r[:, hb * 512:(hb + 1) * 512], op0=ALU.add, op1=ALU.mult))(q, hb),
                         reads=[rpb[q], r_c6], writes=[r_c6])
                else:
                    S.op('dve', (lambda q, hb, dst: lambda e: e.tensor_copy(out=dst[:, hb * 512:(hb + 1) * 512], in_=pbs[q][:, :]))(q, hb, dst),
                         reads=[rpb[q]], writes=[r_c6])
        for k in range(8):
            S.op('dve', (lambda k: lambda e: e.tensor_tensor(out=WOG[:, k, :], in0=wof[:, k, :], in1=G1R[:], op=ALU.mult))(k),
                 reads=[r_m2, r_c6], writes=[r_c6])
        S.barrier()
        A.reset(m6b)

        def two6(shape, dt, nm):
            return [A.tile(shape, dt, nm) for _ in range(2)], [R(), R()]
        oh_t, r_oh_t = two6([128, 512], F32, 'oh_t')
        om_t, r_om_t = two6([128, 512], F32, 'om_t')
        gg_t, r_gg_t = two6([128, 512], BF16, 'gg_t')
        mo_t, r_mo_t = two6([128, 512], BF16, 'mo_t')
        x_t, r_x_t = [A.tile([128, D], F32, 'x_t') for _ in range(3)], [R(), R(), R()]
        sq_t, r_sq_t = two6([128, 512], F32, 'sq_t')
        sq2_t, r_sq2_t = two6([128, 512], F32, 'sq2_t')
        st_t, r_st_t = two6([128, 16], F32, 'st_t')
        st2_t, r_st2_t = two6([128, 16], F32, 'st2_t')
        mix, r_mix = two6([128, D], BF16, 'mix')
        mixT, r_mixT = two6([128, D], BF16, 'mixT')
        xm, r_xm = two6([128, D], F32, 'xm')
        vx, r_vx = two6([128, D], F32, 'vx')
        vxb, r_vxb = two6([128, D], BF16, 'vxb')
        vxT, r_vxT = two6([128, D], F32, 'vxT')
        sm_t, r_sm_t = two6([128, 8], F32, 'sm_t')
        aff_t, r_aff_t = two6([128, 16], F32, 'aff_t')
        ex_t, r_ex_t = two6([128, 16], F32, 'ex_t')
        junk6 = A.tile([128, D], BF16, 'junk6')
        r_junk6 = R()
        r_xmid = R()
        r_vxd = R()
        r_affd = R()

        def p6_loads(i):
            b = i % 2
            xr = slice(i * 128, (i + 1) * 128)
            tr = slice((i + 2) * 128, (i + 3) * 128)
            S.op('sp', lambda e: e.dma_start(out=oh_t[b][:], in_=OH[1][xr, :]), reads=[r_oh[1]], writes=[r_oh_t[b]], chan='6oh%d' % b)
            S.op('sp', lambda e: e.dma_start(out=om_t[b][:], in_=OM[1][xr, :]), reads=[r_oh[1]], writes=[r_om_t[b]], chan='6om%d' % b)
            S.op('sp', lambda e: e.dma_start(out=gg_t[b][:], in_=HGG[tr, :]), reads=[r_dram], writes=[r_gg_t[b]], chan='6gg%d' % b)
            S.op('sp', lambda e: e.dma_start(out=mo_t[b][:], in_=MLO[tr, :]), reads=[r_dram], writes=[r_mo_t[b]], chan='6mo%d' % b)
            S.op('sp', lambda e: e.dma_start(out=x_t[i % 3][:], in_=xc[tr, :]), writes=[r_x_t[i % 3]], chan='6x%d' % (i % 3))

        def v4(t):
            return t.rearrange("p (h v) -> p h v", v=128)

        def p6_A(i):
            b = i % 2
            xr = slice(i * 128, (i + 1) * 128)
            S.op('dve', lambda e: e.tensor_tensor(out=sq_t[b][:], in0=oh_t[b][:], in1=oh_t[b][:], op=ALU.mult), reads=[r_oh_t[b]], writes=[r_sq_t[b]])
            S.op('dve', lambda e: e.tensor_reduce(out=st_t[b][:, 0:4], in_=v4(sq_t[b][:]), axis=AX.X, op=ALU.add), reads=[r_sq_t[b]], writes=[r_st_t[b]])
            S.op('act', lambda e: e.activation(out=st_t[b][:, 4:8], in_=st_t[b][:, 0:4], func=AF.Sqrt, scale=1.0 / 128, bias=epst[:, 0:1]),
                 reads=[r_st_t[b], r_epst], writes=[r_st_t[b]])
            S.op('dve', lambda e: e.reciprocal(out=st_t[b][:, 8:12], in_=st_t[b][:, 4:8]), reads=[r_st_t[b]], writes=[r_st_t[b]])
            S.op('dve', lambda e: e.tensor_tensor(out=v4(sq_t[b][:]), in0=v4(oh_t[b][:]), in1=st_t[b][:, 8:12].unsqueeze(2).to_broadcast([128, 4, 128]),
                                                  op=ALU.mult), reads=[r_oh_t[b], r_st_t[b]], writes=[r_sq_t[b]])
            S.op('dve', lambda e: e.tensor_tensor(out=sq_t[b][:], in0=sq_t[b][:], in1=hgnw[:], op=ALU.mult), reads=[r_sq_t[b], r_c6], writes=[r_sq_t[b]])
            S.op('dve', lambda e: e.tensor_tensor(out=mix[b][:, 0:512], in0=sq_t[b][:], in1=gg_t[b][:], op=ALU.mult),
                 reads=[r_sq_t[b], r_gg_t[b]], writes=[r_mix[b]])
            S.op('dve', lambda e: e.tensor_reduce(out=st2_t[b][:, 0:4], in_=v4(om_t[b][:]), axis=AX.X, op=ALU.add), reads=[r_om_t[b]], writes=[r_st2_t[b]])
            S.op('pool', lambda e: e.tensor_scalar(out=st2_t[b][:, 0:4], in0=st2_t[b][:, 0:4], scalar1=1.0 / 128, scalar2=None, op0=ALU.mult),
                 reads=[r_st2_t[b]], writes=[r_st2_t[b]])
            S.op('pool', lambda e: e.tensor_tensor(out=v4(om_t[b][:]), in0=v4(om_t[b][:]), in1=st2_t[b][:, 0:4].unsqueeze(2).to_broadcast([128, 4, 128]),
                                                   op=ALU.subtract), reads=[r_om_t[b], r_st2_t[b]], writes=[r_om_t[b]])
            S.op('pool', lambda e: e.tensor_tensor(out=sq2_t[b][:], in0=om_t[b][:], in1=om_t[b][:], op=ALU.mult), reads=[r_om_t[b]], writes=[r_sq2_t[b]])
            S.op('dve', lambda e: e.tensor_reduce(out=st2_t[b][:, 4:8], in_=v4(sq2_t[b][:]), axis=AX.X, op=ALU.add), reads=[r_sq2_t[b]], writes=[r_st2_t[b]])
            S.op('act', lambda e: e.activation(out=st2_t[b][:, 8:12], in_=st2_t[b][:, 4:8], func=AF.Sqrt, scale=1.0 / 128, bias=epst[:, 0:1]),
                 reads=[r_st2_t[b], r_epst], writes=[r_st2_t[b]])
            S.op('dve', lambda e: e.reciprocal(out=st2_t[b][:, 12:16], in_=st2_t[b][:, 8:12]), reads=[r_st2_t[b]], writes=[r_st2_t[b]])
            S.op('pool', lambda e: e.tensor_tensor(out=v4(sq2_t[b][:]), in0=v4(om_t[b][:]), in1=st2_t[b][:, 12:16].unsqueeze(2).to_broadcast([128, 4, 128]),
                                                   op=ALU.mult), reads=[r_om_t[b], r_st2_t[b]], writes=[r_sq2_t[b]])
            S.op('pool', lambda e: e.tensor_tensor(out=sq2_t[b][:], in0=sq2_t[b][:], in1=mlnw[:], op=ALU.mult), reads=[r_sq2_t[b], r_c6], writes=[r_sq2_t[b]])
            S.op('pool', lambda e: e.tensor_tensor(out=mix[b][:, 512:1024], in0=sq2_t[b][:], in1=mo_t[b][:], op=ALU.mult),
                 reads=[r_sq2_t[b], r_mo_t[b]], writes=[r_mix[b]])

        def p6_B(i):
            b = i % 2
            xr = slice(i * 128, (i + 1) * 128)
            for k in range(8):
                S.op('pe', (lambda k: lambda e: e.transpose(out=ps_tr[b][:, k * 128:(k + 1) * 128], in_=mix[b][:, k * 128:(k + 1) * 128], identity=identb[:]))(k),
                     reads=[r_mix[b], r_identb], writes=[r_pstr[b]])
            S.op('act', lambda e: e.activation(out=mixT[b][:], in_=ps_tr[b][:, :], func=AF.Copy), reads=[r_pstr[b]], writes=[r_mixT[b]])
            for cb in range(2):
                pi = 2 if cb == 0 else 0
                pt = ps_mm[2] if cb == 0 else ps_mod[b]
                rp = r_psmm[2] if cb == 0 else r_psmod[b]
                for k in range(8):
                    S.op('pe', (lambda k, cb, pt: lambda e: e.matmul(pt[:, :], lhsT=mixT[b][:, k * 128:(k + 1) * 128], rhs=WOG[:, k, cb * 512:(cb + 1) * 512],
                                                                     start=(k == 0), stop=(k == 7)))(k, cb, pt),
                         reads=[r_mixT[b], r_c6], writes=[rp])
                S.op('dve', (lambda cb, pt: lambda e: e.tensor_tensor(out=xm[b][:, cb * 512:(cb + 1) * 512], in0=pt[:, :], in1=x_t[i % 3][:, cb * 512:(cb + 1) * 512],
                                                                      op=ALU.add))(cb, pt),
                     reads=[rp, r_x_t[i % 3]], writes=[r_xm[b]])
            S.op('sp', lambda e: e.dma_start(out=XMID[xr, :], in_=xm[b][:]), reads=[r_xm[b]], writes=[r_xmid], chan='6xm%d' % b)

        def p6_C(i):
            b = i % 2
            xr = slice(i * 128, (i + 1) * 128)
            S.op('act', lambda e: e.activation(out=junk6[:], in_=xm[b][:], func=AF.Square, accum_out=sm_t[b][:, 0:1]),
                 reads=[r_xm[b]], writes=[r_junk6, r_sm_t[b]])
            S.op('act', lambda e: e.activation(out=sm_t[b][:, 1:2], in_=sm_t[b][:, 0:1], func=AF.Sqrt, scale=1.0 / D, bias=epst[:, 0:1]),
                 reads=[r_sm_t[b], r_epst], writes=[r_sm_t[b]])
            S.op('dve', lambda e: e.reciprocal(out=sm_t[b][:, 2:3], in_=sm_t[b][:, 1:2]), reads=[r_sm_t[b]], writes=[r_sm_t[b]])
            S.op('dve', lambda e: e.scalar_tensor_tensor(out=vx[b][:], in0=xm[b][:], scalar=sm_t[b][:, 2:3], in1=SC2R[:], op0=ALU.mult, op1=ALU.mult),
                 reads=[r_xm[b], r_sm_t[b], r_c6], writes=[r_vx[b]])
            S.op('pool', lambda e: e.tensor_tensor(out=vx[b][:], in0=vx[b][:], in1=SH2R[:], op=ALU.add), reads=[r_vx[b], r_c6], writes=[r_vx[b]])
            S.op('act', lambda e: e.activation(out=vxb[b][:], in_=vx[b][:], func=AF.Copy), reads=[r_vx[b]], writes=[r_vxb[b]])
            S.op('sp', lambda e: e.dma_start(out=VX[xr, :], in_=vxb[b][:]), reads=[r_vxb[b]], writes=[r_vxd], chan='6vx%d' % b)
            for k in range(8):
                pt = ps_mm[k // 4]
                rp = r_psmm[k // 4]
                S.op('pe', (lambda k, pt: lambda e: e.transpose(out=pt[:, (k % 4) * 128:(k % 4 + 1) * 128], in_=vx[b][:, k * 128:(k + 1) * 128], identity=ident[:]))(k, pt),
                     reads=[r_vx[b], r_ident], writes=[rp])
            S.op('act', lambda e: e.activation(out=vxT[b][:, 0:512], in_=ps_mm[0][:, :], func=AF.Copy), reads=[r_psmm[0]], writes=[r_vxT[b]])
            S.op('dve', lambda e: e.tensor_copy(out=vxT[b][:, 512:1024], in_=ps_mm[1][:, :]), reads=[r_psmm[1]], writes=[r_vxT[b]])
            pr = ps_x[:, 0:16]
            for k in range(8):
                S.op('pe', (lambda k: lambda e: e.matmul(pr, lhsT=vxT[b][:, k * 128:(k + 1) * 128], rhs=RW[:, k, :], start=(k == 0), stop=(k == 7)))(k),
                     reads=[r_vxT[b], r_c6], writes=[r_psx])
            S.op('dve', lambda e: e.tensor_reduce(out=sm_t[b][:, 3:4], in_=pr, axis=AX.X, op=ALU.max), reads=[r_psx], writes=[r_sm_t[b]])
            S.op('dve', lambda e: e.tensor_scalar(out=sm_t[b][:, 4:5], in0=sm_t[b][:, 3:4], scalar1=-1.0, scalar2=None, op0=ALU.mult),
                 reads=[r_sm_t[b]], writes=[r_sm_t[b]])
            S.op('act', lambda e: e.activation(out=ex_t[b][:], in_=pr, func=AF.Exp, bias=sm_t[b][:, 4:5], accum_out=sm_t[b][:, 5:6]),
                 reads=[r_psx, r_sm_t[b]], writes=[r_ex_t[b], r_sm_t[b]])
            S.op('dve', lambda e: e.reciprocal(out=sm_t[b][:, 6:7], in_=sm_t[b][:, 5:6]), reads=[r_sm_t[b]], writes=[r_sm_t[b]])
            S.op('dve', lambda e: e.tensor_scalar(out=aff_t[b][:], in0=ex_t[b][:], scalar1=sm_t[b][:, 6:7], scalar2=None, op0=ALU.mult),
                 reads=[r_ex_t[b], r_sm_t[b]], writes=[r_aff_t[b]])
            S.op('sp', lambda e: e.dma_start(out=AFF[xr, :], in_=aff_t[b][:]), reads=[r_aff_t[b]], writes=[r_affd], chan='6af%d' % b)
            S.op('pe', lambda e: e.transpose(out=ps_x[0:16, 128:256], in_=aff_t[b][:, :], identity=ident[:]), reads=[r_aff_t[b], r_ident], writes=[r_psx2])
            S.op('act', lambda e: e.activation(out=AFFT[:, i * 128:(i + 1) * 128], in_=ps_x[0:16, 128:256], func=AF.Copy), reads=[r_psx2], writes=[r_AFFT])

        p6_loads(0)
        for i in range(NXT + 2):
            if i + 1 < NXT:
                p6_loads(i + 1)
            if 0 <= i - 1 < NXT:
                p6_B(i - 1)
            if 0 <= i - 2 < NXT:
                p6_C(i - 2)
            if i < NXT:
                p6_A(i)
        S.op('sp', lambda e: e.dma_start(out=AFFTd, in_=AFFT[:]), reads=[r_AFFT], writes=[r_affd], chan='6afT')
        S.barrier()
        A.reset(m6)

    if stage >= 7:
        A.reset(m_keep)
        m7 = A.mark()
        svt = A.tile([128, 8], F32, 'svt')
        CTB = A.tile([128, 16, 64], F32, 'CTB')
        zero1 = A.tile([128, 1], F32, 'zero1')
        TIDX = A.tile([128, NE, 8], U32, 'TIDX')
        GATE = A.tile([128, NE, 8], F32, 'GATE')
        m7b = A.mark()
        A128 = A.tile([128, 1024], F32, 'A128')
        cmpb = A.tile([128, 1024], F32, 'cmpb')
        cum = A.tile([128, 1024], F32, 'cum')
        BD = A.tile([128, 128], F32, 'BD')
        LT = A.tile([128, 128], F32, 'LT')
        bs = A.tile([128, 16], F32, 'bs')
        CTe = A.tile([128, 8], F32, 'CTe')
        r_7 = R()
        r_bs = R()
        r_cmp = R()
        r_cum = R()
        r_cumd = R()
        r_tidx = R()
        S.op('sp', lambda e: e.dma_start(out=A128[:], in_=AFFTd.rearrange("e (g i) -> (e g) i", g=8)), reads=[r_affd], writes=[r_7], chan='7a')
        S.op('sp', lambda e: e.dma_start(out=BD[:], in_=bd_in), writes=[r_7], chan='7b')
        S.op('sp', lambda e: e.dma_start(out=LT[:], in_=lt_in), writes=[r_7], chan='7c')
        S.op('sp', lambda e: e.dma_start(out=svt[:], in_=sv_in), writes=[r_7], chan='7d')
        S.op('dve', lambda e: e.memset(bs[:], 0.0), writes=[r_bs])
        S.op('dve', lambda e: e.memset(bs[:, 1:2], 1.0), reads=[r_bs], writes=[r_bs])
        S.op('dve', lambda e: e.memset(zero1[:], 0.0), writes=[r_7])
        pc = ps_x[:, 256:257]
        pc2 = ps_x[:, 256:258]
        r_pc = r_psx
        for it in range(30):
            S.op('dve', lambda e: e.tensor_tensor(out=bs[:, 2:3], in0=bs[:, 0:1], in1=bs[:, 1:2], op=ALU.add), reads=[r_bs], writes=[r_bs])
            S.op('dve', lambda e: e.tensor_scalar(out=bs[:, 2:3], in0=bs[:, 2:3], scalar1=0.5, scalar2=None, op0=ALU.mult), reads=[r_bs], writes=[r_bs])
            S.op('dve', lambda e: e.tensor_scalar(out=cmpb[:], in0=A128[:], scalar1=bs[:, 2:3], scalar2=None, op0=ALU.is_ge),
                 reads=[r_7, r_bs], writes=[r_cmp])
            S.op('dve', lambda e: e.tensor_reduce(out=bs[:, 3:4], in_=cmpb[:], axis=AX.X, op=ALU.add), reads=[r_cmp, r_bs], writes=[r_bs])
            S.op('pe', lambda e: e.matmul(pc2, lhsT=BD[:], rhs=bs[:, 3:5], start=True, stop=True), reads=[r_7, r_bs], writes=[r_pc])
            S.op('dve', lambda e: e.tensor_scalar(out=bs[:, 4:5], in0=pc, scalar1=float(CAP), scalar2=None, op0=ALU.is_ge), reads=[r_pc, r_bs], writes=[r_bs])
            S.op('dve', lambda e: e.tensor_tensor(out=bs[:, 5:6], in0=bs[:, 2:3], in1=bs[:, 0:1], op=ALU.subtract), reads=[r_bs], writes=[r_bs])
            S.op('dve', lambda e: e.tensor_tensor(out=bs[:, 6:7], in0=bs[:, 1:2], in1=bs[:, 2:3], op=ALU.subtract), reads=[r_bs], writes=[r_bs])
            S.op('dve', lambda e: e.scalar_tensor_tensor(out=bs[:, 0:1], in0=bs[:, 5:6], scalar=bs[:, 4:5], in1=bs[:, 0:1], op0=ALU.mult, op1=ALU.add),
                 reads=[r_bs], writes=[r_bs])
            S.op('dve', lambda e: e.scalar_tensor_tensor(out=bs[:, 1:2], in0=bs[:, 6:7], scalar=bs[:, 4:5], in1=bs[:, 2:3], op0=ALU.mult, op1=ALU.add),
                 reads=[r_bs], writes=[r_bs])
        S.op('dve', lambda e: e.tensor_scalar(out=cmpb[:], in0=A128[:], scalar1=bs[:, 0:1], scalar2=None, op0=ALU.is_ge), reads=[r_7, r_bs], writes=[r_cmp])
        S.op('dve', lambda e: e.tensor_reduce(out=bs[:, 3:4], in_=cmpb[:], axis=AX.X, op=ALU.add), reads=[r_cmp, r_bs], writes=[r_bs])
        S.op('pe', lambda e: e.matmul(pc2, lhsT=LT[:], rhs=bs[:, 3:5], start=True, stop=True), reads=[r_7, r_bs], writes=[r_pc])
        S.op('dve', lambda e: e.tensor_copy(out=bs[:, 7:8], in_=pc), reads=[r_pc, r_bs], writes=[r_bs])
        S.op('dve', lambda e: e.tensor_tensor_scan(out=cum[:], data0=cmpb[:], data1=cmpb[:], initial=bs[:, 7:8],
                                                   op0=ALU.add, op1=ALU.max), reads=[r_cmp, r_bs, r_7], writes=[r_cum])
        S.op('sp', lambda e: e.dma_start(out=CUMd2, in_=cum[:]), reads=[r_cum], writes=[r_cumd], chan='7e')
        S.op('dve', lambda e: e.tensor_copy(out=CTe[:], in_=cum[:, 127:1024:128]), reads=[r_cum], writes=[r_cum])
        S.op('sp', lambda e: e.dma_start(out=CTd, in_=CTe[:]), reads=[r_cum], writes=[r_cumd], chan='7f')
        S.op('sp', lambda e: e.dma_start(out=CTB[:].rearrange("p a b -> p (a b)"), in_=CTd.rearrange("a b -> (a b)").partition_broadcast(128)),
             reads=[r_cumd], writes=[r_7], chan='7g')
        S.barrier()
        A.reset(m7b)
        r_tidx_e = [R() for _ in range(NE)]
        wkb, r_wkb = [A.tile([128, 32], F32, 'wkb') for _ in range(2)], [R(), R()]
        wkub, r_wkub = [A.tile([128, 8], U32, 'wkub') for _ in range(2)], [R(), R()]
        c64b, r_c64b = [A.tile([128, 8, 64], F32, 'c64b') for _ in range(2)], [R(), R()]
        crowb, r_crowb = [A.tile([128, 8, 128], F32, 'crowb') for _ in range(2)], [R(), R()]
        c128b, r_c128b = crowb, r_crowb
        growb, r_growb = [A.tile([128, 8, 16], F32, 'growb') for _ in range(2)], [R(), R()]

        def route(ex):
            b = ex % 2
            S.op('dve', lambda e: e.tensor_tensor(out=c64b[b][:], in0=CTB[:, ex:ex + 1, :].to_broadcast([128, 8, 64]),
                                                  in1=svt[:].unsqueeze(2).to_broadcast([128, 8, 64]), op=ALU.is_le),
                 reads=[r_7], writes=[r_c64b[b]])
            S.op('dve', lambda e: e.tensor_reduce(out=wkb[b][:, 0:8], in_=c64b[b][:], axis=AX.X, op=ALU.add), reads=[r_c64b[b]], writes=[r_wkb[b]])
            S.op('dve', lambda e: e.tensor_scalar(out=wkb[b][:, 8:16], in0=wkb[b][:, 0:8], scalar1=float(ex * 64), scalar2=None, op0=ALU.add),
                 reads=[r_wkb[b]], writes=[r_wkb[b]])
            S.op('dve', lambda e: e.tensor_copy(out=wkub[b][:], in_=wkb[b][:, 8:16]), reads=[r_wkb[b]], writes=[r_wkub[b]])
            for st in range(8):
                S.op('pool', (lambda st: lambda e: e.indirect_dma_start(out=crowb[b][:, st, :], out_offset=None, in_=CUMd3,
                                                                       in_offset=bass.IndirectOffsetOnAxis(ap=wkub[b][:, st:st + 1], axis=0)))(st),
                     reads=[r_wkub[b], r_cumd], writes=[r_crowb[b]], chan='7h%d_%d' % (b, st))
            S.op('dve', lambda e: e.tensor_tensor(out=c128b[b][:], in0=crowb[b][:], in1=svt[:].unsqueeze(2).to_broadcast([128, 8, 128]), op=ALU.is_le),
                 reads=[r_crowb[b], r_7], writes=[r_c128b[b]])
            S.op('dve', lambda e: e.tensor_reduce(out=wkb[b][:, 16:24], in_=c128b[b][:], axis=AX.X, op=ALU.add), reads=[r_c128b[b], r_wkb[b]], writes=[r_wkb[b]])
            S.op('dve', lambda e: e.scalar_tensor_tensor(out=wkb[b][:, 24:32], in0=wkb[b][:, 0:8], scalar=128.0, in1=wkb[b][:, 16:24], op0=ALU.mult, op1=ALU.add),
                 reads=[r_wkb[b]], writes=[r_wkb[b]])
            S.op('dve', lambda e: e.tensor_copy(out=TIDX[:, ex, :], in_=wkb[b][:, 24:32]), reads=[r_wkb[b]], writes=[r_tidx_e[ex]])
            for st in range(8):
                S.op('pool', (lambda st: lambda e: e.indirect_dma_start(out=growb[b][:, st, :], out_offset=None, in_=AFF,
                                                                       in_offset=bass.IndirectOffsetOnAxis(ap=TIDX[:, ex, st:st + 1], axis=0)))(st),
                     reads=[r_tidx_e[ex], r_affd], writes=[r_growb[b]], chan='7i%d_%d' % (b, st))
            S.op('dve', lambda e: e.tensor_copy(out=GATE[:, ex, :], in_=growb[b][:, :, ex]), reads=[r_growb[b], r_tidx_e[ex]], writes=[r_tidx_e[ex]])

        if "TIDXd" in dbg:
            for ex in range(NE):
                route(ex)
            r_tidx = R()
            S.op('dve', lambda e: e.memset(zero1[:], 0.0), reads=[r_tidx_e[ex] for ex in range(NE)], writes=[r_tidx])
        if "TIDXd" in dbg:
            S.op('sp', lambda e: e.dma_start(out=TIDXd, in_=TIDX[:].rearrange("p a b -> p (a b)")), reads=[r_tidx], chan='7z')
            S.op('sp', lambda e: e.dma_start(out=GATEd, in_=GATE[:].rearrange("p a b -> p (a b)")), reads=[r_tidx], chan='7y')

        r_moe = R()
        zt8 = A.tile([128, D], F32, 'zt8')
        r_zt8 = R()
        S.op('pool', lambda e: e.memset(zt8[:], 0.0), writes=[r_zt8])
        for i in range(NXT):
            S.op('sp', (lambda i: lambda e: e.dma_start(out=MOE[i * 128:(i + 1) * 128, :], in_=zt8[:]))(i),
                 reads=[r_zt8], writes=[r_moe], chan='8z%d' % (i % 2))
        WG, r_WG = [A.tile([128, 8, D], BF16, 'WG') for _ in range(2)], [R(), R()]
        WU, r_WU = [A.tile([128, 8, D], BF16, 'WU') for _ in range(2)], [R(), R()]
        WD, r_WD = [A.tile([128, 8, D], BF16, 'WD') for _ in range(2)], [R(), R()]
        xs_t, r_xs_t = [A.tile([128, D], BF16, 'xs_t') for _ in range(8)], [R() for _ in range(8)]
        xsT = A.tile([128, 8, CAP], BF16, 'xsT')
        r_xsT = R()
        hT = A.tile([128, 8, CAP], BF16, 'hT')
        r_hT = R()
        sg, r_sg = [A.tile([128, 512], F32, 'sg') for _ in range(2)], [R(), R()]
        yb, r_yb = [A.tile([128, D], F32, 'yb') for _ in range(2)], [R(), R()]

        NWS = 6
        wst, r_wst = [A.tile([128, D], F32, 'wst') for _ in range(NWS)], [R() for _ in range(NWS)]
        wcnt = {'n': 0}

        def wgen(ex):
            b = ex % 2
            items = []
            for (src, dst, rr) in ((wg_in, WG, r_WG), (wu_in, WU, r_WU), (wd_in, WD, r_WD)):
                v = src[ex].rearrange("(k p) c -> p k c", p=128)
                for k in range(8):
                    items.append((v, dst, rr, k))

            def dma(n):
                v, dst, rr, k = items[n]
                i = (wcnt['n'] + n) % NWS
                S.op('sp', (lambda v, k, i: lambda e: e.dma_start(out=wst[i][:], in_=v[:, k, :]))(v, k, i), writes=[r_wst[i]], chan='8w%d' % i)
            for n in range(min(NWS, len(items))):
                dma(n)
            for n in range(len(items)):
                v, dst, rr, k = items[n]
                i = (wcnt['n'] + n) % NWS
                S.op('dve', (lambda dst, k, b, i: lambda e: e.tensor_copy(out=dst[b][:, k, :], in_=wst[i][:]))(dst, k, b, i),
                     reads=[r_wst[i]], writes=[rr[b]])
                if n + NWS < len(items):
                    dma(n + NWS)
                yield
            wcnt['n'] += len(items)

        def step(g):
            if g is not None:
                try:
                    next(g)
                except StopIteration:
                    pass

        def gathers(ex):
            for st in range(8):
                S.op('pool', (lambda st: lambda e: e.indirect_dma_start(out=xs_t[st][:], out_offset=None, in_=VX,
                                                                       in_offset=bass.IndirectOffsetOnAxis(ap=TIDX[:, ex, st:st + 1], axis=0)))(st),
                     reads=[r_tidx_e[ex], r_vxd], writes=[r_xs_t[st]], chan='8g%d' % st)

        for _ in wgen(0):
            pass
        n8 = 0
        if "TIDXd" not in dbg:
            route(0)
        gathers(0)
        for ex in range(NE):
            wg = None
            if ex + 1 < NE:
                if "TIDXd" not in dbg:
                    route(ex + 1)
                wg = wgen(ex + 1)
            wb = ex % 2
            for st in range(8):
                b = n8 % 2
                n8 += 1

                def gat(ex=ex, st=st, b=b):
                    for k in range(8):
                        S.op('pe', (lambda k: lambda e: e.transpose(out=ps_tr[b][:, k * 128:(k + 1) * 128], in_=xs_t[st][:, k * 128:(k + 1) * 128],
                                                                    identity=identb[:]))(k),
                             reads=[r_xs_t[st], r_identb], writes=[r_pstr[b]])
                    S.op('act' if b else 'dve',
                         (lambda e: e.activation(out=xsT[:, :, st * 128:(st + 1) * 128], in_=ps_tr[b][:, :].rearrange("p (k t) -> p k t", k=8), func=AF.Copy)) if b else
                         (lambda e: e.tensor_copy(out=xsT[:, :, st * 128:(st + 1) * 128], in_=ps_tr[b][:, :].rearrange("p (k t) -> p k t", k=8))),
                         reads=[r_pstr[b]], writes=[r_xsT])
                gat()
            n_gu = 0
            for fc in range(8):
                for sbk in range(2):
                    q = n_gu % 2
                    n_gu += 1

                    def gu(fc=fc, sbk=sbk, q=q, wb=wb):
                        pg = ps_mm[q]
                        pu = ps_mod[q]
                        cs = slice(sbk * 512, (sbk + 1) * 512)
                        for k in range(8):
                            S.op('pe', (lambda k: lambda e: e.matmul(pg[:, :], lhsT=WG[wb][:, k, fc * 128:(fc + 1) * 128], rhs=xsT[:, k, cs],
                                                                     start=(k == 0), stop=(k == 7)))(k),
                                 reads=[r_WG[wb], r_xsT], writes=[r_psmm[q]])
                        for k in range(8):
                            S.op('pe', (lambda k: lambda e: e.matmul(pu[:, :], lhsT=WU[wb][:, k, fc * 128:(fc + 1) * 128], rhs=xsT[:, k, cs],
                                                                     start=(k == 0), stop=(k == 7)))(k),
                                 reads=[r_WU[wb], r_xsT], writes=[r_psmod[q]])
                        S.op('act', lambda e: e.activation(out=sg[q][:], in_=pg[:, :], func=AF.Silu), reads=[r_psmm[q]], writes=[r_sg[q]])
                        S.op('dve', lambda e: e.tensor_tensor(out=hT[:, fc, cs], in0=pu[:, :], in1=sg[q][:], op=ALU.mult),
                             reads=[r_psmod[q], r_sg[q]], writes=[r_hT])
                    gu()
                    step(wg)
            if ex + 1 < NE:
                gathers(ex + 1)
            for st in range(8):
                b = st % 2

                def dn_(ex=ex, st=st, b=b, wb=wb):
                    for cb in range(2):
                        pt = ps_mm[2] if cb == 0 else ps_x
                        rp = r_psmm[2] if cb == 0 else r_psx
                        for fc in range(8):
                            S.op('pe', (lambda fc, cb, pt: lambda e: e.matmul(pt[:, :], lhsT=hT[:, fc, st * 128:(st + 1) * 128],
                                                                              rhs=WD[wb][:, fc, cb * 512:(cb + 1) * 512],
                                                                              start=(fc == 0), stop=(fc == 7)))(fc, cb, pt),
                                 reads=[r_hT, r_WD[wb]], writes=[rp])
                        S.op('act', (lambda cb, pt: lambda e: e.activation(out=yb[b][:, cb * 512:(cb + 1) * 512], in_=pt[:, :], func=AF.Copy,
                                                                           scale=GATE[:, ex, st:st + 1]))(cb, pt),
                             reads=[rp, r_tidx_e[ex]], writes=[r_yb[b]])
                    S.op('pool', lambda e: e.indirect_dma_start(out=MOE, out_offset=bass.IndirectOffsetOnAxis(ap=TIDX[:, ex, st:st + 1], axis=0),
                                                                in_=yb[b][:], in_offset=None, compute_op=ALU.add),
                         reads=[r_yb[b], r_tidx_e[ex], r_moe], writes=[r_moe], chan='8s')
                dn_()
                step(wg)
            if wg is not None:
                for _ in wg:
                    pass
        S.barrier()
        A.reset(m7)

    if stage >= 9:
        m9 = A.mark()
        G2R = A.tile([128, D], F32, 'G2R')
        fnw = A.tile([128, D], F32, 'fnw')
        MOD3 = A.tile([2, 6 * D], F32, 'MOD3')
        r_9 = R()
        S.op('sp', lambda e: e.dma_start(out=MOD3[:], in_=MODd), reads=[r_modd], writes=[r_9], chan='9a')
        S.op('sp', lambda e: e.dma_start(out=fnw[:], in_=fnw_in), writes=[r_9], chan='9b')
        for hb in range(2):
            S.op('pe', (lambda hb: lambda e: e.matmul(ps_mm[hb][:, :], lhsT=sel2[0:2, :], rhs=MOD3[0:2, 5120 + hb * 512:5120 + (hb + 1) * 512],
                                                      start=True, stop=True))(hb),
                 reads=[r_sel2, r_9], writes=[r_psmm[hb]])
            S.op('dve', (lambda hb: lambda e: e.tensor_copy(out=G2R[:, hb * 512:(hb + 1) * 512], in_=ps_mm[hb][:, :]))(hb),
                 reads=[r_psmm[hb]], writes=[r_9])
        xm9, r_xm9 = [A.tile([128, D], F32, 'xm9') for _ in range(2)], [R(), R()]
        mo9, r_mo9 = [A.tile([128, D], F32, 'mo9') for _ in range(2)], [R(), R()]
        y9, r_y9 = [A.tile([128, D], F32, 'y9') for _ in range(2)], [R(), R()]
        s9, r_s9 = [A.tile([128, 4], F32, 's9') for _ in range(2)], [R(), R()]
        junk9 = A.tile([128, D], BF16, 'junk9')
        r_j9 = R()
        for i in range(NXT + 1):
            if i < NXT:
                b = i % 2
                xr = slice(i * 128, (i + 1) * 128)
                S.op('sp', (lambda b, xr: lambda e: e.dma_start(out=xm9[b][:], in_=XMID[xr, :]))(b, xr), reads=[r_xmid], writes=[r_xm9[b]], chan='9x%d' % b)
                S.op('sp', (lambda b, xr: lambda e: e.dma_start(out=mo9[b][:], in_=MOE[xr, :]))(b, xr), reads=[r_moe], writes=[r_mo9[b]], chan='9m%d' % b)
            if i > 0:
                j = i - 1
                b = j % 2
                xr = slice(j * 128, (j + 1) * 128)

                def fin(b=b, xr=xr):
                    S.op('dve', lambda e: e.tensor_tensor(out=mo9[b][:], in0=mo9[b][:], in1=G2R[:], op=ALU.mult), reads=[r_mo9[b], r_9], writes=[r_mo9[b]])
                    S.op('pool', lambda e: e.tensor_tensor(out=xm9[b][:], in0=xm9[b][:], in1=mo9[b][:], op=ALU.add), reads=[r_mo9[b], r_xm9[b]], writes=[r_xm9[b]])
                    S.op('act', lambda e: e.activation(out=junk9[:], in_=xm9[b][:], func=AF.Square, accum_out=s9[b][:, 0:1]),
                         reads=[r_xm9[b]], writes=[r_j9, r_s9[b]])
                    S.op('act', lambda e: e.activation(out=s9[b][:, 1:2], in_=s9[b][:, 0:1], func=AF.Sqrt, scale=1.0 / D, bias=epst[:, 0:1]),
                         reads=[r_s9[b], r_epst], writes=[r_s9[b]])
                    S.op('dve', lambda e: e.reciprocal(out=s9[b][:, 2:3], in_=s9[b][:, 1:2]), reads=[r_s9[b]], writes=[r_s9[b]])
                    S.op('dve', lambda e: e.scalar_tensor_tensor(out=y9[b][:], in0=xm9[b][:], scalar=s9[b][:, 2:3], in1=fnw[:], op0=ALU.mult, op1=ALU.mult),
                         reads=[r_xm9[b], r_s9[b], r_9], writes=[r_y9[b]])
                    S.op('sp', lambda e: e.dma_start(out=out[xr, :], in_=y9[b][:]), reads=[r_y9[b]], chan='9o%d' % b)
                fin()

    if stage < 99:
        zt = A.tile([128, D], F32, 'zt')
        r_zt = R()
        S.op('dve', lambda e: e.memset(zt[:], 0.0), writes=[r_zt])
        S.op('sp', lambda e: e.dma_start(out=out[0:128, :], in_=zt[:]), reads=[r_zt], chan='c_out')

    S.emit()
    return nc


def prep_inputs(inp, b):
    f = np.float32
    x, c, ctx, c_ctx = inp['x'], inp['c'], inp['ctx'], inp['c_ctx']
    m = {}
    m['xc'] = np.ascontiguousarray(np.concatenate([ctx[b], x[b]], axis=0), dtype=f)
    cv = np.stack([c[b], c_ctx], axis=-1).reshape(8, 128, 2).transpose(1, 0, 2)
    m['cvec'] = np.ascontiguousarray(cv, dtype=f)
    m['ada_w'] = np.ascontiguousarray(inp['ada_w'][0], dtype=f)
    m['ada_b2'] = np.ascontiguousarray(np.tile(inp['ada_b'][0][None, :], (2, 1)), dtype=f)
    m['n1w'] = np.ascontiguousarray(inp['norm1_w'][0].reshape(8, 128).T, dtype=f)
    m['w_in'] = np.ascontiguousarray(inp['w_in'][0], dtype=f)
    m['cident'] = np.eye(128, dtype=f)
    sel = np.zeros((2, 128), f)
    sel[0] = 1.0
    m['csel'] = sel
    m['lbl'] = np.ascontiguousarray(np.tile(inp['hg_lb_logits'].reshape(1, 2048), (128, 1)), dtype=f)
    m['gateb'] = np.ascontiguousarray(inp['ml_gate_b'][0].reshape(16, 1), dtype=f)
    m['gatebrow'] = np.ascontiguousarray(np.tile(inp['ml_gate_b'][0][None, :], (128, 1)), dtype=f)
    m['jm_in'] = np.ascontiguousarray(np.eye(128, dtype=f)[::-1])
    cw = inp['conv_w'][0].reshape(9, 8, 128).transpose(2, 1, 0)
    m['convw'] = np.ascontiguousarray(cw, dtype=f)
    m['convb'] = np.ascontiguousarray(inp['conv_b'][0].reshape(8, 128).T, dtype=f)
    jj, ii = np.meshgrid(np.arange(128), np.arange(128), indexing='ij')
    mask = np.stack([(jj <= ii), (jj >= ii)], axis=1).astype(f)
    m['maskin'] = np.ascontiguousarray(mask)
    trix = np.zeros((128, 2, 132), f)
    for p, (tri, mid) in enumerate([((jj <= ii).astype(f), 63), ((jj >= ii).astype(f), 64)]):
        trix[:, p, 0:128] = tri - tri[:, mid:mid + 1]
        trix[:, p, 128] = 1.0 - tri[:, mid]
        trix[:, p, 129] = tri[:, mid]
        trix[:, p, 130] = 1.0
    m['trixin'] = trix
    selh = np.zeros((4, 4, 128), f)
    for h in range(4):
        selh[h, h, :] = 1.0
    m['selhin'] = selh
    kk_, mm_ = np.meshgrid(np.arange(128), np.arange(128), indexing='ij')
    m['bd_in'] = np.ascontiguousarray((kk_ // 8 == mm_ // 8).astype(f))
    m['lt_in'] = np.ascontiguousarray(((kk_ // 8 == mm_ // 8) & (kk_ % 8 < mm_ % 8)).astype(f))
    m['sv_in'] = np.ascontiguousarray((np.arange(8)[None, :] * 128 + np.arange(128)[:, None]).astype(f))
    m['wg_in'] = np.ascontiguousarray(inp['exp_w_gate'][0], dtype=f)
    m['wu_in'] = np.ascontiguousarray(inp['exp_w_up'][0], dtype=f)
    m['wd_in'] = np.ascontiguousarray(inp['exp_w_down'][0], dtype=f)
    m['fnw_in'] = np.ascontiguousarray(np.tile(inp['final_norm_w'][None, :], (128, 1)), dtype=f)
    m['hgnw_in'] = np.ascontiguousarray(np.tile(inp['hg_norm_w'][0][None, :], (128, 1)), dtype=f)
    m['mlnw_in'] = np.ascontiguousarray(np.tile(inp['ml_norm_w'][0][None, :], (128, 1)), dtype=f)
    m['n2r_in'] = np.ascontiguousarray(np.tile(inp['norm2_w'][0][None, :], (128, 1)), dtype=f)
    m['rw_in'] = np.ascontiguousarray(inp['router_w'][0], dtype=f)
    m['wout_in'] = np.ascontiguousarray(inp['w_out'][0], dtype=f)
    return m


def kernel(**inputs):
    inp = {k: np.asarray(v) for k, v in inputs.items()}
    nc = build()
    in_maps = [prep_inputs(inp, c % 4) for c in range(8)]
    res = run_bass_kernel_spmd(nc, in_maps, core_ids=list(range(8)))
    outs = [res.results[c]["out"] for c in range(4)]
    return np.stack(outs, axis=0).astype(np.float32)
```

```python
import numpy as np
from contextlib import ExitStack
import concourse.bass as bass
import concourse.mybir as mybir
from concourse.bass_utils import run_bass_kernel_spmd

F32 = mybir.dt.float32
BF16 = mybir.dt.bfloat16
U32 = mybir.dt.uint32
I32 = mybir.dt.int32
ALU = mybir.AluOpType
AF = mybir.ActivationFunctionType
AX = mybir.AxisListType

D = 1024
SEQ = 8192
CTX = 256
T = SEQ + CTX
NT = T // 128
NXT = SEQ // 128
PROJ = 4624
NE = 16
CAP = 1024
EPS = 1e-6
ENG = ['pe', 'act', 'dve', 'pool', 'sp']
import os as _os
P5MODE = _os.environ.get('P5MODE', 'hg,ml')
FASTSIM = _os.environ.get('FASTSIM', '') == '1'


class Res:
    __slots__ = ('name', 'w', 'r', 'excl')

    def __init__(self, name='', excl=False):
        self.name = name
        self.w = None
        self.r = {}
        self.excl = excl


class Sched:
    def __init__(self, nc, es):
        self.nc = nc
        self.es = es
        self.prog = {e: [] for e in ENG}
        self.cnt = {e: 0 for e in ENG}
        self.sem = {}
        for e in ENG:
            self.sem[e] = es.enter_context(nc.semaphore('S_' + e))
        self.waited = {e: {} for e in ENG}
        self.dcnt = {}
        self.nops = 0
        self.chanmap = {}
        self.freeslots = []
        self.freeslots_sw = []
        self.swslots = set()

    def _need(self, eng, ev, waits):
        if ev is None:
            return
        k, v = ev
        if k == eng and eng == 'pe':
            return
        if self.waited[eng].get(k, 0) >= v:
            return
        if waits.get(k, 0) < v:
            waits[k] = v

    def op(self, eng, fn, reads=(), writes=(), chan=None):
        if any(r.excl for r in reads):
            writes = list(writes) + [r for r in reads if r.excl and r not in writes]
            reads = [r for r in reads if not r.excl]
        waits = {}
        for r in reads:
            self._need(eng, r.w, waits)
        for w in writes:
            self._need(eng, w.w, waits)
            for k, v in w.r.items():
                self._need(eng, (k, v), waits)
        if chan is not None:
            if chan not in self.chanmap:
                pool_ = self.freeslots_sw if eng == 'pool' else self.freeslots
                if pool_:
                    self.chanmap[chan] = pool_.pop()
                else:
                    slot = 'slot%d' % len(self.sem)
                    self.sem[slot] = self.es.enter_context(self.nc.semaphore('D_%d' % len(self.sem)))
                    self.dcnt[slot] = 0
                    self.chanmap[chan] = slot
                    if eng == 'pool':
                        self.swslots.add(slot)
            chan = self.chanmap[chan]
            if self.dcnt[chan] > 0:
                self._need(eng, (chan, self.dcnt[chan]), waits)
            self.dcnt[chan] += 16
            ev = (chan, self.dcnt[chan])
            inc = 16
        else:
            self.cnt[eng] += 1
            ev = (eng, self.cnt[eng])
            inc = 1
        for k, v in waits.items():
            self.prog[eng].append(('w', k, v))
            self.waited[eng][k] = v
        self.prog[eng].append(('o', fn, ev[0], inc))
        self.nops += 1
        for r in reads:
            if r.r.get(ev[0], 0) < ev[1]:
                r.r[ev[0]] = ev[1]
        for w in writes:
            w.w = ev
            w.r = {}
        return ev

    def barrier(self):
        for e in ENG:
            for k in list(self.sem.keys()):
                v = self.cnt[k] if k in self.cnt else self.dcnt.get(k, 0)
                if v > 0 and k != e and self.waited[e].get(k, 0) < v:
                    self.prog[e].append(('w', k, v))
                    self.waited[e][k] = v
        allfree = set(self.chanmap.values()) | set(self.freeslots) | set(self.freeslots_sw)
        self.freeslots = sorted(x for x in allfree if x not in self.swslots)
        self.freeslots_sw = sorted(x for x in allfree if x in self.swslots)
        self.chanmap = {}

    def emit(self):
        nc = self.nc
        for k in list(self.sem.keys()):
            v = self.cnt[k] if k in self.cnt else self.dcnt.get(k, 0)
            if v > 0 and k != 'sp':
                self.prog['sp'].append(('w', k, v))
        sem = self.sem
        prog = self.prog

        def run(name):
            def f(eng):
                for it in prog[name]:
                    if it[0] == 'w':
                        eng.wait_ge(sem[it[1]], it[2])
                    else:
                        it[1](eng).then_inc(sem[it[2]], it[3])
            return f

        with nc.Block() as block:
            block.tensor(run('pe'))
            block.scalar(run('act'))
            block.vector(run('dve'))
            block.gpsimd(run('pool'))
            block.sync(run('sp'))


class Alloc:
    def __init__(self, nc, lo=17408, hi=229376):
        self.nc = nc
        self.lo = lo
        self.hi = hi
        self.cur = lo
        self.n = 0

    def mark(self):
        return self.cur

    def reset(self, m):
        self.cur = m

    def tile(self, shape, dtype, name=None):
        esz = 2 if dtype == BF16 else 4
        n = 1
        for s in shape[1:]:
            n *= s
        nbytes = (n * esz + 63) // 64 * 64
        assert self.cur + nbytes <= self.hi, ('SBUF overflow', name, self.cur, nbytes)
        self.n += 1
        t = self.nc.alloc_sbuf_tensor_at('%s_%d' % (name or 't', self.n), list(shape), dtype, offset=self.cur)
        self.cur += nbytes
        return t


def build(stage=99, debug=()):
    nc = bass.Bass("TRN2", target_bir_lowering=False)
    es = ExitStack()
    S = Sched(nc, es)
    A = Alloc(nc)
    dbg = set(debug)

    def dram_in(name, shape, dt=F32):
        return nc.dram_tensor(name, list(shape), dt, kind="ExternalInput").ap()

    def dram_scr(name, shape, dt=F32):
        kind = "ExternalOutput" if name in dbg else "Internal"
        return nc.dram_tensor(name, list(shape), dt, kind=kind).ap()

    xc = dram_in("xc", [T, D])
    cvec = dram_in("cvec", [128, 8, 2])
    ada_w = dram_in("ada_w", [D, 6 * D])
    ada_b2 = dram_in("ada_b2", [2, 6 * D])
    n1w = dram_in("n1w", [128, 8])
    w_in = dram_in("w_in", [D, PROJ])
    cident = dram_in("cident", [128, 128])
    csel = dram_in("csel", [2, 128])
    lbl = dram_in("lbl", [128, 4 * 512])
    gateb = dram_in("gateb", [16, 1])
    gatebrow = dram_in("gatebrow", [128, 16])
    jm_in = dram_in("jm_in", [128, 128])
    convw = dram_in("convw", [128, 8, 9])
    convb = dram_in("convb", [128, 8])
    maskin = dram_in("maskin", [128, 2, 128])
    trixin = dram_in("trixin", [128, 2, 132])
    selhin = dram_in("selhin", [4, 4, 128])
    bd_in = dram_in("bd_in", [128, 128])
    lt_in = dram_in("lt_in", [128, 128])
    sv_in = dram_in("sv_in", [128, 8])
    wg_in = dram_in("wg_in", [NE, D, D])
    wu_in = dram_in("wu_in", [NE, D, D])
    wd_in = dram_in("wd_in", [NE, D, D])
    fnw_in = dram_in("fnw_in", [128, D])
    hgnw_in = dram_in("hgnw_in", [128, 512])
    mlnw_in = dram_in("mlnw_in", [128, 512])
    n2r_in = dram_in("n2r_in", [128, D])
    rw_in = dram_in("rw_in", [D, 16])
    wout_in = dram_in("wout_in", [D, D])
    out = nc.dram_tensor("out", [SEQ if stage >= 99 else 128, D], F32, kind="ExternalOutput").ap()

    MODd = nc.dram_tensor("MODd", [2, 6 * D], F32, kind=("ExternalOutput" if "MODd" in dbg else "Internal")).ap()
    QTH = dram_scr("QTH", [512, T], BF16)
    PRE = dram_scr("PRE", [1024, T], BF16)
    GATES = dram_scr("GATES", [16, T], F32)
    GATEST = dram_scr("GATEST", [T, 16], F32)
    HGV = dram_scr("HGV", [T, 512], BF16)
    MLV = dram_scr("MLV", [T, 512], BF16)
    GG = [dram_scr("GG%d" % d, [T, 512], F32) for d in range(2)]
    KK = [dram_scr("KK%d" % d, [T, 512], BF16) for d in range(2)]
    HGG = dram_scr("HGG", [T, 512], BF16)
    MLO = dram_scr("MLO", [T, 512], BF16)
    QKT = dram_scr("QKT", [1024, T], BF16)
    TOKd = dram_scr("TOKd", [128, NT * 12], F32)
    OH = [dram_scr("OH%d" % d, [SEQ, 512], F32) for d in range(2)]
    XMID = dram_scr("XMID", [SEQ, D], F32)
    MOE = dram_scr("MOE", [SEQ, D], F32)
    CUMd2 = dram_scr("CUMd", [128, 1024], F32)
    CUMd3 = CUMd2.rearrange("a (t r) -> (a t) r", r=128)
    CTd = dram_scr("CTd", [128, 8], F32)
    TIDXd = dram_scr("TIDXd", [128, 128], U32)
    GATEd = dram_scr("GATEd", [128, 128], F32)
    VX = dram_scr("VX", [SEQ, D], BF16)
    AFF = dram_scr("AFF", [SEQ, 16], F32)
    AFFTd = dram_scr("AFFTd", [16, SEQ], F32)
    OM = [dram_scr("OM%d" % d, [SEQ, 512], F32) for d in range(2)]

    def R(name=''):
        return Res(name)

    ident = A.tile([128, 128], F32, 'ident')
    identb = A.tile([128, 128], BF16, 'identb')
    sel2 = A.tile([2, 128], F32, 'sel2')
    r_ident, r_identb, r_sel2 = R(), R(), R()
    S.op('sp', lambda e: e.dma_start(out=ident[:], in_=cident), writes=[r_ident], chan='c_ident')
    S.op('sp', lambda e: e.dma_start(out=sel2[:], in_=csel), writes=[r_sel2], chan='c_sel2')
    S.op('dve', lambda e: e.tensor_copy(out=identb[:], in_=ident[:]), reads=[r_ident], writes=[r_identb])

    epst = A.tile([128, 1], F32, 'epst')
    r_epst = R()
    S.op('dve', lambda e: e.memset(epst[:], EPS), writes=[r_epst])
    m_keep = A.mark()
    MODT = A.tile([128, 4, 8, 2], F32, 'MODT')
    n1t = A.tile([128, 8], F32, 'n1t')
    scale1 = A.tile([128, 8, 2], F32, 'scale1')
    lbr = A.tile([128, 2, 512], F32, 'lbr')
    omlb = A.tile([128, 2, 512], F32, 'omlb')
    gbt = A.tile([16, 1], F32, 'gbt')
    gbrow = A.tile([128, 16], F32, 'gbrow')
    Jm = A.tile([128, 128], F32, 'Jm')
    m1 = A.mark()
    cv = A.tile([128, 8, 2], F32, 'cv')
    scv = A.tile([128, 8, 2], F32, 'scv')
    adab = A.tile([2, 6 * D], F32, 'adab')
    MOD = A.tile([2, 6 * D], F32, 'MOD')
    lbt = A.tile([128, 4 * 512], F32, 'lbt')
    r_cv, r_scv, r_adab, r_MOD = R(), R(), R(), R()
    S.op('sp', lambda e: e.dma_start(out=cv[:], in_=cvec), writes=[r_cv], chan='c_cv')
    S.op('sp', lambda e: e.dma_start(out=adab[:], in_=ada_b2), writes=[r_adab], chan='c_adab')
    S.op('act', lambda e: e.activation(out=scv[:], in_=cv[:], func=AF.Silu), reads=[r_cv], writes=[r_scv])
    awb = [A.tile([128, 8, 512], F32, 'awb') for _ in range(2)]
    r_awb = [R(), R()]
    ps_mod = [nc.alloc_psum_tensor('ps_mod%d' % i, [128, 512], F32) for i in range(2)]
    r_psmod = [Res('psmod0', True), Res('psmod1', True)]
    ada_v = ada_w.rearrange("(k p) c -> p k c", p=128)
    for cb in range(13):
        if cb < 12:
            b = cb % 2
            S.op('sp', (lambda b, cb: lambda e: e.dma_start(out=awb[b][:], in_=ada_v[:, :, cb * 512:(cb + 1) * 512]))(b, cb),
                 writes=[r_awb[b]], chan='awb%d' % b)
        if cb > 0:
            c0 = cb - 1
            b = c0 % 2
            for k in range(8):
                S.op('pe', (lambda b, k: lambda e: e.matmul(ps_mod[b][0:2, :], lhsT=scv[:, k, :], rhs=awb[b][:, k, :],
                                                           start=(k == 0), stop=(k == 7)))(b, k),
                     reads=[r_scv, r_awb[b]], writes=[r_psmod[b]])
            S.op('dve', (lambda b, c0: lambda e: e.tensor_tensor(out=MOD[0:2, c0 * 512:(c0 + 1) * 512], in0=ps_mod[b][0:2, :],
                                                                in1=adab[0:2, c0 * 512:(c0 + 1) * 512], op=ALU.add))(b, c0),
                 reads=[r_psmod[b], r_adab], writes=[r_MOD])
    r_modd = R()
    S.op('sp', lambda e: e.dma_start(out=MODd, in_=MOD[:]), reads=[r_MOD], writes=[r_modd], chan='c_modd')

    r_MODT = R()
    ps_t = ps_mod[0]
    r_pst = r_psmod[0]
    offs = [0, 1024, 3072, 4096]
    for vi in range(4):
        for k in range(8):
            c0 = offs[vi] + k * 128
            j = (vi * 8 + k) * 2
            S.op('pe', (lambda c0, j: lambda e: e.matmul(ps_t[:, j:j + 2], lhsT=MOD[0:2, c0:c0 + 128], rhs=ident[0:2, 0:2],
                                                        start=True, stop=True))(c0, j),
                 reads=[r_MOD, r_ident], writes=[r_pst])
    S.op('dve', lambda e: e.tensor_copy(out=MODT[:].rearrange("p a k s -> p (a k s)"), in_=ps_t[:, 0:64]),
         reads=[r_pst], writes=[r_MODT])
    r_n1t = R()
    S.op('sp', lambda e: e.dma_start(out=n1t[:], in_=n1w), writes=[r_n1t], chan='c_n1t')
    r_scale1 = R()
    S.op('dve', lambda e: e.scalar_tensor_tensor(out=scale1[:], in0=MODT[:, 1, :, :], scalar=1.0,
                                                 in1=n1t[:].unsqueeze(2).to_broadcast([128, 8, 2]), op0=ALU.add, op1=ALU.mult),
         reads=[r_MODT, r_n1t], writes=[r_scale1])

    r_lbt, r_lbr = R(), R()
    S.op('sp', lambda e: e.dma_start(out=lbt[:], in_=lbl), writes=[r_lbt], chan='c_lbt')
    S.op('dve', lambda e: e.tensor_tensor(out=lbr[:].rearrange("p a c -> p (a c)"), in0=lbt[:, 0:1024], in1=lbt[:, 1024:2048],
                                          op=ALU.subtract), reads=[r_lbt], writes=[r_lbr])
    S.op('act', lambda e: e.activation(out=lbr[:], in_=lbr[:], func=AF.Sigmoid), reads=[r_lbr], writes=[r_lbr])
    S.op('dve', lambda e: e.tensor_scalar(out=omlb[:], in0=lbr[:], scalar1=-1.0, scalar2=1.0, op0=ALU.mult, op1=ALU.add),
         reads=[r_lbr], writes=[r_lbr])
    r_gbt = R()
    S.op('sp', lambda e: e.dma_start(out=gbt[:], in_=gateb), writes=[r_gbt], chan='c_gbt')
    S.op('sp', lambda e: e.dma_start(out=gbrow[:], in_=gatebrow), writes=[r_gbt], chan='c_gbrow')
    S.op('sp', lambda e: e.dma_start(out=Jm[:], in_=jm_in), writes=[r_gbt], chan='c_jm')
    S.barrier()
    A.reset(m1)

    if stage >= 2:
        m2 = A.mark()
        WIN = A.tile([128, 8, PROJ], BF16, 'WIN')
        r_WIN = R()
        w_in_v = w_in.rearrange("(k p) c -> p k c", p=128)
        CB = [(0, 1536), (1536, 3072), (3072, PROJ)]
        if FASTSIM:
            S.op('dve', lambda e: e.memset(WIN[:], 0.01), writes=[r_WIN])
        for k in range(0 if FASTSIM else 8):
            for (c0, c1) in CB:
                S.op('pool', (lambda k, c0, c1: lambda e: e.dma_start(out=WIN[:, k, c0:c1], in_=w_in_v[:, k, c0:c1]))(k, c0, c1),
                     writes=[r_WIN], chan='c_win')
        NXB = 3
        xt = [A.tile([128, D], F32, 'xt') for _ in range(NXB)]
        r_xt = [R() for _ in range(NXB)]
        xs = [A.tile([128, D], BF16, 'xs') for _ in range(2)]
        r_xs = [R(), R()]
        junk = A.tile([128, D], BF16, 'junk')
        r_junk = R()
        ss = [A.tile([128, 2], F32, 'ss') for _ in range(2)]
        r_ss = [R(), R()]
        uT = [A.tile([128, 8, 512], BF16, 'uT') for _ in range(2)]
        r_uT = [R(), R()]
        ps_tr = [nc.alloc_psum_tensor('ps_tr%d' % i, [128, 1024], BF16) for i in range(2)]
        r_pstr = [Res('pstr0', True), Res('pstr1', True)]
        NPS = 3
        ps_mm = [nc.alloc_psum_tensor('ps_mm%d' % i, [128, 512], F32) for i in range(NPS)]
        r_psmm = [Res('psmm%d' % i, True) for i in range(NPS)]
        NEV = 6
        ev_f = [A.tile([128, 512], F32, 'ev_f') for _ in range(NEV)]
        ev_b = [A.tile([128, 512], BF16, 'ev_b') for _ in range(NEV)]
        ev_s = [A.tile([128, 512], F32, 'ev_s') for _ in range(NEV)]
        r_evf = [R() for _ in range(NEV)]
        r_evb = [R() for _ in range(NEV)]
        r_evs = [R() for _ in range(NEV)]
        r_dram = R('p2dram')
        cnt = {'ps': 0, 'ev': 0, 'q': 0}

        def next_ps():
            i = cnt['ps'] % NPS
            cnt['ps'] += 1
            return i

        def next_ev():
            i = cnt['ev'] % NEV
            cnt['ev'] += 1
            return i

        def dq():
            return 'sp'

        sbs = [(i, min(i + 4, NT)) for i in range(0, NT, 4)]
        if FASTSIM:
            sbs = sbs[:1]
        def p2_norm(sbi):
            t0, t1 = sbs[sbi]
            nt_sb = t1 - t0
            ntok = nt_sb * 128
            ub = sbi % 2
            for ti in range(t0, t1):
                xb = ti % NXB
                sb2 = ti % 2
                s_col = 1 if ti < 2 else 0
                lt = ti - t0
                S.op('sp', (lambda xb, ti: lambda e: e.dma_start(out=xt[xb][:], in_=xc[ti * 128:(ti + 1) * 128, :]))(xb, ti),
                     writes=[r_xt[xb]], chan='xt%d' % xb)
                S.op('act', (lambda xb, sb2: lambda e: e.activation(out=junk[:], in_=xt[xb][:], func=AF.Square,
                                                                   accum_out=ss[sb2][:, 0:1]))(xb, sb2),
                     reads=[r_xt[xb]], writes=[r_junk, r_ss[sb2]])
                S.op('act', (lambda sb2: lambda e: e.activation(out=ss[sb2][:, 1:2], in_=ss[sb2][:, 0:1], func=AF.Sqrt,
                                                               scale=1.0 / D, bias=epst[:, 0:1]))(sb2),
                     reads=[r_ss[sb2], r_epst], writes=[r_ss[sb2]])
                S.op('dve', (lambda sb2: lambda e: e.reciprocal(out=ss[sb2][:, 1:2], in_=ss[sb2][:, 1:2]))(sb2),
                     reads=[r_ss[sb2]], writes=[r_ss[sb2]])
                S.op('act', (lambda xb, sb2: lambda e: e.activation(out=xs[sb2][:], in_=xt[xb][:], func=AF.Copy,
                                                                   scale=ss[sb2][:, 1:2]))(xb, sb2),
                     reads=[r_xt[xb], r_ss[sb2]], writes=[r_xs[sb2]])
                for k in range(8):
                    S.op('pe', (lambda sb2, k: lambda e: e.transpose(out=ps_tr[sb2][:, k * 128:(k + 1) * 128],
                                                                    in_=xs[sb2][:, k * 128:(k + 1) * 128], identity=identb[:]))(sb2, k),
                         reads=[r_xs[sb2], r_identb], writes=[r_pstr[sb2]])
                S.op('dve', (lambda sb2, ub, lt, s_col: lambda e: e.tensor_tensor(
                    out=uT[ub][:, :, lt * 128:(lt + 1) * 128], in0=ps_tr[sb2][:, :].rearrange("p (k t) -> p k t", k=8),
                    in1=scale1[:, :, s_col:s_col + 1].to_broadcast([128, 8, 128]), op=ALU.mult))(sb2, ub, lt, s_col),
                     reads=[r_pstr[sb2], r_scale1], writes=[r_uT[ub]])
                S.op('pool', (lambda ub, lt, s_col: lambda e: e.tensor_tensor(
                    out=uT[ub][:, :, lt * 128:(lt + 1) * 128], in0=uT[ub][:, :, lt * 128:(lt + 1) * 128],
                    in1=MODT[:, 0, :, s_col:s_col + 1].to_broadcast([128, 8, 128]), op=ALU.add))(ub, lt, s_col),
                     reads=[r_uT[ub], r_MODT], writes=[r_uT[ub]])

        def p2_mm(sbi):
            t0, t1 = sbs[sbi]
            nt_sb = t1 - t0
            ntok = nt_sb * 128
            ub = sbi % 2
            fm = []
            for h in range(4):
                fm.append((h * 128, QTH[h * 128:(h + 1) * 128, :], 'silu', 128))
            for g in range(8):
                fm.append((2560 + g * 128, PRE[g * 128:(g + 1) * 128, :], 'copy', 128))
            fm.append((4608, GATES[:, :], 'gate', 16))
            for (c0, dst, kind, M) in fm:
                pi = next_ps()
                for k in range(8):
                    S.op('pe', (lambda pi, k, c0, M, ub, ntok: lambda e: e.matmul(
                        ps_mm[pi][0:M, 0:ntok], lhsT=WIN[:, k, c0:c0 + M], rhs=uT[ub][:, k, 0:ntok],
                        start=(k == 0), stop=(k == 7)))(pi, k, c0, M, ub, ntok),
                         reads=[r_WIN, r_uT[ub]], writes=[r_psmm[pi]])
                ei = next_ev()
                if kind == 'gate':
                    S.op('act', (lambda pi, ei, ntok: lambda e: e.activation(out=ev_f[ei][0:16, 0:ntok], in_=ps_mm[pi][0:16, 0:ntok],
                                                                            func=AF.Identity, bias=gbt[:, 0:1], scale=1.0))(pi, ei, ntok),
                         reads=[r_psmm[pi], r_gbt], writes=[r_evf[ei]])
                    S.op(dq(), (lambda ei, ntok, t0, dst: lambda e: e.dma_start(out=dst[:, t0 * 128:t0 * 128 + ntok],
                                                                               in_=ev_f[ei][0:16, 0:ntok]))(ei, ntok, t0, dst),
                         reads=[r_evf[ei]], writes=[r_dram], chan='evf%d' % ei)
                else:
                    fn = AF.Silu if kind == 'silu' else AF.Copy
                    cnt['cp'] = cnt.get('cp', 0) + 1
                    if kind == 'copy' and cnt['cp'] % 2:
                        S.op('dve', (lambda pi, ei, ntok: lambda e: e.tensor_copy(out=ev_b[ei][:, 0:ntok], in_=ps_mm[pi][:, 0:ntok]))(pi, ei, ntok),
                             reads=[r_psmm[pi]], writes=[r_evb[ei]])
                    else:
                        S.op('act', (lambda pi, ei, ntok, fn: lambda e: e.activation(out=ev_b[ei][:, 0:ntok], in_=ps_mm[pi][:, 0:ntok],
                                                                                    func=fn))(pi, ei, ntok, fn),
                             reads=[r_psmm[pi]], writes=[r_evb[ei]])
                    S.op(dq(), (lambda ei, ntok, t0, dst: lambda e: e.dma_start(out=dst[:, t0 * 128:t0 * 128 + ntok],
                                                                               in_=ev_b[ei][:, 0:ntok]))(ei, ntok, t0, dst),
                         reads=[r_evb[ei]], writes=[r_dram], chan='evb%d' % ei)

            tmb = [(512, HGV, 'copy'), (3584, MLV, 'copy'), (1536, 0, 'ff'), (2048, 1, 'ff'),
                   (1024, HGG, 'silu'), (4096, MLO, 'sigm'), (4608, GATEST, 'gatet')]
            for lt in range(nt_sb):
                ti = t0 + lt
                for (c0, dst, kind) in tmb:
                    if kind in ('silu', 'sigm') and ti < 2:
                        continue
                    pi = next_ps()
                    ncol = 16 if kind == 'gatet' else 512
                    for k in range(8):
                        S.op('pe', (lambda pi, k, c0, ub, lt, ncol: lambda e: e.matmul(
                            ps_mm[pi][:, 0:ncol], lhsT=uT[ub][:, k, lt * 128:(lt + 1) * 128], rhs=WIN[:, k, c0:c0 + ncol],
                            start=(k == 0), stop=(k == 7)))(pi, k, c0, ub, lt, ncol),
                             reads=[r_WIN, r_uT[ub]], writes=[r_psmm[pi]])
                    ei = next_ev()
                    rows = slice(ti * 128, (ti + 1) * 128)
                    if kind == 'gatet':
                        S.op('dve', (lambda pi, ei: lambda e: e.tensor_tensor(out=ev_f[ei][:, 0:16], in0=ps_mm[pi][:, 0:16], in1=gbrow[:, :],
                                                                             op=ALU.add))(pi, ei),
                             reads=[r_psmm[pi], r_gbt], writes=[r_evf[ei]])
                        S.op(dq(), (lambda ei, dst, rows: lambda e: e.dma_start(out=dst[rows, :], in_=ev_f[ei][:, 0:16]))(ei, dst, rows),
                             reads=[r_evf[ei]], writes=[r_dram], chan='evf%d' % ei)
                        continue
                    if kind == 'ff':
                        d = dst
                        S.op('act', (lambda pi, ei: lambda e: e.activation(out=ev_s[ei][:], in_=ps_mm[pi][:, :], func=AF.Sigmoid))(pi, ei),
                             reads=[r_psmm[pi]], writes=[r_evs[ei]])
                        S.op('dve', (lambda ei, d: lambda e: e.tensor_tensor(out=ev_s[ei][:], in0=ev_s[ei][:], in1=omlb[:, d, :],
                                                                            op=ALU.mult))(ei, d),
                             reads=[r_evs[ei], r_lbr], writes=[r_evs[ei]])
                        S.op('pool', (lambda ei, d: lambda e: e.tensor_tensor(out=ev_s[ei][:], in0=ev_s[ei][:], in1=lbr[:, d, :],
                                                                             op=ALU.add))(ei, d),
                             reads=[r_evs[ei], r_lbr], writes=[r_evs[ei]])
                        S.op('act', (lambda ei: lambda e: e.activation(out=ev_f[ei][:], in_=ev_s[ei][:], func=AF.Ln))(ei),
                             reads=[r_evs[ei]], writes=[r_evf[ei]])
                        S.op('pool', (lambda ei: lambda e: e.tensor_scalar(out=ev_b[ei][:], in0=ev_s[ei][:], scalar1=-1.0, scalar2=1.0,
                                                                          op0=ALU.mult, op1=ALU.add))(ei),
                             reads=[r_evs[ei]], writes=[r_evb[ei]])
                        S.op(dq(), (lambda ei, d, rows: lambda e: e.dma_start(out=GG[d][rows, :], in_=ev_f[ei][:]))(ei, d, rows),
                             reads=[r_evf[ei]], writes=[r_dram], chan='evf%d' % ei)
                        S.op(dq(), (lambda ei, d, rows: lambda e: e.dma_start(out=KK[d][rows, :], in_=ev_b[ei][:]))(ei, d, rows),
                             reads=[r_evb[ei]], writes=[r_dram], chan='evb%d' % ei)
                    else:
                        fn = {'copy': AF.Copy, 'silu': AF.Silu, 'sigm': AF.Sigmoid}[kind]
                        if kind == 'copy':
                            S.op('dve', (lambda pi, ei: lambda e: e.tensor_copy(out=ev_b[ei][:], in_=ps_mm[pi][:, :]))(pi, ei),
                                 reads=[r_psmm[pi]], writes=[r_evb[ei]])
                        else:
                            S.op('act', (lambda pi, ei, fn: lambda e: e.activation(out=ev_b[ei][:], in_=ps_mm[pi][:, :], func=fn))(pi, ei, fn),
                                 reads=[r_psmm[pi]], writes=[r_evb[ei]])
                        S.op(dq(), (lambda ei, dst, rows: lambda e: e.dma_start(out=dst[rows, :], in_=ev_b[ei][:]))(ei, dst, rows),
                             reads=[r_evb[ei]], writes=[r_dram], chan='evb%d' % ei)
        p2_norm(0)
        for sbi in range(len(sbs)):
            if sbi + 1 < len(sbs):
                p2_norm(sbi + 1)
            p2_mm(sbi)
        S.barrier()
        A.reset(m2)

    ps_x = nc.alloc_psum_tensor('ps_x', [128, 512], F32)
    r_psx = Res('psx', True)
    r_psx2 = r_psx
    if stage >= 3:
        m3 = A.mark()
        cwt = A.tile([128, 8, 9], F32, 'cwt')
        cbt = A.tile([128, 8], F32, 'cbt')
        r_cw = R()
        S.op('sp', lambda e: e.dma_start(out=cwt[:], in_=convw), writes=[r_cw], chan='c_cw')
        S.op('sp', lambda e: e.dma_start(out=cbt[:], in_=convb), writes=[r_cw], chan='c_cb')
        pre = [A.tile([128, T], BF16, 'pre') for _ in range(2)]
        acc = [A.tile([128, T], F32, 'acc') for _ in range(2)]
        post = [A.tile([128, T], BF16, 'post') for _ in range(2)]
        r_pre, r_acc, r_post = [R(), R()], [R(), R()], [R(), R()]
        r_qkt = R('qkt')
        for g in range(1 if FASTSIM else 8):
            b = g % 2
            ce = 'dve'
            S.op('sp', (lambda b, g: lambda e: e.dma_start(out=pre[b][:], in_=PRE[g * 128:(g + 1) * 128, :]))(b, g),
                 reads=[r_dram], writes=[r_pre[b]], chan='pre%d' % b)
            S.op(ce, (lambda b, g: lambda e: e.tensor_scalar(out=acc[b][:], in0=pre[b][:], scalar1=cwt[:, g, 4:5], scalar2=None,
                                                             op0=ALU.mult))(b, g),
                 reads=[r_pre[b], r_cw], writes=[r_acc[b]])
            for dx in (0, 2):
                ox = dx - 1
                d0, d1 = max(0, -ox), CTX - max(0, ox)
                S.op(ce, (lambda b, g, dx, ox, d0, d1: lambda e: e.scalar_tensor_tensor(
                    out=acc[b][:, d0:d1], in0=pre[b][:, d0 + ox:d1 + ox], scalar=cwt[:, g, 3 + dx:4 + dx],
                    in1=acc[b][:, d0:d1], op0=ALU.mult, op1=ALU.add))(b, g, dx, ox, d0, d1),
                     reads=[r_pre[b], r_cw], writes=[r_acc[b]])
            for dy in range(3):
                for dx in range(3):
                    if dy == 1 and dx == 1:
                        continue
                    oy, ox = dy - 1, dx - 1
                    r0, r1 = max(0, -oy), 128 - max(0, oy)
                    c0, c1 = max(0, -ox), 64 - max(0, ox)

                    def mk(b, g, dy, dx, oy, ox, r0, r1, c0, c1):
                        def f(e):
                            av = acc[b][:, CTX:].rearrange("p (r c) -> p r c", c=64)
                            pv = pre[b][:, CTX:].rearrange("p (r c) -> p r c", c=64)
                            return e.scalar_tensor_tensor(out=av[:, r0:r1, c0:c1], in0=pv[:, r0 + oy:r1 + oy, c0 + ox:c1 + ox],
                                                          scalar=cwt[:, g, dy * 3 + dx:dy * 3 + dx + 1], in1=av[:, r0:r1, c0:c1],
                                                          op0=ALU.mult, op1=ALU.add)
                        return f
                    S.op(ce, mk(b, g, dy, dx, oy, ox, r0, r1, c0, c1), reads=[r_pre[b], r_cw], writes=[r_acc[b]])
            S.op('act', (lambda b, g: lambda e: e.activation(out=post[b][:], in_=acc[b][:], func=AF.Silu, bias=cbt[:, g:g + 1]))(b, g),
                 reads=[r_acc[b], r_cw], writes=[r_post[b]])
            S.op('sp', (lambda b, g: lambda e: e.dma_start(out=QKT[g * 128:(g + 1) * 128, :], in_=post[b][:]))(b, g),
                 reads=[r_post[b]], writes=[r_qkt], chan='post%d' % b)
        S.barrier()
        A.reset(m3)

    if stage >= 4:
        cmask = A.tile([128, 2, 128], F32, 'cmask')
        ctrix = A.tile([128, 2, 132], F32, 'ctrix')
        cselh = A.tile([4, 4, 128], F32, 'cselh')
        r_cm = R()
        S.op('sp', lambda e: e.dma_start(out=cmask[:], in_=maskin), writes=[r_cm], chan='c_m1')
        S.op('sp', lambda e: e.dma_start(out=ctrix[:], in_=trixin), writes=[r_cm], chan='c_m2')
        S.op('sp', lambda e: e.dma_start(out=cselh[:], in_=selhin), writes=[r_cm], chan='c_m3')
        TOK = [A.tile([128, NT, 8], F32, 'TOK') for _ in range(2)]
        WSB = [A.tile([128, 4, NT], F32, 'WSB') for _ in range(2)]
        r_TOK = [R(), R()]
        m4 = A.mark()
        X = [A.tile([4, T], F32, 'X%d' % i) for i in range(4)]
        AE = A.tile([4, NT], F32, 'AE')
        WS = A.tile([4, NT], F32, 'WS')
        cln = A.tile([4, 2], F32, 'cln')
        r_rows = R()
        r_zr = R()
        S.op('dve', lambda e: e.memset(cln[:, 0:1], 0.5 * float(np.log(128.0))), writes=[r_zr])
        S.op('dve', lambda e: e.memset(cln[:, 1:2], 0.0), writes=[r_zr])
        ps_g = ps_mm[0]
        r_psg = r_psmm[0]

        def rop(eng, fn):
            S.op(eng, fn, reads=[r_rows, r_zr], writes=[r_rows])

        GT = A.tile([128, NT, 16], F32, 'GT')
        TOKr = A.tile([128, NT, 8], F32, 'TOKr')
        r_GT = R()
        r_TOKr = R()
        S.op('sp', lambda e: e.dma_start(out=GT[:], in_=GATEST.rearrange("(t p) g -> p t g", p=128)), reads=[r_dram], writes=[r_GT], chan='c_gt')
        order1 = [1, 0] + list(range(NT - 1, 1, -1))
        ps_a, r_psa = ps_mm[1], r_psmm[1]
        ps_b, r_psb = ps_mm[2], r_psmm[2]

        for p in range(2):
            if p == 0:
                S.op('sp', lambda e: e.dma_start(out=X[0][:], in_=GATES[0:4, :]), reads=[r_dram], writes=[r_rows], chan='c_li')
                S.op('sp', lambda e: e.dma_start(out=X[1][:], in_=GATES[4:8, :]), reads=[r_dram], writes=[r_rows], chan='c_fz')
                LIb, LFb, Bb, Mb = X[0], X[1], X[2], X[3]
            else:
                LIb, LFb, Bb, Mb = X[2], X[3], X[0], X[1]
                for c0 in range(0, NT, 4):
                    c1 = min(c0 + 4, NT)
                    for c in range(c0, c1):
                        ti = order1[c]
                        j = (c - c0) * 128
                        S.op('pe', (lambda ti, j: lambda e: e.matmul(ps_a[0:4, j:j + 128], lhsT=GT[:, ti, 8:12], rhs=Jm[:, :], start=True, stop=True))(ti, j),
                             reads=[r_GT, r_gbt], writes=[r_psa])
                        S.op('pe', (lambda ti, j: lambda e: e.matmul(ps_b[0:4, j:j + 128], lhsT=GT[:, ti, 12:16], rhs=Jm[:, :], start=True, stop=True))(ti, j),
                             reads=[r_GT, r_gbt], writes=[r_psb])
                    n = (c1 - c0) * 128
                    S.op('dve', (lambda c0, n, LIb: lambda e: e.tensor_copy(out=LIb[:, c0 * 128:c0 * 128 + n], in_=ps_a[0:4, 0:n]))(c0, n, LIb),
                         reads=[r_psa, r_rows], writes=[r_rows])
                    S.op('act', (lambda c0, n, LFb: lambda e: e.activation(out=LFb[:, c0 * 128:c0 * 128 + n], in_=ps_b[0:4, 0:n], func=AF.Copy))(c0, n, LFb),
                         reads=[r_psb, r_rows], writes=[r_rows])
            rop('act', (lambda LFb: lambda e: e.activation(out=LFb[:], in_=LFb[:], func=AF.Sigmoid))(LFb))
            rop('act', (lambda LFb: lambda e: e.activation(out=LFb[:], in_=LFb[:], func=AF.Ln))(LFb))

            def body(LIb=LIb, LFb=LFb, Bb=Bb, Mb=Mb):
                rop('dve', lambda e: e.tensor_tensor_scan(out=Bb[:], data0=LFb[:], data1=LFb[:], initial=0.0, op0=ALU.add, op1=ALU.min))
                rop('dve', lambda e: e.tensor_tensor_scan(out=Mb[:], data0=LFb[:], data1=LIb[:], initial=0.0, op0=ALU.add, op1=ALU.max))
                rop('dve', lambda e: e.tensor_tensor(out=Bb[:], in0=Bb[:], in1=Mb[:], op=ALU.subtract))
                rop('dve', lambda e: e.memset(AE[:, 0:1], 0.0))
                rop('dve', lambda e: e.tensor_copy(out=AE[:, 1:NT], in_=Bb[:, 127:T - 1:128]))
                rop('dve', lambda e: e.tensor_tensor(out=Bb[:].rearrange("p (c t) -> p c t", t=128),
                                                     in0=Bb[:].rearrange("p (c t) -> p c t", t=128),
                                                     in1=AE[:].unsqueeze(2).to_broadcast([4, NT, 128]), op=ALU.subtract))
                rop('act', lambda e: e.activation(out=WS[:], in_=Bb[:, 127:T:128], func=AF.Exp))
                rop('dve', lambda e: e.tensor_tensor(out=Mb[:], in0=Mb[:], in1=Bb[:], op=ALU.add))
                rop('dve', lambda e: e.tensor_tensor(out=LIb[:], in0=LIb[:], in1=Mb[:], op=ALU.subtract))
                rop('act', lambda e: e.activation(out=LIb[:], in_=LIb[:], func=AF.Exp))
                rop('act', lambda e: e.activation(out=Mb[:], in_=Mb[:], func=AF.Exp, scale=-1.0, bias=cln[:, 0:1]))
            body()
            SKn, EMn = LIb, Mb
            dstT = TOK[0] if p == 0 else TOKr
            r_dstT = r_TOK[0] if p == 0 else r_TOKr
            for c0 in range(0, NT, 64):
                c1 = min(NT, c0 + 64)
                for c in range(c0, c1):
                    j = (c - c0) * 8
                    S.op('pe', (lambda c, j, SKn: lambda e: e.matmul(ps_g[:, j:j + 4], lhsT=SKn[0:4, c * 128:(c + 1) * 128],
                                                                    rhs=ident[0:4, 0:4], start=True, stop=True))(c, j, SKn),
                         reads=[r_rows, r_ident], writes=[r_psg])
                    S.op('pe', (lambda c, j, EMn: lambda e: e.matmul(ps_g[:, j + 4:j + 8], lhsT=EMn[0:4, c * 128:(c + 1) * 128],
                                                                    rhs=ident[0:4, 0:4], start=True, stop=True))(c, j, EMn),
                         reads=[r_rows, r_ident], writes=[r_psg])
                if p == 0:
                    S.op('dve', (lambda c0, c1: lambda e: e.tensor_copy(out=TOK[0][:, c0:c1, :].rearrange("p a b -> p (a b)"),
                                                                       in_=ps_g[:, 0:(c1 - c0) * 8]))(c0, c1),
                         reads=[r_psg], writes=[r_TOK[0]])
                else:
                    for c in range(c0, c1):
                        ti = order1[c]
                        j = (c - c0) * 8
                        S.op('dve' if c % 2 else 'act',
                             (lambda ti, j: (lambda e: e.tensor_copy(out=TOKr[:, ti, :], in_=ps_g[:, j:j + 8])))(ti, j) if c % 2 else
                             (lambda ti, j: (lambda e: e.activation(out=TOKr[:, ti, :], in_=ps_g[:, j:j + 8], func=AF.Copy)))(ti, j),
                             reads=[r_psg], writes=[r_TOKr])
            if p == 1:
                TRf = TOKr[:].rearrange("p a b -> p (a b)")
                TKf = TOK[1][:].rearrange("p a b -> p (a b)")
                half = NT * 4
                for hh in range(2):
                    S.op('pe', (lambda hh: lambda e: e.matmul(ps_g[:, 0:half], lhsT=Jm[:, :], rhs=TRf[:, hh * half:(hh + 1) * half], start=True, stop=True))(hh),
                         reads=[r_TOKr, r_gbt], writes=[r_psg])
                    S.op('dve', (lambda hh: lambda e: e.tensor_copy(out=TKf[:, hh * half:(hh + 1) * half], in_=ps_g[:, 0:half]))(hh),
                         reads=[r_psg], writes=[r_TOK[1]])
            for h in range(4):
                S.op('pe', (lambda h: lambda e: e.matmul(ps_g[:, h * NT:(h + 1) * NT], lhsT=cselh[0:4, h, :], rhs=WS[0:4, :],
                                                        start=True, stop=True))(h),
                     reads=[r_rows, r_cm], writes=[r_psg])
            S.op('dve', (lambda p: lambda e: e.tensor_copy(out=WSB[p][:].rearrange("p a b -> p (a b)"), in_=ps_g[:, 0:4 * NT]))(p),
                 reads=[r_psg], writes=[r_TOK[p]])
        if "TOKd" in dbg:
            S.op('sp', lambda e: e.dma_start(out=TOKd[:, 0:NT * 8], in_=TOK[1][:].rearrange("p a b -> p (a b)")), reads=[r_TOK[1]], chan='c_tokd')
            S.op('sp', lambda e: e.dma_start(out=TOKd[:, NT * 8:NT * 12], in_=WSB[1][:].rearrange("p a b -> p (a b)")), reads=[r_TOK[1]], chan='c_tokd')
        S.barrier()
        A.reset(m4)

    if stage >= 5:
        m5 = A.mark()
        hgb = [ps_mm[0], ps_mm[1]]
        hgo = [ps_mod[0], ps_mod[1]]
        mlb = [ps_mm[2], ps_x]
        r_pA = [r_psmm[0], r_psmm[1]]; r_pB = r_pA; r_pS = r_pA
        r_pO = [r_psmod[0], r_psmod[1]]; r_pU = r_pO
        r_pT = [r_pstr[0], r_pstr[1]]; r_mT = r_pT
        r_mS = [r_psmm[2], r_psx]; r_mO = r_mS; r_mU = r_mS
        def two(shape, dt, nm):
            return [A.tile(shape, dt, nm) for _ in range(2)], [R(), R()]
        EQ, r_EQ = two([128, 128], F32, 'EQ')
        QtT, r_QtT = two([128, 128], BF16, 'QtT')
        EK, r_EK = two([128, 128], F32, 'EK')
        Kt, r_Kt = two([128, 128], BF16, 'Kt')
        KtT, r_KtT = two([128, 128], BF16, 'KtT')
        ET, r_ET = two([128, 4], F32, 'ET')
        Spb, r_Spb = two([128, 128], BF16, 'Spb')
        ATh, r_ATh = two([128, 128], BF16, 'ATh')
        ATm, r_ATm = two([128, 128], BF16, 'ATm')
        Kh, r_Kh = two([128, 128], BF16, 'Kh')
        Cb, r_Cb = two([128, 132], BF16, 'Cb')
        dn, r_dn = two([128, 2], F32, 'dn')
        obuf, r_obuf = two([128, 512], F32, 'obuf')
        hbuf, r_hbuf = two([128, 512], F32, 'hbuf')
        o0, r_o0 = two([128, 512], F32, 'o0')
        h0, r_h0 = two([128, 512], F32, 'h0')
        gt, r_gt = two([128, 512], F32, 'gt')
        kt, r_kt = two([128, 512], BF16, 'kt')
        hv, r_hv = two([128, 512], BF16, 'hv')
        vb, r_vb = two([128, 4, 132], BF16, 'vb')
        qth, r_qth = two([128, 4, 128], BF16, 'qth')
        qtm, r_qtm = two([128, 4, 128], BF16, 'qtm')
        ktm, r_ktm = two([128, 4, 128], BF16, 'ktm')
        Sst = [A.tile([128, 128], F32, 'Sst') for _ in range(4)]
        r_S = [R() for _ in range(4)]
        Cst = [A.tile([128, 132], F32, 'Cst') for _ in range(4)]
        r_C = [R() for _ in range(4)]
        r_oh = [R(), R()]
        for b in range(2):
            S.op('pool', (lambda b: lambda e: e.memset(vb[b][:], 1.0))(b), writes=[r_vb[b]])
        QTHv = QTH.rearrange("(h d) t -> d h t", d=128)
        QTMv = QKT[0:512, :].rearrange("(h d) t -> d h t", d=128)
        KTMv = QKT[512:1024, :].rearrange("(h d) t -> d h t", d=128)
        MLVv = MLV.rearrange("t (h v) -> t h v", v=128)
        for p in range(2):
            for qq in range(2):
                S.op('pool', (lambda qq: lambda e: e.memset(ATh[qq][:], 0.0))(qq), writes=[r_ATh[qq]])
            for h in range(4):
                S.op('pool', (lambda h: lambda e: e.memset(Sst[h][:], 0.0))(h), writes=[r_S[h]])
                S.op('pool', (lambda h: lambda e: e.memset(Cst[h][:], 0.0))(h), writes=[r_C[h]])
            order = list(range(NT)) if p == 0 else [1, 0] + list(range(NT - 1, 1, -1))
            if FASTSIM:
                order = order[:4]
            MK = cmask[:, p, :]
            TX = ctrix[:, p, 0:132]
            TM = ctrix[:, p, 0:128]

            def loads(c, ti, gg_=GG[p], kk_=KK[p]):
                b = c % 2
                rows = slice(ti * 128, (ti + 1) * 128)
                cols = slice(ti * 128, (ti + 1) * 128)
                S.op('sp', lambda e: e.dma_start(out=gt[b][:], in_=gg_[rows, :]), reads=[r_dram], writes=[r_gt[b]], chan='l_gt%d' % b)
                S.op('sp', lambda e: e.dma_start(out=kt[b][:], in_=kk_[rows, :]), reads=[r_dram], writes=[r_kt[b]], chan='l_kt%d' % b)
                S.op('sp', lambda e: e.dma_start(out=hv[b][:], in_=HGV[rows, :]), reads=[r_dram], writes=[r_hv[b]], chan='l_hv%d' % b)
                S.op('sp', lambda e: e.dma_start(out=vb[b][:, :, 0:128], in_=MLVv[rows, :, :]), reads=[r_dram], writes=[r_vb[b]], chan='l_vb%d' % b)
                S.op('sp', lambda e: e.dma_start(out=ktm[b][:], in_=KTMv[:, :, cols]), reads=[r_qkt], writes=[r_ktm[b]], chan='l_ktm%d' % b)
                if ti >= 2:
                    S.op('sp', lambda e: e.dma_start(out=qth[b][:], in_=QTHv[:, :, cols]), reads=[r_dram], writes=[r_qth[b]], chan='l_qth%d' % b)
                    S.op('sp', lambda e: e.dma_start(out=qtm[b][:], in_=QTMv[:, :, cols]), reads=[r_qkt], writes=[r_qtm[b]], chan='l_qtm%d' % b)
                    if p == 1:
                        xr = slice((ti - 2) * 128, (ti - 1) * 128)
                        S.op('sp', lambda e: e.dma_start(out=o0[b][:], in_=OH[0][xr, :]), reads=[r_oh[0]], writes=[r_o0[b]], chan='l_o0%d' % b)
                        S.op('sp', lambda e: e.dma_start(out=h0[b][:], in_=OM[0][xr, :]), reads=[r_oh[0]], writes=[r_h0[b]], chan='l_h0%d' % b)

            def hg_step(c, ti, h, MK=MK, TX=TX, TM=TM, p=p):
                b = c % 2
                q = (c * 4 + h) % 2
                isx = ti >= 2
                hs = slice(h * 128, (h + 1) * 128)
                pA = hgb[q][:, 0:132]; pB = hgb[q][:, 132:260]; pS = hgb[q][:, 260:388]
                pO = hgo[q][:, 0:128]; pU = hgo[q][:, 128:256]; pT = ps_tr[q][:, 0:128]
                S.op('pe', lambda e: e.matmul(pA, lhsT=gt[b][:, hs], rhs=TX, start=True, stop=True), reads=[r_gt[b], r_cm], writes=[r_pA[q]])
                yield
                S.op('pe', lambda e: e.matmul(pB, lhsT=TM, rhs=gt[b][:, hs], start=True, stop=True), reads=[r_gt[b], r_cm], writes=[r_pB[q]])
                yield
                S.op('act', lambda e: e.activation(out=ET[q][:, 0:3], in_=hgb[q][:, 128:131], func=AF.Exp), reads=[r_pA[q]], writes=[r_ET[q]])
                yield
                if isx:
                    S.op('act', lambda e: e.activation(out=EQ[q][:], in_=hgb[q][:, 0:128], func=AF.Exp), reads=[r_pA[q]], writes=[r_EQ[q]])
                    yield
                    S.op('dve', lambda e: e.tensor_tensor(out=QtT[q][:], in0=EQ[q][:], in1=qth[b][:, h, :], op=ALU.mult),
                         reads=[r_EQ[q], r_qth[b]], writes=[r_QtT[q]])
                    yield
                S.op('act', lambda e: e.activation(out=EK[q][:], in_=pB, func=AF.Exp, scale=-1.0), reads=[r_pB[q]], writes=[r_EK[q]])
                yield
                S.op('dve', lambda e: e.tensor_tensor(out=Kt[q][:], in0=EK[q][:], in1=kt[b][:, hs], op=ALU.mult),
                     reads=[r_EK[q], r_kt[b]], writes=[r_Kt[q]])
                yield
                if isx:
                    S.op('pe', lambda e: e.transpose(out=pT, in_=Kt[q][:], identity=identb[:]), reads=[r_Kt[q], r_identb], writes=[r_pT[q]])
                    yield
                    S.op('act', lambda e: e.activation(out=KtT[q][:], in_=pT, func=AF.Copy), reads=[r_pT[q]], writes=[r_KtT[q]])
                    yield
                    S.op('act', lambda e: e.activation(out=Spb[q][:], in_=Sst[h][:], func=AF.Copy, scale=ET[q][:, 1:2]),
                         reads=[r_S[h], r_ET[q]], writes=[r_Spb[q]])
                    yield
                    fr = slice(0, 64) if p == 0 else slice(64, 128)
                    hr = slice(64, 128) if p == 0 else slice(0, 64)
                    S.op('pe', lambda e: e.matmul(hgb[q][fr, 260:388], lhsT=KtT[q][:, fr], rhs=QtT[q][:], start=True, stop=True),
                         reads=[r_KtT[q], r_QtT[q]], writes=[r_pS[q]])
                    yield
                    S.op('pe', lambda e: e.matmul(hgb[q][hr, 260 + hr.start:260 + hr.stop], lhsT=KtT[q][:, hr], rhs=QtT[q][:, hr], start=True, stop=True),
                         reads=[r_KtT[q], r_QtT[q]], writes=[r_pS[q]])
                    yield
                    S.op('dve', lambda e: e.tensor_tensor(out=ATh[q][fr, :], in0=hgb[q][fr, 260:388], in1=cmask[fr, p, :], op=ALU.mult),
                         reads=[r_pS[q], r_cm], writes=[r_ATh[q]])
                    yield
                    S.op('dve', lambda e: e.tensor_tensor(out=ATh[q][hr, hr], in0=hgb[q][hr, 260 + hr.start:260 + hr.stop], in1=cmask[hr, p, hr], op=ALU.mult),
                         reads=[r_pS[q], r_cm], writes=[r_ATh[q]])
                    yield
                    S.op('pe', lambda e: e.matmul(pO, lhsT=ATh[q][:], rhs=hv[b][:, hs], start=True, stop=False),
                         reads=[r_ATh[q], r_hv[b]], writes=[r_pO[q]])
                    yield
                    S.op('pe', lambda e: e.matmul(pO, lhsT=QtT[q][:], rhs=Spb[q][:], start=False, stop=True),
                         reads=[r_QtT[q], r_Spb[q]], writes=[r_pO[q]])
                    yield
                S.op('pe', lambda e: e.matmul(pU, lhsT=Kt[q][:], rhs=hv[b][:, hs], start=True, stop=True),
                     reads=[r_Kt[q], r_hv[b]], writes=[r_pU[q]])
                yield
                S.op('act', lambda e: e.activation(out=Sst[h][:], in_=Sst[h][:], func=AF.Copy, scale=ET[q][:, 2:3]),
                     reads=[r_S[h], r_ET[q]], writes=[r_S[h]])
                yield
                S.op('dve', lambda e: e.scalar_tensor_tensor(out=Sst[h][:], in0=pU, scalar=ET[q][:, 0:1], in1=Sst[h][:],
                                                             op0=ALU.mult, op1=ALU.add),
                     reads=[r_pU[q], r_ET[q], r_S[h]], writes=[r_S[h]])
                yield
                if isx:
                    if p == 0:
                        S.op('act', lambda e: e.activation(out=obuf[b][:, hs], in_=pO, func=AF.Copy), reads=[r_pO[q]], writes=[r_obuf[b]])
                        yield
                    else:
                        S.op('dve', lambda e: e.tensor_tensor(out=obuf[b][:, hs], in0=pO, in1=o0[b][:, hs], op=ALU.add),
                             reads=[r_pO[q], r_o0[b]], writes=[r_obuf[b]])
                        yield

            def ml_step(c, ti, h, MK=MK, p=p):
                b = c % 2
                q = (c * 4 + h) % 2
                isx = ti >= 2
                hs = slice(h * 128, (h + 1) * 128)
                pS = mlb[q][:, 0:128]; pO = mlb[q][:, 128:258]; pU = mlb[q][:, 260:390]; pT = ps_tr[q][:, 128:256]
                sk = TOK[p][:, ti, h:h + 1]
                em = TOK[p][:, ti, 4 + h:5 + h]
                cidx = c
                ws = WSB[p][:, h, cidx:cidx + 1]
                if isx:
                    S.op('pe', lambda e: e.matmul(pS, lhsT=ktm[b][:, h, :], rhs=qtm[b][:, h, :], start=True, stop=True),
                         reads=[r_ktm[b], r_qtm[b]], writes=[r_mS[q]])
                    yield
                    S.op('dve', lambda e: e.scalar_tensor_tensor(out=ATm[q][:], in0=pS, scalar=sk, in1=MK, op0=ALU.mult, op1=ALU.mult),
                         reads=[r_mS[q], r_TOK[p], r_cm], writes=[r_ATm[q]])
                    yield
                S.op('pe', lambda e: e.transpose(out=pT, in_=ktm[b][:, h, :], identity=identb[:]), reads=[r_ktm[b], r_identb], writes=[r_mT[q]])
                yield
                S.op('act', lambda e: e.activation(out=Kh[q][:], in_=pT, func=AF.Copy, scale=sk), reads=[r_mT[q], r_TOK[p]], writes=[r_Kh[q]])
                yield
                if isx:
                    S.op('pool', lambda e: e.tensor_copy(out=Cb[q][:, 0:130], in_=Cst[h][:, 0:130]), reads=[r_C[h]], writes=[r_Cb[q]])
                    yield
                    S.op('pe', lambda e: e.matmul(pO, lhsT=ATm[q][:], rhs=vb[b][:, h, 0:130], start=True, stop=False),
                         reads=[r_ATm[q], r_vb[b]], writes=[r_mO[q]])
                    yield
                    S.op('pe', lambda e: e.matmul(pO, lhsT=qtm[b][:, h, :], rhs=Cb[q][:, 0:130], start=False, stop=True),
                         reads=[r_qtm[b], r_Cb[q]], writes=[r_mO[q]])
                    yield
                S.op('pe', lambda e: e.matmul(pU, lhsT=Kh[q][:], rhs=vb[b][:, h, 0:130], start=True, stop=True),
                     reads=[r_Kh[q], r_vb[b]], writes=[r_mU[q]])
                yield
                S.op('act', lambda e: e.activation(out=Cst[h][:, 0:130], in_=Cst[h][:, 0:130], func=AF.Copy, scale=ws),
                     reads=[r_C[h], r_TOK[p]], writes=[r_C[h]])
                yield
                S.op('dve', lambda e: e.scalar_tensor_tensor(out=Cst[h][:, 0:130], in0=pU, scalar=ws, in1=Cst[h][:, 0:130],
                                                             op0=ALU.mult, op1=ALU.add),
                     reads=[r_mU[q], r_TOK[p], r_C[h]], writes=[r_C[h]])
                yield
                if isx:
                    S.op('act', lambda e: e.activation(out=dn[q][:, 0:1], in_=mlb[q][:, 256:257], func=AF.Abs),
                         reads=[r_mO[q]], writes=[r_dn[q]])
                    yield
                    S.op('dve', lambda e: e.tensor_tensor(out=dn[q][:, 0:1], in0=dn[q][:, 0:1], in1=em, op=ALU.max),
                         reads=[r_dn[q], r_TOK[p]], writes=[r_dn[q]])
                    yield
                    S.op('dve', lambda e: e.reciprocal(out=dn[q][:, 1:2], in_=dn[q][:, 0:1]), reads=[r_dn[q]], writes=[r_dn[q]])
                    yield
                    if p == 0:
                        S.op('act', lambda e: e.activation(out=hbuf[b][:, hs], in_=mlb[q][:, 128:256], func=AF.Copy, scale=dn[q][:, 1:2]),
                             reads=[r_mO[q], r_dn[q]], writes=[r_hbuf[b]])
                        yield
                    else:
                        S.op('dve', lambda e: e.scalar_tensor_tensor(out=hbuf[b][:, hs], in0=mlb[q][:, 128:256], scalar=dn[q][:, 1:2],
                                                                     in1=h0[b][:, hs], op0=ALU.mult, op1=ALU.add),
                             reads=[r_mO[q], r_dn[q], r_h0[b]], writes=[r_hbuf[b]])
                        yield

            for c in range(len(order) + 1):
                if c < len(order):
                    loads(c, order[c])
                if c > 0:
                    cc = c - 1
                    ti = order[cc]
                    for hbase in (0, 2):
                        gens = []
                        for h in (hbase, hbase + 1):
                            gens.append(hg_step(cc, ti, h))
                            gens.append(ml_step(cc, ti, h))
                        while gens:
                            for g in list(gens):
                                try:
                                    next(g)
                                except StopIteration:
                                    gens.remove(g)
                    if ti >= 2:
                        b = cc % 2
                        xr = slice((ti - 2) * 128, (ti - 1) * 128)
                        S.op('sp', (lambda b, xr, dst: lambda e: e.dma_start(out=dst[xr, :], in_=obuf[b][:]))(b, xr, OH[p]),
                             reads=[r_obuf[b]], writes=[r_oh[p]], chan='s_ob%d' % b)
                        S.op('sp', (lambda b, xr, dst: lambda e: e.dma_start(out=dst[xr, :], in_=hbuf[b][:]))(b, xr, OM[p]),
                             reads=[r_hbuf[b]], writes=[r_oh[p]], chan='s_hb%d' % b)
        S.barrier()
        A.reset(m5)

    if stage >= 6:
        m6 = A.mark()
        hgnw = A.tile([128, 512], F32, 'hgnw')
        mlnw = A.tile([128, 512], F32, 'mlnw')
        n2r = A.tile([128, D], F32, 'n2r')
        RW = A.tile([128, 8, 16], F32, 'RW')
        WOG = A.tile([128, 8, D], BF16, 'WOG')
        SC2R = A.tile([128, D], F32, 'SC2R')
        SH2R = A.tile([128, D], F32, 'SH2R')
        AFFT = A.tile([16, SEQ], F32, 'AFFT')
        r_c6 = R()
        r_AFFT = R()
        S.op('sp', lambda e: e.dma_start(out=hgnw[:], in_=hgnw_in), writes=[r_c6], chan='c6a')
        S.op('sp', lambda e: e.dma_start(out=mlnw[:], in_=mlnw_in), writes=[r_c6], chan='c6b')
        S.op('sp', lambda e: e.dma_start(out=n2r[:], in_=n2r_in), writes=[r_c6], chan='c6c')
        S.op('sp', lambda e: e.dma_start(out=RW[:], in_=rw_in.rearrange("(k p) e -> p k e", p=128)), writes=[r_c6], chan='c6d')
        m6b = A.mark()
        MOD2 = A.tile([2, 6 * D], F32, 'MOD2')
        wof = A.tile([128, 8, D], F32, 'wof')
        r_m2 = R()
        S.op('sp', lambda e: e.dma_start(out=MOD2[:], in_=MODd), reads=[r_modd], writes=[r_m2], chan='c6e')
        S.op('sp', lambda e: e.dma_start(out=wof[:], in_=wout_in.rearrange("(k p) c -> p k c", p=128)), writes=[r_m2], chan='c6f')
        G1R = A.tile([128, D], F32, 'G1R')
        pbs = [ps_mm[0], ps_mm[1]]
        rpb = [r_psmm[0], r_psmm[1]]
        for vi, (off, dst) in enumerate([(2048, G1R), (3072, SH2R), (4096, SC2R)]):
            for hb in range(2):
                q = (vi * 2 + hb) % 2
                S.op('pe', (lambda q, off, hb: lambda e: e.matmul(pbs[q][:, :], lhsT=sel2[0:2, :], rhs=MOD2[0:2, off + hb * 512:off + (hb + 1) * 512],
                                                                  start=True, stop=True))(q, off, hb),
                     reads=[r_sel2, r_m2], writes=[rpb[q]])
                if off == 4096:
                    S.op('dve', (lambda q, hb: lambda e: e.scalar_tensor_tensor(out=SC2R[:, hb * 512:(hb + 1) * 512], in0=pbs[q][:, :], scalar=1.0,
                                                                               in1=n2r[:, hb * 512:(hb + 1) * 512], op0=ALU.add, op1=ALU.mult))(q, hb),
                         reads=[rpb[q], r_c6], writes=[r_c6])
                else:
                    S.op('dve', (lambda q, hb, dst: lambda e: e.tensor_copy(out=dst[:, hb * 512:(hb + 1) * 512], in_=pbs[q][:, :]))(q, hb, dst),
                         reads=[rpb[q]], writes=[r_c6])
        for k in range(8):
            S.op('dve', (lambda k: lambda e: e.tensor_tensor(out=WOG[:, k, :], in0=wof[:, k, :], in1=G1R[:], op=ALU.mult))(k),
                 reads=[r_m2, r_c6], writes=[r_c6])
        S.barrier()
        A.reset(m6b)

        def two6(shape, dt, nm):
            return [A.tile(shape, dt, nm) for _ in range(2)], [R(), R()]
        oh_t, r_oh_t = two6([128, 512], F32, 'oh_t')
        om_t, r_om_t = two6([128, 512], F32, 'om_t')
        gg_t, r_gg_t = two6([128, 512], BF16, 'gg_t')
        mo_t, r_mo_t = two6([128, 512], BF16, 'mo_t')
        x_t, r_x_t = two6([128, D], F32, 'x_t')
        sq_t, r_sq_t = two6([128, 512], F32, 'sq_t')
        sq2_t, r_sq2_t = two6([128, 512], F32, 'sq2_t')
        st_t, r_st_t = two6([128, 16], F32, 'st_t')
        st2_t, r_st2_t = two6([128, 16], F32, 'st2_t')
        mix, r_mix = two6([128, D], BF16, 'mix')
        mixT, r_mixT = two6([128, D], BF16, 'mixT')
        xm, r_xm = two6([128, D], F32, 'xm')
        vx, r_vx = two6([128, D], F32, 'vx')
        vxb, r_vxb = two6([128, D], BF16, 'vxb')
        vxT, r_vxT = two6([128, D], F32, 'vxT')
        sm_t, r_sm_t = two6([128, 8], F32, 'sm_t')
        aff_t, r_aff_t = two6([128, 16], F32, 'aff_t')
        ex_t, r_ex_t = two6([128, 16], F32, 'ex_t')
        junk6 = A.tile([128, D], BF16, 'junk6')
        r_junk6 = R()
        r_xmid = R()
        r_vxd = R()
        r_affd = R()

        def p6_loads(i):
            b = i % 2
            xr = slice(i * 128, (i + 1) * 128)
            tr = slice((i + 2) * 128, (i + 3) * 128)
            S.op('sp', lambda e: e.dma_start(out=oh_t[b][:], in_=OH[1][xr, :]), reads=[r_oh[1]], writes=[r_oh_t[b]], chan='6oh%d' % b)
            S.op('sp', lambda e: e.dma_start(out=om_t[b][:], in_=OM[1][xr, :]), reads=[r_oh[1]], writes=[r_om_t[b]], chan='6om%d' % b)
            S.op('sp', lambda e: e.dma_start(out=gg_t[b][:], in_=HGG[tr, :]), reads=[r_dram], writes=[r_gg_t[b]], chan='6gg%d' % b)
            S.op('sp', lambda e: e.dma_start(out=mo_t[b][:], in_=MLO[tr, :]), reads=[r_dram], writes=[r_mo_t[b]], chan='6mo%d' % b)
            S.op('sp', lambda e: e.dma_start(out=x_t[b][:], in_=xc[tr, :]), writes=[r_x_t[b]], chan='6x%d' % b)

        def v4(t):
            return t.rearrange("p (h v) -> p h v", v=128)

        def p6_A(i):
            b = i % 2
            xr = slice(i * 128, (i + 1) * 128)
            S.op('dve', lambda e: e.tensor_tensor(out=sq_t[b][:], in0=oh_t[b][:], in1=oh_t[b][:], op=ALU.mult), reads=[r_oh_t[b]], writes=[r_sq_t[b]])
            S.op('dve', lambda e: e.tensor_reduce(out=st_t[b][:, 0:4], in_=v4(sq_t[b][:]), axis=AX.X, op=ALU.add), reads=[r_sq_t[b]], writes=[r_st_t[b]])
            S.op('act', lambda e: e.activation(out=st_t[b][:, 4:8], in_=st_t[b][:, 0:4], func=AF.Sqrt, scale=1.0 / 128, bias=epst[:, 0:1]),
                 reads=[r_st_t[b], r_epst], writes=[r_st_t[b]])
            S.op('dve', lambda e: e.reciprocal(out=st_t[b][:, 8:12], in_=st_t[b][:, 4:8]), reads=[r_st_t[b]], writes=[r_st_t[b]])
            S.op('dve', lambda e: e.tensor_tensor(out=v4(sq_t[b][:]), in0=v4(oh_t[b][:]), in1=st_t[b][:, 8:12].unsqueeze(2).to_broadcast([128, 4, 128]),
                                                  op=ALU.mult), reads=[r_oh_t[b], r_st_t[b]], writes=[r_sq_t[b]])
            S.op('dve', lambda e: e.tensor_tensor(out=sq_t[b][:], in0=sq_t[b][:], in1=hgnw[:], op=ALU.mult), reads=[r_sq_t[b], r_c6], writes=[r_sq_t[b]])
            S.op('dve', lambda e: e.tensor_tensor(out=mix[b][:, 0:512], in0=sq_t[b][:], in1=gg_t[b][:], op=ALU.mult),
                 reads=[r_sq_t[b], r_gg_t[b]], writes=[r_mix[b]])
            S.op('dve', lambda e: e.tensor_reduce(out=st2_t[b][:, 0:4], in_=v4(om_t[b][:]), axis=AX.X, op=ALU.add), reads=[r_om_t[b]], writes=[r_st2_t[b]])
            S.op('pool', lambda e: e.tensor_scalar(out=st2_t[b][:, 0:4], in0=st2_t[b][:, 0:4], scalar1=1.0 / 128, scalar2=None, op0=ALU.mult),
                 reads=[r_st2_t[b]], writes=[r_st2_t[b]])
            S.op('pool', lambda e: e.tensor_tensor(out=v4(om_t[b][:]), in0=v4(om_t[b][:]), in1=st2_t[b][:, 0:4].unsqueeze(2).to_broadcast([128, 4, 128]),
                                                   op=ALU.subtract), reads=[r_om_t[b], r_st2_t[b]], writes=[r_om_t[b]])
            S.op('pool', lambda e: e.tensor_tensor(out=sq2_t[b][:], in0=om_t[b][:], in1=om_t[b][:], op=ALU.mult), reads=[r_om_t[b]], writes=[r_sq2_t[b]])
            S.op('dve', lambda e: e.tensor_reduce(out=st2_t[b][:, 4:8], in_=v4(sq2_t[b][:]), axis=AX.X, op=ALU.add), reads=[r_sq2_t[b]], writes=[r_st2_t[b]])
            S.op('act', lambda e: e.activation(out=st2_t[b][:, 8:12], in_=st2_t[b][:, 4:8], func=AF.Sqrt, scale=1.0 / 128, bias=epst[:, 0:1]),
                 reads=[r_st2_t[b], r_epst], writes=[r_st2_t[b]])
            S.op('dve', lambda e: e.reciprocal(out=st2_t[b][:, 12:16], in_=st2_t[b][:, 8:12]), reads=[r_st2_t[b]], writes=[r_st2_t[b]])
            S.op('pool', lambda e: e.tensor_tensor(out=v4(sq2_t[b][:]), in0=v4(om_t[b][:]), in1=st2_t[b][:, 12:16].unsqueeze(2).to_broadcast([128, 4, 128]),
                                                   op=ALU.mult), reads=[r_om_t[b], r_st2_t[b]], writes=[r_sq2_t[b]])
            S.op('pool', lambda e: e.tensor_tensor(out=sq2_t[b][:], in0=sq2_t[b][:], in1=mlnw[:], op=ALU.mult), reads=[r_sq2_t[b], r_c6], writes=[r_sq2_t[b]])
            S.op('pool', lambda e: e.tensor_tensor(out=mix[b][:, 512:1024], in0=sq2_t[b][:], in1=mo_t[b][:], op=ALU.mult),
                 reads=[r_sq2_t[b], r_mo_t[b]], writes=[r_mix[b]])

        def p6_B(i):
            b = i % 2
            xr = slice(i * 128, (i + 1) * 128)
            for k in range(8):
                S.op('pe', (lambda k: lambda e: e.transpose(out=ps_tr[b][:, k * 128:(k + 1) * 128], in_=mix[b][:, k * 128:(k + 1) * 128], identity=identb[:]))(k),
                     reads=[r_mix[b], r_identb], writes=[r_pstr[b]])
            S.op('act', lambda e: e.activation(out=mixT[b][:], in_=ps_tr[b][:, :], func=AF.Copy), reads=[r_pstr[b]], writes=[r_mixT[b]])
            for cb in range(2):
                pi = 2 if cb == 0 else 0
                pt = ps_mm[2] if cb == 0 else ps_mod[b]
                rp = r_psmm[2] if cb == 0 else r_psmod[b]
                for k in range(8):
                    S.op('pe', (lambda k, cb, pt: lambda e: e.matmul(pt[:, :], lhsT=mixT[b][:, k * 128:(k + 1) * 128], rhs=WOG[:, k, cb * 512:(cb + 1) * 512],
                                                                     start=(k == 0), stop=(k == 7)))(k, cb, pt),
                         reads=[r_mixT[b], r_c6], writes=[rp])
                S.op('dve', (lambda cb, pt: lambda e: e.tensor_tensor(out=xm[b][:, cb * 512:(cb + 1) * 512], in0=pt[:, :], in1=x_t[b][:, cb * 512:(cb + 1) * 512],
                                                                      op=ALU.add))(cb, pt),
                     reads=[rp, r_x_t[b]], writes=[r_xm[b]])
            S.op('sp', lambda e: e.dma_start(out=XMID[xr, :], in_=xm[b][:]), reads=[r_xm[b]], writes=[r_xmid], chan='6xm%d' % b)

        def p6_C(i):
            b = i % 2
            xr = slice(i * 128, (i + 1) * 128)
            S.op('act', lambda e: e.activation(out=junk6[:], in_=xm[b][:], func=AF.Square, accum_out=sm_t[b][:, 0:1]),
                 reads=[r_xm[b]], writes=[r_junk6, r_sm_t[b]])
            S.op('act', lambda e: e.activation(out=sm_t[b][:, 1:2], in_=sm_t[b][:, 0:1], func=AF.Sqrt, scale=1.0 / D, bias=epst[:, 0:1]),
                 reads=[r_sm_t[b], r_epst], writes=[r_sm_t[b]])
            S.op('dve', lambda e: e.reciprocal(out=sm_t[b][:, 2:3], in_=sm_t[b][:, 1:2]), reads=[r_sm_t[b]], writes=[r_sm_t[b]])
            S.op('dve', lambda e: e.scalar_tensor_tensor(out=vx[b][:], in0=xm[b][:], scalar=sm_t[b][:, 2:3], in1=SC2R[:], op0=ALU.mult, op1=ALU.mult),
                 reads=[r_xm[b], r_sm_t[b], r_c6], writes=[r_vx[b]])
            S.op('pool', lambda e: e.tensor_tensor(out=vx[b][:], in0=vx[b][:], in1=SH2R[:], op=ALU.add), reads=[r_vx[b], r_c6], writes=[r_vx[b]])
            S.op('act', lambda e: e.activation(out=vxb[b][:], in_=vx[b][:], func=AF.Copy), reads=[r_vx[b]], writes=[r_vxb[b]])
            S.op('sp', lambda e: e.dma_start(out=VX[xr, :], in_=vxb[b][:]), reads=[r_vxb[b]], writes=[r_vxd], chan='6vx%d' % b)
            for k in range(8):
                pt = ps_mm[k // 4]
                rp = r_psmm[k // 4]
                S.op('pe', (lambda k, pt: lambda e: e.transpose(out=pt[:, (k % 4) * 128:(k % 4 + 1) * 128], in_=vx[b][:, k * 128:(k + 1) * 128], identity=ident[:]))(k, pt),
                     reads=[r_vx[b], r_ident], writes=[rp])
            S.op('act', lambda e: e.activation(out=vxT[b][:, 0:512], in_=ps_mm[0][:, :], func=AF.Copy), reads=[r_psmm[0]], writes=[r_vxT[b]])
            S.op('dve', lambda e: e.tensor_copy(out=vxT[b][:, 512:1024], in_=ps_mm[1][:, :]), reads=[r_psmm[1]], writes=[r_vxT[b]])
            pr = ps_x[:, 0:16]
            for k in range(8):
                S.op('pe', (lambda k: lambda e: e.matmul(pr, lhsT=vxT[b][:, k * 128:(k + 1) * 128], rhs=RW[:, k, :], start=(k == 0), stop=(k == 7)))(k),
                     reads=[r_vxT[b], r_c6], writes=[r_psx])
            S.op('dve', lambda e: e.tensor_reduce(out=sm_t[b][:, 3:4], in_=pr, axis=AX.X, op=ALU.max), reads=[r_psx], writes=[r_sm_t[b]])
            S.op('dve', lambda e: e.tensor_scalar(out=sm_t[b][:, 4:5], in0=sm_t[b][:, 3:4], scalar1=-1.0, scalar2=None, op0=ALU.mult),
                 reads=[r_sm_t[b]], writes=[r_sm_t[b]])
            S.op('act', lambda e: e.activation(out=ex_t[b][:], in_=pr, func=AF.Exp, bias=sm_t[b][:, 4:5], accum_out=sm_t[b][:, 5:6]),
                 reads=[r_psx, r_sm_t[b]], writes=[r_ex_t[b], r_sm_t[b]])
            S.op('dve', lambda e: e.reciprocal(out=sm_t[b][:, 6:7], in_=sm_t[b][:, 5:6]), reads=[r_sm_t[b]], writes=[r_sm_t[b]])
            S.op('dve', lambda e: e.tensor_scalar(out=aff_t[b][:], in0=ex_t[b][:], scalar1=sm_t[b][:, 6:7], scalar2=None, op0=ALU.mult),
                 reads=[r_ex_t[b], r_sm_t[b]], writes=[r_aff_t[b]])
            S.op('sp', lambda e: e.dma_start(out=AFF[xr, :], in_=aff_t[b][:]), reads=[r_aff_t[b]], writes=[r_affd], chan='6af%d' % b)
            S.op('pe', lambda e: e.transpose(out=ps_x[0:16, 128:256], in_=aff_t[b][:, :], identity=ident[:]), reads=[r_aff_t[b], r_ident], writes=[r_psx2])
            S.op('act', lambda e: e.activation(out=AFFT[:, i * 128:(i + 1) * 128], in_=ps_x[0:16, 128:256], func=AF.Copy), reads=[r_psx2], writes=[r_AFFT])

        p6_loads(0)
        for i in range(NXT + 2):
            if 0 <= i - 1 < NXT:
                p6_B(i - 1)
            if 0 <= i - 2 < NXT:
                p6_C(i - 2)
            if i + 1 < NXT:
                p6_loads(i + 1)
            if i < NXT:
                p6_A(i)
        S.op('sp', lambda e: e.dma_start(out=AFFTd, in_=AFFT[:]), reads=[r_AFFT], writes=[r_affd], chan='6afT')
        S.barrier()
        A.reset(m6)

    if stage >= 7:
        A.reset(m_keep)
        m7 = A.mark()
        svt = A.tile([128, 8], F32, 'svt')
        CTB = A.tile([128, 16, 64], F32, 'CTB')
        zero1 = A.tile([128, 1], F32, 'zero1')
        TIDX = A.tile([128, NE, 8], U32, 'TIDX')
        GATE = A.tile([128, NE, 8], F32, 'GATE')
        m7b = A.mark()
        A128 = A.tile([128, 1024], F32, 'A128')
        cmpb = A.tile([128, 1024], F32, 'cmpb')
        cum = A.tile([128, 1024], F32, 'cum')
        BD = A.tile([128, 128], F32, 'BD')
        LT = A.tile([128, 128], F32, 'LT')
        bs = A.tile([128, 16], F32, 'bs')
        CTe = A.tile([128, 8], F32, 'CTe')
        r_7 = R()
        r_bs = R()
        r_cmp = R()
        r_cum = R()
        r_cumd = R()
        r_tidx = R()
        S.op('sp', lambda e: e.dma_start(out=A128[:], in_=AFFTd.rearrange("e (g i) -> (e g) i", g=8)), reads=[r_affd], writes=[r_7], chan='7a')
        S.op('sp', lambda e: e.dma_start(out=BD[:], in_=bd_in), writes=[r_7], chan='7b')
        S.op('sp', lambda e: e.dma_start(out=LT[:], in_=lt_in), writes=[r_7], chan='7c')
        S.op('sp', lambda e: e.dma_start(out=svt[:], in_=sv_in), writes=[r_7], chan='7d')
        S.op('dve', lambda e: e.memset(bs[:], 0.0), writes=[r_bs])
        S.op('dve', lambda e: e.memset(bs[:, 1:2], 1.0), reads=[r_bs], writes=[r_bs])
        S.op('dve', lambda e: e.memset(zero1[:], 0.0), writes=[r_7])
        pc = ps_x[:, 256:257]
        pc2 = ps_x[:, 256:258]
        r_pc = r_psx
        for it in range(30):
            S.op('dve', lambda e: e.tensor_tensor(out=bs[:, 2:3], in0=bs[:, 0:1], in1=bs[:, 1:2], op=ALU.add), reads=[r_bs], writes=[r_bs])
            S.op('dve', lambda e: e.tensor_scalar(out=bs[:, 2:3], in0=bs[:, 2:3], scalar1=0.5, scalar2=None, op0=ALU.mult), reads=[r_bs], writes=[r_bs])
            S.op('dve', lambda e: e.tensor_scalar(out=cmpb[:], in0=A128[:], scalar1=bs[:, 2:3], scalar2=None, op0=ALU.is_ge),
                 reads=[r_7, r_bs], writes=[r_cmp])
            S.op('dve', lambda e: e.tensor_reduce(out=bs[:, 3:4], in_=cmpb[:], axis=AX.X, op=ALU.add), reads=[r_cmp, r_bs], writes=[r_bs])
            S.op('pe', lambda e: e.matmul(pc2, lhsT=BD[:], rhs=bs[:, 3:5], start=True, stop=True), reads=[r_7, r_bs], writes=[r_pc])
            S.op('dve', lambda e: e.tensor_scalar(out=bs[:, 4:5], in0=pc, scalar1=float(CAP), scalar2=None, op0=ALU.is_ge), reads=[r_pc, r_bs], writes=[r_bs])
            S.op('dve', lambda e: e.tensor_tensor(out=bs[:, 5:6], in0=bs[:, 2:3], in1=bs[:, 0:1], op=ALU.subtract), reads=[r_bs], writes=[r_bs])
            S.op('dve', lambda e: e.tensor_tensor(out=bs[:, 6:7], in0=bs[:, 1:2], in1=bs[:, 2:3], op=ALU.subtract), reads=[r_bs], writes=[r_bs])
            S.op('dve', lambda e: e.scalar_tensor_tensor(out=bs[:, 0:1], in0=bs[:, 5:6], scalar=bs[:, 4:5], in1=bs[:, 0:1], op0=ALU.mult, op1=ALU.add),
                 reads=[r_bs], writes=[r_bs])
            S.op('dve', lambda e: e.scalar_tensor_tensor(out=bs[:, 1:2], in0=bs[:, 6:7], scalar=bs[:, 4:5], in1=bs[:, 2:3], op0=ALU.mult, op1=ALU.add),
                 reads=[r_bs], writes=[r_bs])
        S.op('dve', lambda e: e.tensor_scalar(out=cmpb[:], in0=A128[:], scalar1=bs[:, 0:1], scalar2=None, op0=ALU.is_ge), reads=[r_7, r_bs], writes=[r_cmp])
        S.op('dve', lambda e: e.tensor_reduce(out=bs[:, 3:4], in_=cmpb[:], axis=AX.X, op=ALU.add), reads=[r_cmp, r_bs], writes=[r_bs])
        S.op('pe', lambda e: e.matmul(pc2, lhsT=LT[:], rhs=bs[:, 3:5], start=True, stop=True), reads=[r_7, r_bs], writes=[r_pc])
        S.op('dve', lambda e: e.tensor_copy(out=bs[:, 7:8], in_=pc), reads=[r_pc, r_bs], writes=[r_bs])
        S.op('dve', lambda e: e.tensor_tensor_scan(out=cum[:], data0=cmpb[:], data1=cmpb[:], initial=bs[:, 7:8],
                                                   op0=ALU.add, op1=ALU.max), reads=[r_cmp, r_bs, r_7], writes=[r_cum])
        S.op('sp', lambda e: e.dma_start(out=CUMd2, in_=cum[:]), reads=[r_cum], writes=[r_cumd], chan='7e')
        S.op('dve', lambda e: e.tensor_copy(out=CTe[:], in_=cum[:, 127:1024:128]), reads=[r_cum], writes=[r_cum])
        S.op('sp', lambda e: e.dma_start(out=CTd, in_=CTe[:]), reads=[r_cum], writes=[r_cumd], chan='7f')
        S.op('sp', lambda e: e.dma_start(out=CTB[:].rearrange("p a b -> p (a b)"), in_=CTd.rearrange("a b -> (a b)").partition_broadcast(128)),
             reads=[r_cumd], writes=[r_7], chan='7g')
        S.barrier()
        A.reset(m7b)
        r_tidx_e = [R() for _ in range(NE)]
        wkb, r_wkb = [A.tile([128, 32], F32, 'wkb') for _ in range(2)], [R(), R()]
        wkub, r_wkub = [A.tile([128, 8], U32, 'wkub') for _ in range(2)], [R(), R()]
        c64b, r_c64b = [A.tile([128, 8, 64], F32, 'c64b') for _ in range(2)], [R(), R()]
        crowb, r_crowb = [A.tile([128, 8, 128], F32, 'crowb') for _ in range(2)], [R(), R()]
        c128b, r_c128b = crowb, r_crowb
        growb, r_growb = [A.tile([128, 8, 16], F32, 'growb') for _ in range(2)], [R(), R()]

        def route(ex):
            b = ex % 2
            S.op('dve', lambda e: e.tensor_tensor(out=c64b[b][:], in0=CTB[:, ex:ex + 1, :].to_broadcast([128, 8, 64]),
                                                  in1=svt[:].unsqueeze(2).to_broadcast([128, 8, 64]), op=ALU.is_le),
                 reads=[r_7], writes=[r_c64b[b]])
            S.op('dve', lambda e: e.tensor_reduce(out=wkb[b][:, 0:8], in_=c64b[b][:], axis=AX.X, op=ALU.add), reads=[r_c64b[b]], writes=[r_wkb[b]])
            S.op('dve', lambda e: e.tensor_scalar(out=wkb[b][:, 8:16], in0=wkb[b][:, 0:8], scalar1=float(ex * 64), scalar2=None, op0=ALU.add),
                 reads=[r_wkb[b]], writes=[r_wkb[b]])
            S.op('dve', lambda e: e.tensor_copy(out=wkub[b][:], in_=wkb[b][:, 8:16]), reads=[r_wkb[b]], writes=[r_wkub[b]])
            for st in range(8):
                S.op('pool', (lambda st: lambda e: e.indirect_dma_start(out=crowb[b][:, st, :], out_offset=None, in_=CUMd3,
                                                                       in_offset=bass.IndirectOffsetOnAxis(ap=wkub[b][:, st:st + 1], axis=0)))(st),
                     reads=[r_wkub[b], r_cumd], writes=[r_crowb[b]], chan='7h%d_%d' % (b, st))
            S.op('dve', lambda e: e.tensor_tensor(out=c128b[b][:], in0=crowb[b][:], in1=svt[:].unsqueeze(2).to_broadcast([128, 8, 128]), op=ALU.is_le),
                 reads=[r_crowb[b], r_7], writes=[r_c128b[b]])
            S.op('dve', lambda e: e.tensor_reduce(out=wkb[b][:, 16:24], in_=c128b[b][:], axis=AX.X, op=ALU.add), reads=[r_c128b[b], r_wkb[b]], writes=[r_wkb[b]])
            S.op('dve', lambda e: e.scalar_tensor_tensor(out=wkb[b][:, 24:32], in0=wkb[b][:, 0:8], scalar=128.0, in1=wkb[b][:, 16:24], op0=ALU.mult, op1=ALU.add),
                 reads=[r_wkb[b]], writes=[r_wkb[b]])
            S.op('dve', lambda e: e.tensor_copy(out=TIDX[:, ex, :], in_=wkb[b][:, 24:32]), reads=[r_wkb[b]], writes=[r_tidx_e[ex]])
            for st in range(8):
                S.op('pool', (lambda st: lambda e: e.indirect_dma_start(out=growb[b][:, st, :], out_offset=None, in_=AFF,
                                                                       in_offset=bass.IndirectOffsetOnAxis(ap=TIDX[:, ex, st:st + 1], axis=0)))(st),
                     reads=[r_tidx_e[ex], r_affd], writes=[r_growb[b]], chan='7i%d_%d' % (b, st))
            S.op('dve', lambda e: e.tensor_copy(out=GATE[:, ex, :], in_=growb[b][:, :, ex]), reads=[r_growb[b], r_tidx_e[ex]], writes=[r_tidx_e[ex]])

        if "TIDXd" in dbg:
            for ex in range(NE):
                route(ex)
            r_tidx = R()
            S.op('dve', lambda e: e.memset(zero1[:], 0.0), reads=[r_tidx_e[ex] for ex in range(NE)], writes=[r_tidx])
        if "TIDXd" in dbg:
            S.op('sp', lambda e: e.dma_start(out=TIDXd, in_=TIDX[:].rearrange("p a b -> p (a b)")), reads=[r_tidx], chan='7z')
            S.op('sp', lambda e: e.dma_start(out=GATEd, in_=GATE[:].rearrange("p a b -> p (a b)")), reads=[r_tidx], chan='7y')

        r_moe = R()
        zt8 = A.tile([128, D], F32, 'zt8')
        r_zt8 = R()
        S.op('pool', lambda e: e.memset(zt8[:], 0.0), writes=[r_zt8])
        for i in range(NXT):
            S.op('sp', (lambda i: lambda e: e.dma_start(out=MOE[i * 128:(i + 1) * 128, :], in_=zt8[:]))(i),
                 reads=[r_zt8], writes=[r_moe], chan='8z%d' % (i % 2))
        WG, r_WG = [A.tile([128, 8, D], BF16, 'WG') for _ in range(2)], [R(), R()]
        WU, r_WU = [A.tile([128, 8, D], BF16, 'WU') for _ in range(2)], [R(), R()]
        WD, r_WD = [A.tile([128, 8, D], BF16, 'WD') for _ in range(2)], [R(), R()]
        xs_t, r_xs_t = [A.tile([128, D], BF16, 'xs_t') for _ in range(8)], [R() for _ in range(8)]
        xsT = A.tile([128, 8, CAP], BF16, 'xsT')
        r_xsT = R()
        hT = A.tile([128, 8, CAP], BF16, 'hT')
        r_hT = R()
        sg, r_sg = [A.tile([128, 512], F32, 'sg') for _ in range(2)], [R(), R()]
        yb, r_yb = [A.tile([128, D], F32, 'yb') for _ in range(2)], [R(), R()]

        NWS = 6
        wst, r_wst = [A.tile([128, D], F32, 'wst') for _ in range(NWS)], [R() for _ in range(NWS)]
        wcnt = {'n': 0}

        def wgen(ex):
            b = ex % 2
            items = []
            for (src, dst, rr) in ((wg_in, WG, r_WG), (wu_in, WU, r_WU), (wd_in, WD, r_WD)):
                v = src[ex].rearrange("(k p) c -> p k c", p=128)
                for k in range(8):
                    items.append((v, dst, rr, k))

            def dma(n):
                v, dst, rr, k = items[n]
                i = (wcnt['n'] + n) % NWS
                S.op('sp', (lambda v, k, i: lambda e: e.dma_start(out=wst[i][:], in_=v[:, k, :]))(v, k, i), writes=[r_wst[i]], chan='8w%d' % i)
            for n in range(min(NWS, len(items))):
                dma(n)
            for n in range(len(items)):
                v, dst, rr, k = items[n]
                i = (wcnt['n'] + n) % NWS
                S.op('dve', (lambda dst, k, b, i: lambda e: e.tensor_copy(out=dst[b][:, k, :], in_=wst[i][:]))(dst, k, b, i),
                     reads=[r_wst[i]], writes=[rr[b]])
                if n + NWS < len(items):
                    dma(n + NWS)
                yield
            wcnt['n'] += len(items)

        def step(g):
            if g is not None:
                try:
                    next(g)
                except StopIteration:
                    pass

        def gathers(ex):
            for st in range(8):
                S.op('pool', (lambda st: lambda e: e.indirect_dma_start(out=xs_t[st][:], out_offset=None, in_=VX,
                                                                       in_offset=bass.IndirectOffsetOnAxis(ap=TIDX[:, ex, st:st + 1], axis=0)))(st),
                     reads=[r_tidx_e[ex], r_vxd], writes=[r_xs_t[st]], chan='8g%d' % st)

        for _ in wgen(0):
            pass
        n8 = 0
        if "TIDXd" not in dbg:
            route(0)
        gathers(0)
        for ex in range(NE):
            wg = None
            if ex + 1 < NE:
                if "TIDXd" not in dbg:
                    route(ex + 1)
                wg = wgen(ex + 1)
            wb = ex % 2
            for st in range(8):
                b = n8 % 2
                n8 += 1

                def gat(ex=ex, st=st, b=b):
                    for k in range(8):
                        S.op('pe', (lambda k: lambda e: e.transpose(out=ps_tr[b][:, k * 128:(k + 1) * 128], in_=xs_t[st][:, k * 128:(k + 1) * 128],
                                                                    identity=identb[:]))(k),
                             reads=[r_xs_t[st], r_identb], writes=[r_pstr[b]])
                    S.op('act' if b else 'dve',
                         (lambda e: e.activation(out=xsT[:, :, st * 128:(st + 1) * 128], in_=ps_tr[b][:, :].rearrange("p (k t) -> p k t", k=8), func=AF.Copy)) if b else
                         (lambda e: e.tensor_copy(out=xsT[:, :, st * 128:(st + 1) * 128], in_=ps_tr[b][:, :].rearrange("p (k t) -> p k t", k=8))),
                         reads=[r_pstr[b]], writes=[r_xsT])
                gat()
            n_gu = 0
            for fc in range(8):
                for sbk in range(2):
                    q = n_gu % 2
                    n_gu += 1

                    def gu(fc=fc, sbk=sbk, q=q, wb=wb):
                        pg = ps_mm[q]
                        pu = ps_mod[q]
                        cs = slice(sbk * 512, (sbk + 1) * 512)
                        for k in range(8):
                            S.op('pe', (lambda k: lambda e: e.matmul(pg[:, :], lhsT=WG[wb][:, k, fc * 128:(fc + 1) * 128], rhs=xsT[:, k, cs],
                                                                     start=(k == 0), stop=(k == 7)))(k),
                                 reads=[r_WG[wb], r_xsT], writes=[r_psmm[q]])
                        for k in range(8):
                            S.op('pe', (lambda k: lambda e: e.matmul(pu[:, :], lhsT=WU[wb][:, k, fc * 128:(fc + 1) * 128], rhs=xsT[:, k, cs],
                                                                     start=(k == 0), stop=(k == 7)))(k),
                                 reads=[r_WU[wb], r_xsT], writes=[r_psmod[q]])
                        S.op('act', lambda e: e.activation(out=sg[q][:], in_=pg[:, :], func=AF.Silu), reads=[r_psmm[q]], writes=[r_sg[q]])
                        S.op('dve', lambda e: e.tensor_tensor(out=hT[:, fc, cs], in0=pu[:, :], in1=sg[q][:], op=ALU.mult),
                             reads=[r_psmod[q], r_sg[q]], writes=[r_hT])
                    gu()
                    step(wg)
            if ex + 1 < NE:
                gathers(ex + 1)
            for st in range(8):
                b = st % 2

                def dn_(ex=ex, st=st, b=b, wb=wb):
                    for cb in range(2):
                        pt = ps_mm[2] if cb == 0 else ps_x
                        rp = r_psmm[2] if cb == 0 else r_psx
                        for fc in range(8):
                            S.op('pe', (lambda fc, cb, pt: lambda e: e.matmul(pt[:, :], lhsT=hT[:, fc, st * 128:(st + 1) * 128],
                                                                              rhs=WD[wb][:, fc, cb * 512:(cb + 1) * 512],
                                                                              start=(fc == 0), stop=(fc == 7)))(fc, cb, pt),
                                 reads=[r_hT, r_WD[wb]], writes=[rp])
                        S.op('act', (lambda cb, pt: lambda e: e.activation(out=yb[b][:, cb * 512:(cb + 1) * 512], in_=pt[:, :], func=AF.Copy,
                                                                           scale=GATE[:, ex, st:st + 1]))(cb, pt),
                             reads=[rp, r_tidx_e[ex]], writes=[r_yb[b]])
                    S.op('pool', lambda e: e.indirect_dma_start(out=MOE, out_offset=bass.IndirectOffsetOnAxis(ap=TIDX[:, ex, st:st + 1], axis=0),
                                                                in_=yb[b][:], in_offset=None, compute_op=ALU.add),
                         reads=[r_yb[b], r_tidx_e[ex], r_moe], writes=[r_moe], chan='8s')
                dn_()
                step(wg)
            if wg is not None:
                for _ in wg:
                    pass
        S.barrier()
        A.reset(m7)

    if stage >= 9:
        m9 = A.mark()
        G2R = A.tile([128, D], F32, 'G2R')
        fnw = A.tile([128, D], F32, 'fnw')
        MOD3 = A.tile([2, 6 * D], F32, 'MOD3')
        r_9 = R()
        S.op('sp', lambda e: e.dma_start(out=MOD3[:], in_=MODd), reads=[r_modd], writes=[r_9], chan='9a')
        S.op('sp', lambda e: e.dma_start(out=fnw[:], in_=fnw_in), writes=[r_9], chan='9b')
        for hb in range(2):
            S.op('pe', (lambda hb: lambda e: e.matmul(ps_mm[hb][:, :], lhsT=sel2[0:2, :], rhs=MOD3[0:2, 5120 + hb * 512:5120 + (hb + 1) * 512],
                                                      start=True, stop=True))(hb),
                 reads=[r_sel2, r_9], writes=[r_psmm[hb]])
            S.op('dve', (lambda hb: lambda e: e.tensor_copy(out=G2R[:, hb * 512:(hb + 1) * 512], in_=ps_mm[hb][:, :]))(hb),
                 reads=[r_psmm[hb]], writes=[r_9])
        xm9, r_xm9 = [A.tile([128, D], F32, 'xm9') for _ in range(2)], [R(), R()]
        mo9, r_mo9 = [A.tile([128, D], F32, 'mo9') for _ in range(2)], [R(), R()]
        y9, r_y9 = [A.tile([128, D], F32, 'y9') for _ in range(2)], [R(), R()]
        s9, r_s9 = [A.tile([128, 4], F32, 's9') for _ in range(2)], [R(), R()]
        junk9 = A.tile([128, D], BF16, 'junk9')
        r_j9 = R()
        for i in range(NXT + 1):
            if i < NXT:
                b = i % 2
                xr = slice(i * 128, (i + 1) * 128)
                S.op('sp', (lambda b, xr: lambda e: e.dma_start(out=xm9[b][:], in_=XMID[xr, :]))(b, xr), reads=[r_xmid], writes=[r_xm9[b]], chan='9x%d' % b)
                S.op('sp', (lambda b, xr: lambda e: e.dma_start(out=mo9[b][:], in_=MOE[xr, :]))(b, xr), reads=[r_moe], writes=[r_mo9[b]], chan='9m%d' % b)
            if i > 0:
                j = i - 1
                b = j % 2
                xr = slice(j * 128, (j + 1) * 128)

                def fin(b=b, xr=xr):
                    S.op('dve', lambda e: e.tensor_tensor(out=mo9[b][:], in0=mo9[b][:], in1=G2R[:], op=ALU.mult), reads=[r_mo9[b], r_9], writes=[r_mo9[b]])
                    S.op('pool', lambda e: e.tensor_tensor(out=xm9[b][:], in0=xm9[b][:], in1=mo9[b][:], op=ALU.add), reads=[r_mo9[b], r_xm9[b]], writes=[r_xm9[b]])
                    S.op('act', lambda e: e.activation(out=junk9[:], in_=xm9[b][:], func=AF.Square, accum_out=s9[b][:, 0:1]),
                         reads=[r_xm9[b]], writes=[r_j9, r_s9[b]])
                    S.op('act', lambda e: e.activation(out=s9[b][:, 1:2], in_=s9[b][:, 0:1], func=AF.Sqrt, scale=1.0 / D, bias=epst[:, 0:1]),
                         reads=[r_s9[b], r_epst], writes=[r_s9[b]])
                    S.op('dve', lambda e: e.reciprocal(out=s9[b][:, 2:3], in_=s9[b][:, 1:2]), reads=[r_s9[b]], writes=[r_s9[b]])
                    S.op('dve', lambda e: e.scalar_tensor_tensor(out=y9[b][:], in0=xm9[b][:], scalar=s9[b][:, 2:3], in1=fnw[:], op0=ALU.mult, op1=ALU.mult),
                         reads=[r_xm9[b], r_s9[b], r_9], writes=[r_y9[b]])
                    S.op('sp', lambda e: e.dma_start(out=out[xr, :], in_=y9[b][:]), reads=[r_y9[b]], chan='9o%d' % b)
                fin()

    if stage < 99:
        zt = A.tile([128, D], F32, 'zt')
        r_zt = R()
        S.op('dve', lambda e: e.memset(zt[:], 0.0), writes=[r_zt])
        S.op('sp', lambda e: e.dma_start(out=out[0:128, :], in_=zt[:]), reads=[r_zt], chan='c_out')

    S.emit()
    return nc


def prep_inputs(inp, b):
    f = np.float32
    x, c, ctx, c_ctx = inp['x'], inp['c'], inp['ctx'], inp['c_ctx']
    m = {}
    m['xc'] = np.ascontiguousarray(np.concatenate([ctx[b], x[b]], axis=0), dtype=f)
    cv = np.stack([c[b], c_ctx], axis=-1).reshape(8, 128, 2).transpose(1, 0, 2)
    m['cvec'] = np.ascontiguousarray(cv, dtype=f)
    m['ada_w'] = np.ascontiguousarray(inp['ada_w'][0], dtype=f)
    m['ada_b2'] = np.ascontiguousarray(np.tile(inp['ada_b'][0][None, :], (2, 1)), dtype=f)
    m['n1w'] = np.ascontiguousarray(inp['norm1_w'][0].reshape(8, 128).T, dtype=f)
    m['w_in'] = np.ascontiguousarray(inp['w_in'][0], dtype=f)
    m['cident'] = np.eye(128, dtype=f)
    sel = np.zeros((2, 128), f)
    sel[0] = 1.0
    m['csel'] = sel
    m['lbl'] = np.ascontiguousarray(np.tile(inp['hg_lb_logits'].reshape(1, 2048), (128, 1)), dtype=f)
    m['gateb'] = np.ascontiguousarray(inp['ml_gate_b'][0].reshape(16, 1), dtype=f)
    m['gatebrow'] = np.ascontiguousarray(np.tile(inp['ml_gate_b'][0][None, :], (128, 1)), dtype=f)
    m['jm_in'] = np.ascontiguousarray(np.eye(128, dtype=f)[::-1])
    cw = inp['conv_w'][0].reshape(9, 8, 128).transpose(2, 1, 0)
    m['convw'] = np.ascontiguousarray(cw, dtype=f)
    m['convb'] = np.ascontiguousarray(inp['conv_b'][0].reshape(8, 128).T, dtype=f)
    jj, ii = np.meshgrid(np.arange(128), np.arange(128), indexing='ij')
    mask = np.stack([(jj <= ii), (jj >= ii)], axis=1).astype(f)
    m['maskin'] = np.ascontiguousarray(mask)
    trix = np.zeros((128, 2, 132), f)
    for p, (tri, mid) in enumerate([((jj <= ii).astype(f), 63), ((jj >= ii).astype(f), 64)]):
        trix[:, p, 0:128] = tri - tri[:, mid:mid + 1]
        trix[:, p, 128] = 1.0 - tri[:, mid]
        trix[:, p, 129] = tri[:, mid]
        trix[:, p, 130] = 1.0
    m['trixin'] = trix
    selh = np.zeros((4, 4, 128), f)
    for h in range(4):
        selh[h, h, :] = 1.0
    m['selhin'] = selh
    kk_, mm_ = np.meshgrid(np.arange(128), np.arange(128), indexing='ij')
    m['bd_in'] = np.ascontiguousarray((kk_ // 8 == mm_ // 8).astype(f))
    m['lt_in'] = np.ascontiguousarray(((kk_ // 8 == mm_ // 8) & (kk_ % 8 < mm_ % 8)).astype(f))
    m['sv_in'] = np.ascontiguousarray((np.arange(8)[None, :] * 128 + np.arange(128)[:, None]).astype(f))
    m['wg_in'] = np.ascontiguousarray(inp['exp_w_gate'][0], dtype=f)
    m['wu_in'] = np.ascontiguousarray(inp['exp_w_up'][0], dtype=f)
    m['wd_in'] = np.ascontiguousarray(inp['exp_w_down'][0], dtype=f)
    m['fnw_in'] = np.ascontiguousarray(np.tile(inp['final_norm_w'][None, :], (128, 1)), dtype=f)
    m['hgnw_in'] = np.ascontiguousarray(np.tile(inp['hg_norm_w'][0][None, :], (128, 1)), dtype=f)
    m['mlnw_in'] = np.ascontiguousarray(np.tile(inp['ml_norm_w'][0][None, :], (128, 1)), dtype=f)
    m['n2r_in'] = np.ascontiguousarray(np.tile(inp['norm2_w'][0][None, :], (128, 1)), dtype=f)
    m['rw_in'] = np.ascontiguousarray(inp['router_w'][0], dtype=f)
    m['wout_in'] = np.ascontiguousarray(inp['w_out'][0], dtype=f)
    return m


def kernel(**inputs):
    inp = {k: np.asarray(v) for k, v in inputs.items()}
    nc = build()
    in_maps = [prep_inputs(inp, c % 4) for c in range(8)]
    res = run_bass_kernel_spmd(nc, in_maps, core_ids=list(range(8)))
    outs = [res.results[c]["out"] for c in range(4)]
    return np.stack(outs, axis=0).astype(np.float32)
```
